# Optimizing a Trainium2 kernel written in Bass

```python
import math
import jax
import jax.numpy as jnp
from jax import lax
import numpy as np

D_MODEL = 2048
BATCH = 2
SEQ = 4096
DEPTH = 1

EPS = 1e-6
SSD_HEADS = 32
SSD_HEAD_DIM = 64
SSD_INNER = SSD_HEADS * SSD_HEAD_DIM
SSD_GROUPS = 4
SSD_STATE = 128
SSD_CONV = 4
SSD_CHUNK = 128
SSD_XBC = SSD_INNER + 2 * SSD_GROUPS * SSD_STATE
SB_HEADS = 16
SB_HEAD_DIM = 128
SB_INNER = SB_HEADS * SB_HEAD_DIM
SB_BLOCK = 128
IN_SPLITS = (SSD_INNER, SSD_XBC, SSD_HEADS, SB_INNER, SB_INNER, SB_INNER, D_MODEL, D_MODEL)
IN_DIM = SSD_INNER + SSD_XBC + SSD_HEADS + 3 * SB_INNER + 2 * D_MODEL
N_EXPERTS = 32
TOP_K = 4
D_EXPERT = D_MODEL
SWIGLU_LIMIT = 7.0
SWIGLU_ALPHA = 1.702
MOE_BLOCK = 256
PLE_DIM = 256

kernel_name = "hybrid_ssd_stickbreak_moe_block"


def rms_norm(x, g):
    xf = x.astype(jnp.float32)
    y = xf * lax.rsqrt(jnp.mean(xf * xf, axis=-1, keepdims=True) + EPS)
    return (y * g.astype(jnp.float32)).astype(x.dtype)


def ssd_mixer(z, xbc, dt_raw, conv_w, conv_b, dt_bias, a_log, d_skip, norm_w):
    f32 = jnp.float32
    b, l, _ = xbc.shape
    nc = l // SSD_CHUNK
    hpg = SSD_HEADS // SSD_GROUPS
    gn = SSD_GROUPS * SSD_STATE
    xbc = lax.conv_general_dilated(
        xbc, conv_w[:, None, :].astype(xbc.dtype), window_strides=(1,),
        padding=[(SSD_CONV - 1, 0)], dimension_numbers=("NWC", "WIO", "NWC"),
        feature_group_count=SSD_XBC)
    xbc = jax.nn.silu(xbc + conv_b)
    xs, bm, cm = jnp.split(xbc, [SSD_INNER, SSD_INNER + gn], axis=-1)
    dt = jax.nn.softplus((dt_raw + dt_bias).astype(f32))
    a = dt * (-jnp.exp(a_log.astype(f32)))
    xh = xs.reshape(b, l, SSD_HEADS, SSD_HEAD_DIM).astype(f32)
    X = (xh * dt[..., None]).reshape(b, nc, SSD_CHUNK, SSD_GROUPS, hpg, SSD_HEAD_DIM)
    A = a.reshape(b, nc, SSD_CHUNK, SSD_GROUPS, hpg).transpose(0, 3, 4, 1, 2)
    Bc = bm.reshape(b, nc, SSD_CHUNK, SSD_GROUPS, SSD_STATE).astype(f32)
    Cc = cm.reshape(b, nc, SSD_CHUNK, SSD_GROUPS, SSD_STATE).astype(f32)
    a_cs = jnp.cumsum(A, axis=-1)
    seg = a_cs[..., :, None] - a_cs[..., None, :]
    causal = jnp.tril(jnp.ones((SSD_CHUNK, SSD_CHUNK), dtype=bool))
    decay = jnp.exp(jnp.where(causal, seg, -jnp.inf))
    cb = jnp.einsum("bclgn,bcsgn->bgcls", Cc, Bc)
    y_diag = jnp.einsum("bgjcls,bcsgjp->bclgjp", cb[:, :, None] * decay, X)
    decay_states = jnp.exp(a_cs[..., -1:] - a_cs)
    states = jnp.einsum("bclgn,bgjcl,bclgjp->bcgjpn", Bc, decay_states, X)
    chunk_decay = jnp.exp(a_cs[..., -1])

    def step(carry, inp):
        dec, st = inp
        return dec[..., None, None] * carry + st, carry

    init = jnp.zeros((b, SSD_GROUPS, hpg, SSD_HEAD_DIM, SSD_STATE), f32)
    _, prev = lax.scan(step, init, (jnp.moveaxis(chunk_decay, -1, 0), jnp.moveaxis(states, 1, 0)))
    prev = jnp.moveaxis(prev, 0, 1)
    y_off = jnp.einsum("bclgn,bcgjpn,bgjcl->bclgjp", Cc, prev, jnp.exp(a_cs))
    y = (y_diag + y_off).reshape(b, l, SSD_HEADS, SSD_HEAD_DIM) + xh * d_skip.astype(f32)[:, None]
    y = y.reshape(b, l, SSD_INNER) * jax.nn.silu(z.astype(f32))
    yg = y.reshape(b, l, SSD_GROUPS, SSD_INNER // SSD_GROUPS)
    yg = yg * lax.rsqrt(jnp.mean(yg * yg, axis=-1, keepdims=True) + EPS)
    return (yg.reshape(b, l, SSD_INNER) * norm_w.astype(f32)).astype(z.dtype)


def stick_breaking_attention(q, k, v):
    b, l, H, d = q.shape
    nb = l // SB_BLOCK
    scale = d ** -0.5
    qb = q.reshape(b, nb, SB_BLOCK, H, d).transpose(1, 0, 2, 3, 4)
    key_pos = jnp.arange(l)

    def block(args):
        q_blk, i = args
        q_pos = i * SB_BLOCK + jnp.arange(SB_BLOCK)
        z = jnp.einsum("bqhd,bkhd->bhqk", q_blk, k).astype(jnp.float32) * scale
        strict = key_pos[None, :] < q_pos[:, None]
        log_beta = jax.nn.log_sigmoid(z)
        log_1m = jnp.where(strict, jax.nn.log_sigmoid(-z), 0.0)
        log_surv = lax.cumsum(log_1m, axis=3, reverse=True) - log_1m
        w = jnp.where(strict, jnp.exp(log_beta + log_surv), 0.0)
        return jnp.einsum("bhqk,bkhd->bqhd", w.astype(v.dtype), v)

    out = lax.map(block, (qb, jnp.arange(nb)))
    return out.transpose(1, 0, 2, 3, 4).reshape(b, l, H * d)


def moe_ffn(h, w_router, b_router, w_gu, b_gu, w_down, b_down):
    b, l, d = h.shape
    n_tok = b * l
    n_asg = n_tok * TOP_K
    t = h.reshape(n_tok, d)
    logits = (t @ w_router + b_router).astype(jnp.float32)
    top_v, top_e = lax.top_k(logits, TOP_K)
    top_w = jax.nn.softmax(top_v, axis=-1)
    e_flat = top_e.reshape(-1)
    w_flat = top_w.reshape(-1)
    tok_flat = jnp.arange(n_asg, dtype=jnp.int32) // TOP_K
    order = jnp.argsort(e_flat, stable=True)
    e_sorted = e_flat[order]
    counts = jnp.bincount(e_flat, length=N_EXPERTS)
    padded = (counts + MOE_BLOCK - 1) // MOE_BLOCK * MOE_BLOCK
    start = jnp.cumsum(counts) - counts
    pad_end = jnp.cumsum(padded)
    pad_start = pad_end - padded
    dest = pad_start[e_sorted] + (jnp.arange(n_asg) - start[e_sorted])
    n_blocks = (n_asg + N_EXPERTS * (MOE_BLOCK - 1) + MOE_BLOCK - 1) // MOE_BLOCK
    slot_tok = jnp.zeros((n_blocks * MOE_BLOCK,), jnp.int32).at[dest].set(tok_flat[order])
    slot_w = jnp.zeros((n_blocks * MOE_BLOCK,), jnp.float32).at[dest].set(w_flat[order])
    block_e = jnp.minimum(
        jnp.searchsorted(pad_end, jnp.arange(n_blocks) * MOE_BLOCK, side="right"), N_EXPERTS - 1)

    def expert_block(args):
        tok, e = args
        xb = t[tok]
        gu = xb @ w_gu[e] + b_gu[e]
        glu, lin = jnp.split(gu, 2, axis=-1)
        glu = jnp.minimum(glu, SWIGLU_LIMIT)
        lin = jnp.clip(lin, -SWIGLU_LIMIT, SWIGLU_LIMIT)
        act = glu * jax.nn.sigmoid(SWIGLU_ALPHA * glu) * (lin + 1.0)
        return act @ w_down[e] + b_down[e]

    out = lax.map(expert_block, (slot_tok.reshape(n_blocks, MOE_BLOCK), block_e))
    out = out.reshape(-1, d) * slot_w[:, None].astype(out.dtype)
    y = jnp.zeros((n_tok, d), out.dtype).at[slot_tok].add(out)
    return y.reshape(b, l, d)


def setup_inputs(seed: int = 0) -> dict:
    key = jax.random.key(seed)
    ks = jax.random.split(key, 25)
    f32 = jnp.float32
    L = DEPTH

    def nrm(k, shape, scale):
        return jax.random.normal(k, shape, f32) * scale

    def gain(k, shape):
        return 1.0 + 0.01 * jax.random.normal(k, shape, f32)

    x = nrm(ks[0], (BATCH, SEQ, D_MODEL), 1.0)
    p = nrm(ks[1], (L, BATCH, SEQ, PLE_DIM), 1.0)
    w_in = nrm(ks[2], (L, D_MODEL, IN_DIM), D_MODEL ** -0.5)
    conv_w = nrm(ks[3], (L, SSD_CONV, SSD_XBC), SSD_CONV ** -0.5)
    conv_b = nrm(ks[4], (L, SSD_XBC), 0.01)
    dt0 = jnp.exp(jax.random.uniform(ks[5], (L, SSD_HEADS), f32, math.log(1e-3), math.log(1e-1)))
    dt_bias = dt0 + jnp.log(-jnp.expm1(-dt0))
    a_log = jnp.log(jax.random.uniform(ks[6], (L, SSD_HEADS), f32, 1.0, 16.0))
    d_skip = gain(ks[7], (L, SSD_HEADS))
    ssd_norm_w = gain(ks[8], (L, SSD_INNER))
    w_branch_a = nrm(ks[9], (L, SSD_INNER, D_MODEL), SSD_INNER ** -0.5)
    w_branch_b = nrm(ks[10], (L, SB_INNER, D_MODEL), SB_INNER ** -0.5)
    w_out = nrm(ks[11], (L, D_MODEL, D_MODEL), D_MODEL ** -0.5)
    g_mix = gain(ks[12], (L, D_MODEL))
    g_ffn = gain(ks[13], (L, D_MODEL))
    w_router = nrm(ks[14], (L, D_MODEL, N_EXPERTS), D_MODEL ** -0.5)
    b_router = nrm(ks[15], (L, N_EXPERTS), 0.01)
    w_gate_up = nrm(ks[16], (L, N_EXPERTS, D_MODEL, 2 * D_EXPERT), D_MODEL ** -0.5)
    b_gate_up = nrm(ks[17], (L, N_EXPERTS, 2 * D_EXPERT), 0.01)
    w_down = nrm(ks[18], (L, N_EXPERTS, D_EXPERT, D_MODEL), D_EXPERT ** -0.5)
    b_down = nrm(ks[19], (L, N_EXPERTS, D_MODEL), 0.01)
    g_ple = gain(ks[20], (L, D_MODEL))
    w_ple_gate = nrm(ks[21], (L, D_MODEL, D_MODEL), D_MODEL ** -0.5)
    w_ple_proj = nrm(ks[22], (L, PLE_DIM, D_MODEL), PLE_DIM ** -0.5)
    g_ple_post = gain(ks[23], (L, D_MODEL))
    g_final = gain(ks[24], (D_MODEL,))
    return {"x": x, "p": p, "w_in": w_in, "conv_w": conv_w, "conv_b": conv_b,
            "dt_bias": dt_bias, "a_log": a_log, "d_skip": d_skip, "ssd_norm_w": ssd_norm_w,
            "w_branch_a": w_branch_a, "w_branch_b": w_branch_b, "w_out": w_out,
            "g_mix": g_mix, "g_ffn": g_ffn, "w_router": w_router, "b_router": b_router,
            "w_gate_up": w_gate_up, "b_gate_up": b_gate_up, "w_down": w_down, "b_down": b_down,
            "g_ple": g_ple, "w_ple_gate": w_ple_gate, "w_ple_proj": w_ple_proj,
            "g_ple_post": g_ple_post, "g_final": g_final}


def reference(x, p, w_in, conv_w, conv_b, dt_bias, a_log, d_skip, ssd_norm_w,
              w_branch_a, w_branch_b, w_out, g_mix, g_ffn, w_router, b_router,
              w_gate_up, b_gate_up, w_down, b_down, g_ple, w_ple_gate, w_ple_proj,
              g_ple_post, g_final):
    b, l, _ = x.shape
    cuts = np.cumsum(np.array(IN_SPLITS))[:-1].tolist()
    for i in range(DEPTH):
        h = rms_norm(x, g_mix[i])
        proj = h @ w_in[i]
        z, xbc, dt_raw, q, k, v, gate_a, gate_b = jnp.split(proj, cuts, axis=-1)
        y_a = ssd_mixer(z, xbc, dt_raw, conv_w[i], conv_b[i], dt_bias[i], a_log[i],
                        d_skip[i], ssd_norm_w[i])
        y_b = stick_breaking_attention(q.reshape(b, l, SB_HEADS, SB_HEAD_DIM),
                                       k.reshape(b, l, SB_HEADS, SB_HEAD_DIM),
                                       v.reshape(b, l, SB_HEADS, SB_HEAD_DIM))
        merged = (jax.nn.sigmoid(gate_a) * (y_a @ w_branch_a[i])
                  + jax.nn.sigmoid(gate_b) * (y_b @ w_branch_b[i]))
        x = x + merged @ w_out[i]
        x = x + moe_ffn(rms_norm(x, g_ffn[i]), w_router[i], b_router[i], w_gate_up[i],
                        b_gate_up[i], w_down[i], b_down[i])
        ple_gate = jax.nn.sigmoid(rms_norm(x, g_ple[i]) @ w_ple_gate[i])
        x = x + rms_norm(ple_gate * (p[i] @ w_ple_proj[i]), g_ple_post[i])
    return rms_norm(x, g_final)
```

```python
import numpy as np
from contextlib import ExitStack
import concourse.bass as bass
import concourse.mybir as mybir
from concourse.bass_utils import run_bass_kernel_spmd

F32 = mybir.dt.float32
BF16 = mybir.dt.bfloat16
AF = mybir.ActivationFunctionType
ALU = mybir.AluOpType
AX = mybir.AxisListType

ENGS = ("pe", "act", "dve", "pool", "sp")
NDMASEM = 8
D = 2048
SEQ = 4096
NE = 32
CAP = 256
EPS = 1e-6
IN_DIM = 15392
C_Z, C_XBC, C_DT, C_Q, C_K, C_V, C_GA, C_GB = 0, 2048, 5120, 5152, 7200, 9248, 11296, 13344


class Sched:
    def __init__(self, nc, stack):
        self.nc = nc
        self.ops = []
        self.last_w = {}
        self.rd_eng = {}
        self.rd_dma = {}
        self.stack = stack

    def add(self, eng, fn, r=(), w=(), dma=False, prio=None):
        oid = len(self.ops)
        deps = set()
        for k in r:
            d = self.last_w.get(k)
            if d is not None:
                deps.add(d)
        for k in w:
            d = self.last_w.get(k)
            if d is not None:
                deps.add(d)
            for d in self.rd_eng.get(k, {}).values():
                deps.add(d)
            for d in self.rd_dma.get(k, ()):
                deps.add(d)
        for k in w:
            self.last_w[k] = oid
            self.rd_eng[k] = {}
            self.rd_dma[k] = []
        for k in r:
            if dma:
                self.rd_dma.setdefault(k, []).append(oid)
            else:
                self.rd_eng.setdefault(k, {})[eng] = oid
        deps.discard(oid)
        self.ops.append(dict(eng=eng, fn=fn, deps=deps, dma=dma, prio=(oid if prio is None else prio)))
        return oid

    def wait_all(self, eng, ids):
        self.ops.append(dict(eng=eng, fn=None, deps=set(ids), dma=False, prio=len(self.ops)))

    def barrier(self):
        self.nbar = getattr(self, "nbar", 0) + 1
        last = {}
        dmas = []
        for i, o in enumerate(self.ops):
            if o["fn"] is None:
                continue
            if o["dma"]:
                dmas.append(i)
            else:
                last[o["eng"]] = i
        ids = list(last.values()) + dmas[-64:]
        for e in ENGS:
            self.wait_all(e, ids)

    def emit(self):
        nc = self.nc
        ops = self.ops

        def skip(p, o):
            return (not p["dma"]) and (not o["dma"]) and p["eng"] == o["eng"] and p["eng"] == "pe"

        need = [False] * len(ops)
        for o in ops:
            for d in o["deps"]:
                if not skip(ops[d], o):
                    need[d] = True
        per_eng = {e: [] for e in ENGS}
        for i, o in enumerate(ops):
            per_eng[o["eng"]].append(i)
        for e in ENGS:
            per_eng[e].sort(key=lambda i: (ops[i]["prio"], i))
        cnt = {e: 0 for e in ENGS}
        dcnt = {e: 0 for e in ENGS}
        for e in ENGS:
            for i in per_eng[e]:
                o = ops[i]
                o["sig"] = None
                if o["fn"] is None:
                    continue
                if o["dma"]:
                    n = dcnt[e]
                    dcnt[e] += 1
                    o["sig"] = ("d", e, n % NDMASEM, 16 * (n // NDMASEM + 1))
                elif need[i]:
                    cnt[e] += 1
                    o["sig"] = ("c", e, 0, cnt[e])
        sems = {}
        for e in ENGS:
            sems[("c", e, 0)] = self.stack.enter_context(nc.semaphore(f"c_{e}"))
            if dcnt[e] > 0:
                for k in range(NDMASEM):
                    sems[("d", e, k)] = self.stack.enter_context(nc.semaphore(f"d_{e}_{k}"))

        def run_engine(ename, handle):
            known = {}
            for i in per_eng[ename]:
                o = ops[i]
                waits = {}
                for d in o["deps"]:
                    p = ops[d]
                    if skip(p, o):
                        continue
                    s = p["sig"]
                    if waits.get(s[:3], 0) < s[3]:
                        waits[s[:3]] = s[3]
                for key, v in waits.items():
                    if known.get(key, 0) >= v:
                        continue
                    known[key] = v
                    handle.wait_ge(sems[key], v)
                if o["fn"] is None:
                    continue
                ins = o["fn"](handle)
                s = o["sig"]
                if s is not None:
                    ins.then_inc(sems[s[:3]], 16 if s[0] == "d" else 1)

        block = self.stack.enter_context(nc.Block())

        @block.tensor
        def _(e):
            run_engine("pe", e)

        @block.scalar
        def _(e):
            run_engine("act", e)

        @block.vector
        def _(e):
            run_engine("dve", e)

        @block.gpsimd
        def _(e):
            run_engine("pool", e)

        @block.sync
        def _(e):
            run_engine("sp", e)


class Arena:
    def __init__(self, t, nbytes):
        self.t = t
        self.nbytes = nbytes
        self.off = 0

    def alloc(self, dt, shape):
        shape = list(shape)
        if shape[0] == 128 and len(shape) >= 2:
            shape = shape[1:]
        n = int(np.prod(shape))
        sz = n * (4 if dt == F32 else 2)
        sz = (sz + 31) // 32 * 32
        assert self.off + sz <= self.nbytes, ("arena overflow", self.off, sz)
        v = self.t[:, self.off // 4:(self.off + sz) // 4]
        self.off += sz
        if dt != F32:
            v = v.bitcast(dt)
        v = v[:, 0:n]
        if len(shape) == 2:
            v = v.rearrange("p (a b) -> p a b", a=shape[0])
        elif len(shape) == 3:
            v = v.rearrange("p (a b c) -> p a b c", a=shape[0], b=shape[1])
        return v


def build(NG=8, dbg=False, phases="ABCDE"):
    nc = bass.Bass("TRN2", target_bir_lowering=False)
    dram_in = lambda n, s: nc.dram_tensor(n, list(s), F32, kind="ExternalInput").ap()
    x_all = dram_in("x_all", [SEQ, D])
    x_own = dram_in("x_own", [1024, D])
    p_own = dram_in("p_own", [1024, 256])
    msel = dram_in("msel", [128, 4])
    amask = dram_in("amask", [128, 4, 128])
    w_in = dram_in("w_in", [D, IN_DIM])
    conv_w = dram_in("conv_w", [4, 3072])
    conv_b = dram_in("conv_b", [3072])
    dt_bias = dram_in("dt_bias", [32])
    a_log = dram_in("a_log", [32])
    d_skip = dram_in("d_skip", [32])
    ssd_norm_w = dram_in("ssd_norm_w", [D])
    w_branch_a = dram_in("w_branch_a", [D, D])
    w_branch_b = dram_in("w_branch_b", [D, D])
    w_out = dram_in("w_out", [D, D])
    g_mix = dram_in("g_mix", [D])
    g_ffn = dram_in("g_ffn", [D])
    w_router = dram_in("w_router", [D, NE])
    b_router = dram_in("b_router", [NE])
    w_gate_up = dram_in("w_gate_up", [NE, D, 2 * D])
    b_gate_up = dram_in("b_gate_up", [NE, 2 * D])
    w_down = dram_in("w_down", [NE, D, D])
    b_down = dram_in("b_down", [NE, D])
    g_ple = dram_in("g_ple", [D])
    w_ple_gate = dram_in("w_ple_gate", [D, D])
    w_ple_proj = dram_in("w_ple_proj", [256, D])
    g_ple_post = dram_in("g_ple_post", [D])
    g_final = dram_in("g_final", [D])
    out = nc.dram_tensor("out", [1024, D], F32, kind="ExternalOutput").ap()
    kT_d = nc.dram_tensor("kT_d", [16, 128, SEQ], BF16).ap()
    v_d = nc.dram_tensor("v_d", [16, 128, 32, 128], BF16).ap()
    ya_d = nc.dram_tensor("ya_d", [8, 128, D], BF16).ap()
    dbg_t = {}
    if dbg:
        dbg_t["ya"] = nc.dram_tensor("dbg_ya", [NG, 128, D], F32, kind="ExternalOutput").ap()
        dbg_t["yb"] = nc.dram_tensor("dbg_yb", [128, 16, 1024], F32, kind="ExternalOutput").ap()
        dbg_t["x1"] = nc.dram_tensor("dbg_x1", [1024, D], F32, kind="ExternalOutput").ap()
        dbg_t["q"] = nc.dram_tensor("dbg_q", [128, 16, 1024], F32, kind="ExternalOutput").ap()
        dbg_t["x2"] = nc.dram_tensor("dbg_x2", [1024, D], F32, kind="ExternalOutput").ap()

    st = ExitStack()
    with st:
        S = Sched(nc, st)
        ARN = 207 * 1024
        arena_t = st.enter_context(nc.sbuf_tensor("arena", [128, ARN // 4], F32))
        AR = Arena(arena_t, ARN)
        pgs = [st.enter_context(nc.psum_tensor(f"pg{i}", [128, 512], F32))[:] for i in range(6)]
        pts = [st.enter_context(nc.psum_tensor(f"pt{i}", [128, 1024], BF16))[:] for i in range(2)]
        cnt = {"pg": 0, "pt": 0, "wb": 0, "npg": 6}

        def bank():
            i = cnt["pg"] % cnt["npg"]
            cnt["pg"] += 1
            return pgs[i], f"pg{i}"

        def tbank():
            i = cnt["pt"] % 2
            cnt["pt"] += 1
            return pts[i], f"pt{i}"

        out_dmas = []

        ident = AR.alloc(BF16, [128, 128])
        io_row = AR.alloc(F32, [128, 256])
        pidx = AR.alloc(F32, [128, 2])
        UI = AR.alloc(F32, [128, 128])
        SL = AR.alloc(F32, [128, 128])
        ones32 = AR.alloc(F32, [128, 128])
        UIb = AR.alloc(BF16, [128, 128])
        onesb = AR.alloc(BF16, [128, 128])
        SLTb = AR.alloc(BF16, [128, 128])
        junkB2 = AR.alloc(BF16, [128, D])
        msel_t = AR.alloc(F32, [128, 4])
        amask_t = AR.alloc(F32, [128, 4, 128])
        gain = [AR.alloc(F32, [128, D]) for _ in range(2)]
        CONST_END = AR.off

        S.add("pool", lambda e: e.iota(io_row, pattern=[[1, 256]], base=0, channel_multiplier=0,
                                       allow_small_or_imprecise_dtypes=True), w=["io_row"])
        S.add("pool", lambda e: e.iota(pidx[:, 0:1], pattern=[[0, 1]], base=0, channel_multiplier=1,
                                       allow_small_or_imprecise_dtypes=True), w=["pidx"])
        S.add("pool", lambda e: e.iota(pidx[:, 1:2], pattern=[[0, 1]], base=128, channel_multiplier=1,
                                       allow_small_or_imprecise_dtypes=True), w=["pidx"])
        S.add("dve", lambda e: e.tensor_scalar(ones32, io_row[:, 0:128], pidx[:, 0:1], None, ALU.is_equal),
              r=["io_row", "pidx"], w=["ones32"])
        S.add("dve", lambda e: e.tensor_copy(ident, ones32), r=["ones32"], w=["ident"])
        S.add("dve", lambda e: e.tensor_scalar(UI, io_row[:, 0:128], pidx[:, 0:1], None, ALU.is_ge),
              r=["io_row", "pidx"], w=["UI"])
        S.add("dve", lambda e: e.tensor_scalar(SL, io_row[:, 0:128], pidx[:, 0:1], None, ALU.is_lt),
              r=["io_row", "pidx"], w=["SL"])
        S.add("dve", lambda e: e.tensor_scalar(UIb, io_row[:, 0:128], pidx[:, 0:1], None, ALU.is_le),
              r=["io_row", "pidx"], w=["UIb"])
        S.add("dve", lambda e: e.tensor_scalar(SLTb, io_row[:, 0:128], pidx[:, 0:1], None, ALU.is_gt),
              r=["io_row", "pidx"], w=["SLTb"])
        S.add("pool", lambda e: e.memset(ones32, 1.0), r=["ident"], w=["ones32"])
        S.add("pool", lambda e: e.memset(onesb, 1.0), w=["onesb"])
        S.add("sp", lambda e: e.dma_start(out=msel_t, in_=msel[:, :]), w=["msel"], dma=True)
        S.add("sp", lambda e: e.dma_start(out=amask_t, in_=amask[:, :, :]), w=["amask"], dma=True)

        def load_gain(slot, src):
            S.add("sp", lambda e: e.dma_start(out=gain[slot], in_=src.partition_broadcast(128)),
                  w=[f"gain{slot}"], dma=True)

        NWB = 2
        wbs = [AR.alloc(BF16, [16, 512]) for _ in range(NWB)]
        WB_END = AR.off

        def wload(src2d, ncols=512, rows=D):
            i = cnt["wb"] % NWB
            cnt["wb"] += 1
            nch = rows // 128
            dst = wbs[i][:, 0:nch, 0:ncols]
            pos_now = len(S.ops)
            pr = cnt.get("wmark")
            S.add("pool", lambda e: e.dma_start(out=dst, in_=src2d.rearrange("(c p) n -> p c n", p=128)),
                  w=[f"wb{i}"], dma=True)
            cnt["wmark"] = pos_now
            return wbs[i], f"wb{i}"

        def rmsnorm_rows(xt, xkey, gslot, outb, outkey, ss, sskey, junk, junkkey):
            S.add("act", lambda e: e.activation(junk, xt, AF.Square, accum_out=ss), r=[xkey], w=[junkkey, sskey])
            S.add("act", lambda e: e.activation(ss, ss, AF.Ln, bias=EPS, scale=1.0 / D), r=[sskey], w=[sskey])
            S.add("act", lambda e: e.activation(ss, ss, AF.Exp, scale=-0.5), r=[sskey], w=[sskey])
            S.add("dve", lambda e: e.scalar_tensor_tensor(outb, xt, ss, gain[gslot], ALU.mult, ALU.mult),
                  r=[xkey, sskey, f"gain{gslot}"], w=[outkey])

        def transpose_rows(srcb, srckey, dstT, dstkey_fn, tok0, ntok=128, nfc=16):
            for half in range(nfc // 8):
                pt, ptk = tbank()
                for k in range(8):
                    fc = half * 8 + k
                    S.add("pe", lambda e, fc=fc, k=k, pt=pt: e.transpose(pt[:, k * 128:(k + 1) * 128],
                                                                        srcb[:, fc * 128:(fc + 1) * 128], ident),
                          r=[srckey, "ident"], w=[ptk])
                S.add("act", lambda e, half=half, pt=pt: e.copy(
                    dstT[:, half * 8:(half + 1) * 8, tok0:tok0 + ntok],
                    pt.rearrange("p (a b) -> p a b", a=8)),
                    r=[ptk], w=[dstkey_fn(half)])

        A0 = AR.off
        xt = AR.alloc(F32, [128, D])
        hb = AR.alloc(BF16, [128, D])
        hT = AR.alloc(BF16, [16, 512])
        zs = AR.alloc(BF16, [4, D])
        xtm = AR.alloc(BF16, [4, D])
        Btm = AR.alloc(BF16, [4, 512])
        BT = AR.alloc(BF16, [4, 512])
        CT = AR.alloc(BF16, [4, 512])
        raw = [AR.alloc(F32, [128, 515]) for _ in range(2)]
        ctmp = [AR.alloc(F32, [128, 512]) for _ in range(2)]
        xcb = [AR.alloc(BF16, [128, 512]) for _ in range(2)]
        halo = AR.alloc(F32, [24, 3])
        convw_t = AR.alloc(F32, [4, 24])
        convb_t = AR.alloc(F32, [128, 24])
        wdt = AR.alloc(BF16, [16, 32])
        dtb_b = AR.alloc(F32, [128, 32])
        negA_b = AR.alloc(F32, [128, 32])
        dskip_b = AR.alloc(F32, [128, 32])
        ss = AR.alloc(F32, [128, 8])
        dt_all = AR.alloc(F32, [4, 32])
        a_all = AR.alloc(F32, [4, 32])
        dtr = AR.alloc(F32, [128, 32])
        acs = AR.alloc(F32, [128, 64])
        eacs = AR.alloc(F32, [128, 32])
        dst = AR.alloc(F32, [128, 32])
        cdec = AR.alloc(F32, [128, 32])
        Xdt = AR.alloc(BF16, [128, D])
        Xds = AR.alloc(BF16, [128, D])
        CBm = AR.alloc(F32, [4, 128])
        lh = [AR.alloc(F32, [8, 128]) for _ in range(2)]
        dec = [AR.alloc(F32, [4, 128]) for _ in range(2)]
        Mb = [AR.alloc(BF16, [4, 128]) for _ in range(2)]
        yo = [AR.alloc(F32, [128, 512]) for _ in range(2)]
        yy = [AR.alloc(F32, [128, 512]) for _ in range(2)]
        ssg = AR.alloc(F32, [128, 4])
        rm = AR.alloc(F32, [128, 4])
        state = AR.alloc(F32, [128, D])
        prevb = AR.alloc(BF16, [128, D])
        kst = [AR.alloc(BF16, [128, 512]) for _ in range(2)]
        vst = [AR.alloc(BF16, [128, 512]) for _ in range(2)]
        ya_cur = AR.alloc(BF16, [128, D])
        A_END = AR.off

        if "A" in phases:
            load_gain(0, g_mix)
            load_gain(1, ssd_norm_w)
            for k in range(4):
                S.add("sp", lambda e, k=k: e.dma_start(out=convw_t[:, k, :], in_=conv_w[k].rearrange("(c p) -> p c", p=128),
                                                       allow_slow_non_contiguous=True), w=[("convw", k)], dma=True)
            S.add("sp", lambda e: e.dma_start(out=convb_t, in_=conv_b.rearrange("(c p) -> p c", p=128),
                                              allow_slow_non_contiguous=True), w=["convb"], dma=True)
            S.add("pool", lambda e: e.dma_start(out=wdt, in_=w_in[:, C_DT:C_DT + 32].rearrange("(c p) n -> p c n", p=128)),
                  w=["wdt"], dma=True)
            S.add("sp", lambda e: e.dma_start(out=dtb_b, in_=dt_bias.partition_broadcast(128)), w=["dtb"], dma=True)
            S.add("sp", lambda e: e.dma_start(out=negA_b, in_=a_log.partition_broadcast(128)), w=["negA"], dma=True)
            S.add("sp", lambda e: e.dma_start(out=dskip_b, in_=d_skip.partition_broadcast(128)), w=["dskip"], dma=True)
            S.add("act", lambda e: e.activation(negA_b, negA_b, AF.Exp), r=["negA"], w=["negA"])
            S.add("dve", lambda e: e.tensor_scalar(negA_b, negA_b, -1.0, None, ALU.mult), r=["negA"], w=["negA"])
            S.add("pool", lambda e: e.memset(halo, 0.0), w=["halo"])
            S.add("pool", lambda e: e.memset(state, 0.0), w=["state"])
            S.add("pool", lambda e: e.memset(prevb, 0.0), w=["prevb"])

            own_flag = [False]

            def SY(*a_, **k_):
                if own_flag[0]:
                    S.add(*a_, **k_)

            for G in range(NG):
                for cc in range(4):
                    c = 4 * G + cc
                    S.add("sp", lambda e, c=c: e.dma_start(out=xt, in_=x_all[c * 128:(c + 1) * 128, :]), w=["xt"], dma=True)
                    rmsnorm_rows(xt, "xt", 0, hb, "hb", ss[:, 0:1], "ss0", junkB2, "junkB2")
                    transpose_rows(hb, "hb", hT, lambda half, cc=cc: ("hT", cc, half), cc * 128)
                hTkeys = [("hT", cc, half) for cc in range(4) for half in range(2)]
                for jb in range(8):
                    col0 = (C_Z + jb * 512) if jb < 4 else (C_V + (jb - 4) * 512)
                    wb, wk = wload(w_in[:, col0:col0 + 512])
                    for cc in ((3,) if jb < 4 else range(4)):
                        c = 4 * G + cc
                        pg, pk = bank()
                        for fc in range(16):
                            S.add("pe", lambda e, pg=pg, fc=fc, cc=cc, wb=wb: e.matmul(
                                pg, hT[:, fc, cc * 128:(cc + 1) * 128], wb[:, fc, :], start=(fc == 0), stop=(fc == 15)),
                                r=[("hT", cc, fc // 8), wk], w=[pk])
                        if jb < 4:
                            S.add("act", lambda e, pg=pg, cc=cc, jb=jb: e.activation(
                                zs[:, cc, jb * 512:(jb + 1) * 512], pg, AF.Silu), r=[pk], w=[("zs", cc, jb)])
                        else:
                            vi = cnt.setdefault("vst", 0) % 2
                            cnt["vst"] += 1
                            S.add("act", lambda e, pg=pg, vi=vi: e.copy(vst[vi], pg), r=[pk], w=[f"vst{vi}"])
                            S.add("sp", lambda e, vi=vi, c=c, jb=jb: e.dma_start(
                                out=v_d[(jb - 4) * 4:(jb - 3) * 4, :, c, :].rearrange("h p d -> p h d"),
                                in_=vst[vi].rearrange("p (h d) -> p h d", h=4)),
                                r=[f"vst{vi}"], w=[("v_d", c)], dma=True)
                for cc in range(4):
                    pg, pk = bank()
                    for fc in range(16):
                        S.add("pe", lambda e, pg=pg, fc=fc, cc=cc: e.matmul(
                            pg[:, 0:32], hT[:, fc, cc * 128:(cc + 1) * 128], wdt[:, fc, :], start=(fc == 0), stop=(fc == 15)),
                            r=[("hT", cc, fc // 8), "wdt"], w=[pk])
                    S.add("dve", lambda e, pg=pg: e.tensor_tensor(dtr, pg[:, 0:32], dtb_b, ALU.add), r=[pk, "dtb"], w=["dtr"])
                    S.add("act", lambda e: e.activation(dtr, dtr, AF.Exp), r=["dtr"], w=["dtr"])
                    S.add("act", lambda e, cc=cc: e.activation(dt_all[:, cc, :], dtr, AF.Ln, bias=1.0), r=["dtr"], w=[("dt", cc)])
                    S.add("dve", lambda e, cc=cc: e.tensor_tensor(a_all[:, cc, :], dt_all[:, cc, :], negA_b, ALU.mult),
                          r=[("dt", cc), "negA"], w=[("a", cc)])
                for piece in range(10):
                    col0 = (C_XBC + piece * 512) if piece < 6 else (C_K + (piece - 6) * 512)
                    wb, wk = wload(w_in[:, col0:col0 + 512])
                    for sub in range(4):
                        i = piece * 4 + sub
                        pg, pk = bank()
                        for fc in range(16):
                            S.add("pe", lambda e, pg=pg, fc=fc, sub=sub, wb=wb: e.matmul(
                                pg, wb[:, fc, sub * 128:(sub + 1) * 128], hT[:, fc, :], start=(fc == 0), stop=(fc == 15)),
                                r=hTkeys + [wk], w=[pk])
                        if i < 24:
                            ri = i % 2
                            rw, ct, xc = raw[ri], ctmp[ri], xcb[ri]
                            S.add("act", lambda e, pg=pg, rw=rw: e.copy(rw[:, 3:515], pg), r=[pk], w=[f"raw{ri}"])
                            S.add("act", lambda e, rw=rw, i=i: e.copy(rw[:, 0:3], halo[:, i, :]),
                                  r=[("halo", i)], w=[f"rawh{ri}"])
                            rk = [f"raw{ri}", f"rawh{ri}"] + [("convw", k) for k in range(4)]
                            S.add("dve", lambda e, rw=rw, ct=ct, i=i: e.tensor_scalar(
                                ct, rw[:, 0:512], convw_t[:, 0, i:i + 1], None, ALU.mult), r=rk, w=[f"ct{ri}"])
                            for k in (1, 2, 3):
                                S.add("dve", lambda e, rw=rw, ct=ct, i=i, k=k: e.scalar_tensor_tensor(
                                    ct, rw[:, k:512 + k], convw_t[:, k, i:i + 1], ct, ALU.mult, ALU.add),
                                    r=rk + [f"ct{ri}"], w=[f"ct{ri}"])
                            S.add("act", lambda e, rw=rw, i=i: e.copy(halo[:, i, :], rw[:, 512:515]),
                                  r=[f"raw{ri}"], w=[("halo", i)])
                            if i < 16:
                                S.add("act", lambda e, ct=ct, xc=xc, i=i: e.activation(xc, ct, AF.Silu, bias=convb_t[:, i:i + 1]),
                                      r=[f"ct{ri}", "convb"], w=[f"xc{ri}"])
                                src, sk = xc, f"xc{ri}"
                            elif i < 20:
                                g = i - 16
                                S.add("act", lambda e, ct=ct, g=g, i=i: e.activation(BT[:, g, :], ct, AF.Silu, bias=convb_t[:, i:i + 1]),
                                      r=[f"ct{ri}", "convb"], w=[("BT", g)])
                                src, sk = BT[:, g, :], ("BT", g)
                            else:
                                g = i - 20
                                S.add("act", lambda e, ct=ct, g=g, i=i: e.activation(CT[:, g, :], ct, AF.Silu, bias=convb_t[:, i:i + 1]),
                                      r=[f"ct{ri}", "convb"], w=[("CT", g)])
                                src = None
                            if src is not None:
                                pt, ptk = tbank()
                                for cc in range(4):
                                    S.add("pe", lambda e, pt=pt, cc=cc, src=src: e.transpose(
                                        pt[:, cc * 128:(cc + 1) * 128], src[:, cc * 128:(cc + 1) * 128], ident),
                                        r=[sk, "ident"], w=[ptk])
                                if i < 16:
                                    S.add("act", lambda e, pt=pt, i=i: e.copy(
                                        xtm[:, :, i * 128:(i + 1) * 128], pt[:, 0:512].rearrange("p (a b) -> p a b", a=4)),
                                        r=[ptk], w=[("xtm", i)])
                                else:
                                    S.add("act", lambda e, pt=pt, g=g: e.copy(
                                        Btm[:, :, g * 128:(g + 1) * 128], pt[:, 0:512].rearrange("p (a b) -> p a b", a=4)),
                                        r=[ptk], w=[("Btm", g)])
                        else:
                            hd = i - 24
                            ki = hd % 2
                            S.add("act", lambda e, pg=pg, ki=ki: e.copy(kst[ki], pg), r=[pk], w=[f"kst{ki}"])
                            S.add("sp", lambda e, ki=ki, hd=hd, G=G: e.dma_start(
                                out=kT_d[hd, :, G * 512:(G + 1) * 512], in_=kst[ki]),
                                r=[f"kst{ki}"], w=[("kT_d", hd)], dma=True)
                xtm_keys = [("xtm", i) for i in range(16)]
                for cc in range(4):
                    own_flag[0] = (cc == 3)
                    pg, pk = bank()
                    S.add("pe", lambda e, pg=pg, cc=cc: e.matmul(pg[:, 0:32], UI, a_all[:, cc, :], start=True, stop=True),
                          r=[("a", cc), "UI"], w=[pk])
                    S.add("pe", lambda e, pg=pg, cc=cc: e.matmul(pg[:, 32:64], ones32, a_all[:, cc, :], start=True, stop=True),
                          r=[("a", cc), "ones32"], w=[pk])
                    S.add("act", lambda e, pg=pg: e.copy(acs, pg[:, 0:64]), r=[pk], w=["acs"])
                    SY("act", lambda e: e.activation(eacs, acs[:, 0:32], AF.Exp), r=["acs"], w=["eacs"])
                    S.add("act", lambda e: e.activation(cdec, acs[:, 32:64], AF.Exp), r=["acs"], w=["cdec"])
                    S.add("dve", lambda e: e.tensor_tensor(dst, acs[:, 32:64], acs[:, 0:32], ALU.subtract), r=["acs"], w=["dst"])
                    S.add("act", lambda e: e.activation(dst, dst, AF.Exp), r=["dst"], w=["dst"])
                    if G == 0 and cc < 3:
                        S.add("dve", lambda e, cc=cc: e.tensor_scalar(dst, dst, msel_t[:, cc:cc + 1], None, ALU.mult),
                              r=["dst", "msel"], w=["dst"])
                    x3 = xtm[:, cc, :].rearrange("p (h d) -> p h d", h=32)
                    S.add("dve", lambda e, cc=cc, x3=x3: e.tensor_tensor(
                        Xdt.rearrange("p (h d) -> p h d", h=32), x3,
                        dt_all[:, cc, :].unsqueeze(2).to_broadcast([128, 32, 64]), ALU.mult),
                        r=xtm_keys + [("dt", cc)], w=["Xdt"])
                    S.add("dve", lambda e: e.tensor_tensor(
                        Xds.rearrange("p (h d) -> p h d", h=32), Xdt.rearrange("p (h d) -> p h d", h=32),
                        dst.unsqueeze(2).to_broadcast([128, 32, 64]), ALU.mult), r=["Xdt", "dst"], w=["Xds"])
                    pg, pk = bank()
                    for g in range(4):
                        SY("pe", lambda e, pg=pg, g=g, cc=cc: e.matmul(
                            pg[:, g * 128:(g + 1) * 128], BT[:, g, cc * 128:(cc + 1) * 128], CT[:, g, cc * 128:(cc + 1) * 128],
                            start=True, stop=True), r=[("BT", g), ("CT", g)], w=[pk])
                    SY("dve", lambda e, pg=pg: e.tensor_tensor(
                        CBm, pg.rearrange("p (g l) -> p g l", g=4), UI.unsqueeze(1).to_broadcast([128, 4, 128]), ALU.mult),
                        r=[pk, "UI"], w=["CBm"])
                    for g in range(4):
                        gi = g % 2
                        SY("dve", lambda e, g=g, gi=gi, cc=cc: e.tensor_tensor(
                            lh[gi], SL.unsqueeze(1).to_broadcast([128, 8, 128]),
                            a_all[:, cc, g * 8:(g + 1) * 8].unsqueeze(2).to_broadcast([128, 8, 128]), ALU.mult),
                            r=["SL", ("a", cc)], w=[f"lh{gi}"])
                        pY, pYk = bank()
                        for half in range(2):
                            pseg, psk = bank()
                            for j in range(4):
                                SY("pe", lambda e, pseg=pseg, j=j, gi=gi, half=half: e.matmul(
                                    pseg[:, j * 128:(j + 1) * 128], lh[gi][:, half * 4 + j, :], UI, start=True, stop=True),
                                    r=[f"lh{gi}", "UI"], w=[psk])
                            SY("act", lambda e, pseg=pseg, half=half: e.activation(
                                dec[half], pseg.rearrange("p (j l) -> p j l", j=4), AF.Exp), r=[psk], w=[f"dec{half}"])
                            SY("dve", lambda e, half=half, g=g: e.tensor_tensor(
                                Mb[half], dec[half], CBm[:, g:g + 1, :].to_broadcast([128, 4, 128]), ALU.mult),
                                r=[f"dec{half}", "CBm"], w=[f"Mb{half}"])
                            for j in range(4):
                                h = g * 8 + half * 4 + j
                                SY("pe", lambda e, pY=pY, half=half, j=j, h=h: e.matmul(
                                    pY[:, (half * 4 + j) * 64:(half * 4 + j + 1) * 64], Mb[half][:, j, :],
                                    Xdt[:, h * 64:(h + 1) * 64], start=True, stop=True),
                                    r=[f"Mb{half}", "Xdt"], w=[pYk])
                        pYo, pYok = bank()
                        SY("pe", lambda e, pYo=pYo, g=g, cc=cc: e.matmul(
                            pYo, CT[:, g, cc * 128:(cc + 1) * 128], prevb[:, g * 512:(g + 1) * 512], start=True, stop=True),
                            r=[("CT", g), ("prevb", g)], w=[pYok])
                        SY("act", lambda e, pYo=pYo, gi=gi: e.copy(yo[gi], pYo), r=[pYok], w=[f"yo{gi}"])
                        SY("dve", lambda e, gi=gi, g=g: e.tensor_tensor(
                            yo[gi].rearrange("p (h d) -> p h d", h=8), yo[gi].rearrange("p (h d) -> p h d", h=8),
                            eacs[:, g * 8:(g + 1) * 8].unsqueeze(2).to_broadcast([128, 8, 64]), ALU.mult),
                            r=[f"yo{gi}", "eacs"], w=[f"yo{gi}"])
                        SY("dve", lambda e, gi=gi, pY=pY: e.tensor_tensor(yy[gi], pY, yo[gi], ALU.add),
                              r=[pYk, f"yo{gi}"], w=[f"yy{gi}"])
                        SY("dve", lambda e, gi=gi, g=g, cc=cc: e.tensor_tensor(
                            yo[gi].rearrange("p (h d) -> p h d", h=8),
                            xtm[:, cc, g * 512:(g + 1) * 512].rearrange("p (h d) -> p h d", h=8),
                            dskip_b[:, g * 8:(g + 1) * 8].unsqueeze(2).to_broadcast([128, 8, 64]), ALU.mult),
                            r=xtm_keys + ["dskip", f"yy{gi}"], w=[f"yo{gi}"])
                        SY("dve", lambda e, gi=gi: e.tensor_tensor(yy[gi], yy[gi], yo[gi], ALU.add),
                              r=[f"yy{gi}", f"yo{gi}"], w=[f"yy{gi}"])
                        SY("dve", lambda e, gi=gi, g=g, cc=cc: e.tensor_tensor(
                            yy[gi], yy[gi], zs[:, cc, g * 512:(g + 1) * 512], ALU.mult),
                            r=[f"yy{gi}", ("zs", cc, g)], w=[f"yy{gi}"])
                        SY("act", lambda e, gi=gi, g=g: e.activation(yo[gi], yy[gi], AF.Square, accum_out=ssg[:, g:g + 1]),
                              r=[f"yy{gi}"], w=[f"yo{gi}", ("ssg", g)])
                        SY("act", lambda e, g=g: e.activation(rm[:, g:g + 1], ssg[:, g:g + 1], AF.Ln, bias=EPS, scale=1.0 / 512),
                              r=[("ssg", g)], w=[("rm", g)])
                        SY("act", lambda e, g=g: e.activation(rm[:, g:g + 1], rm[:, g:g + 1], AF.Exp, scale=-0.5),
                              r=[("rm", g)], w=[("rm", g)])
                        SY("dve", lambda e, gi=gi, g=g: e.tensor_tensor(
                            yy[gi], yy[gi], gain[1][:, g * 512:(g + 1) * 512], ALU.mult),
                            r=[f"yy{gi}", "gain1"], w=[f"yy{gi}"])
                        dstv = ya_cur[:, g * 512:(g + 1) * 512]
                        if True:
                            SY("dve", lambda e, dstv=dstv, g=g, gi=gi: e.tensor_scalar(
                                dstv, yy[gi], rm[:, g:g + 1], None, ALU.mult),
                                r=[f"yy{gi}", ("rm", g)], w=[("ya", g)])
                        else:
                            SY("dve", lambda e, dstv=dstv, g=g, gi=gi: e.scalar_tensor_tensor(
                                dstv, yy[gi], rm[:, g:g + 1], dstv, ALU.mult, ALU.add),
                                r=[f"yy{gi}", ("rm", g), ("ya", g)], w=[("ya", g)])
                        pSt, pStk = bank()
                        S.add("pe", lambda e, pSt=pSt, g=g, cc=cc: e.matmul(
                            pSt, Btm[:, cc, g * 128:(g + 1) * 128], Xds[:, g * 512:(g + 1) * 512], start=True, stop=True),
                            r=[("Btm", g), "Xds"], w=[pStk])
                        sv = state[:, g * 512:(g + 1) * 512]
                        S.add("dve", lambda e, sv=sv, g=g: e.tensor_tensor(
                            sv.rearrange("p (h d) -> p h d", h=8), sv.rearrange("p (h d) -> p h d", h=8),
                            cdec[:, g * 8:(g + 1) * 8].unsqueeze(2).to_broadcast([128, 8, 64]), ALU.mult),
                            r=[("state", g), "cdec"], w=[("state", g)])
                        S.add("dve", lambda e, sv=sv, pSt=pSt: e.tensor_tensor(sv, sv, pSt, ALU.add),
                              r=[("state", g), pStk], w=[("state", g)])
                        S.add("act", lambda e, sv=sv, g=g: e.copy(prevb[:, g * 512:(g + 1) * 512], sv),
                              r=[("state", g)], w=[("prevb", g)])
                S.add("sp", lambda e, G=G: e.dma_start(out=ya_d[G], in_=ya_cur),
                      r=[("ya", g) for g in range(4)], w=[("ya_d", G)], dma=True)
                if dbg:
                    S.add("pool", lambda e, G=G: e.dma_start(out=dbg_t["ya"][G], in_=ya_cur),
                          r=[("ya", g) for g in range(4)], dma=True)
                    out_dmas.append(len(S.ops) - 1)


        NJ = NG
        NT = NJ * 128
        R1 = WB_END

        def at(off_kb, dt, shape):
            AR.off = R1 + off_kb * 1024
            return AR.alloc(dt, shape)

        S.barrier()
        hTo = at(0, BF16, [16, 1024])
        qT = at(32, BF16, [16, 1024])
        ybT = at(64, BF16, [16, 1024])
        kTh = [at(96 + 8 * i, BF16, [128, SEQ]) for i in range(2)]
        vh = [at(112 + 8 * i, BF16, [32, 128]) for i in range(2)]
        xo = at(64, F32, [128, D])
        hbo = at(72, BF16, [128, D])
        junkB = at(76, BF16, [128, D])
        AR.off = R1 + 128 * 1024
        ssb = AR.alloc(F32, [128, 8])
        Et = [AR.alloc(F32, [128, 512]) for _ in range(2)]
        spb = [AR.alloc(BF16, [128, 512]) for _ in range(2)]
        expc = [AR.alloc(F32, [128, 512]) for _ in range(2)]
        wbf = [AR.alloc(BF16, [128, 512]) for _ in range(2)]
        Trow = [AR.alloc(BF16, [128, 512]) for _ in range(2)]

        def own_hT(gslot, src_tiles_fn, dstT, keyname):
            for j in range(NJ):
                src_tiles_fn(j)
                rmsnorm_rows(xo, "xo", gslot, hbo, "hbo", ssb[:, 0:1], "ssb0", junkB, "junkB")
                transpose_rows(hbo, "hbo", dstT, lambda half, j=j: (keyname, j, half), j * 128)

        if "B" in phases:
            load_gain(0, g_mix)
            own_hT(0, lambda j: S.add("sp", lambda e: e.dma_start(out=xo, in_=x_own[j * 128:(j + 1) * 128, :]),
                                      w=["xo"], dma=True), hTo, "hTo")
            hTo_keys = [("hTo", j, half) for j in range(NJ) for half in range(2)]
            NTB = (NT + 511) // 512
            for piece in range(4):
                wb, wk = wload(w_in[:, C_Q + piece * 512:C_Q + (piece + 1) * 512])
                for sub in range(4):
                    hd = piece * 4 + sub
                    for tb in range(NTB):
                        n = min(512, NT - tb * 512)
                        pg, pk = bank()
                        for fc in range(16):
                            S.add("pe", lambda e, pg=pg, fc=fc, sub=sub, wb=wb, tb=tb, n=n: e.matmul(
                                pg[:, 0:n], wb[:, fc, sub * 128:(sub + 1) * 128], hTo[:, fc, tb * 512:tb * 512 + n],
                                start=(fc == 0), stop=(fc == 15)), r=hTo_keys + [wk], w=[pk])
                        S.add("act", lambda e, pg=pg, hd=hd, tb=tb, n=n: e.mul(
                            qT[:, hd, tb * 512:tb * 512 + n], pg[:, 0:n], float(128 ** -0.5)), r=[pk], w=[("qT", hd)])

            if dbg:
                S.add("pool", lambda e: e.dma_start(out=dbg_t["q"][:, :, 0:NT], in_=qT[:, :, 0:NT]),
                      r=[("qT", hd) for hd in range(16)], dma=True)
            NKB = 4 * NJ
            cnt["npg"] = 4
            for hp in range(8):
                for ci in range(2):
                    hd = hp * 2 + ci
                    S.add("sp", lambda e, hd=hd, ci=ci: e.dma_start(out=kTh[ci][:, 0:NKB * 128], in_=kT_d[hd, :, 0:NKB * 128]),
                          r=[("kT_d", hd)], w=[f"kTh{ci}"], dma=True)
                    S.add("sp", lambda e, hd=hd, ci=ci: e.dma_start(
                        out=vh[ci][:, 0:NKB, :], in_=v_d[hd, :, 0:NKB, :]),
                        r=[("v_d", c) for c in range(NKB)], w=[f"vh{ci}"], dma=True)
                for Q in range((NJ + 3) // 4):
                    j0 = 4 * Q
                    W = min(4, NJ - j0)
                    WN = W * 128
                    pys = []
                    for ci in range(2):
                        S.add("dve", lambda e, ci=ci: e.memset(Trow[ci], 0.0), w=[f"Trow{ci}"])
                        pys.append((pgs[4 + ci], f"pg{4 + ci}"))
                        S.add("pe", lambda e, ci=ci, WN=WN: e.matmul(pgs[4 + ci][:, 0:WN], onesb[0:1, :], Trow[ci][0:1, 0:WN], start=True, stop=False),
                              r=["onesb", f"Trow{ci}"], w=[f"pg{4 + ci}"])
                    kmax = 4 * (j0 + W - 1) + 3
                    for kb in range(kmax, -1, -1):
                        jmin = max(j0, (kb - 3 + 3) // 4)
                        c0 = (jmin - j0) * 128
                        jm = kb // 4
                        pzs = []
                        for ci in range(2):
                            hd = hp * 2 + ci
                            pz, pzk = bank()
                            pzs.append((pz, pzk))
                            S.add("pe", lambda e, pz=pz, ci=ci, hd=hd, kb=kb, c0=c0, WN=WN, j0=j0: e.matmul(
                                pz[:, c0:WN], kTh[ci][:, kb * 128:(kb + 1) * 128], qT[:, hd, j0 * 128 + c0:j0 * 128 + WN],
                                start=True, stop=True), r=[f"kTh{ci}", ("qT", hd)], w=[pzk])
                            S.add("act", lambda e, pz=pz, ci=ci, c0=c0, WN=WN: e.activation(Et[ci][:, c0:WN], pz[:, c0:WN], AF.Exp),
                                  r=[pzk], w=[f"E{ci}"])
                            if kb % 4 == 3 and j0 <= jm < j0 + W:
                                cm = (jm - j0) * 128
                                S.add("dve", lambda e, ci=ci, cm=cm: e.tensor_tensor(
                                    Et[ci][:, cm:cm + 128], Et[ci][:, cm:cm + 128], amask_t[:, 3, :], ALU.mult),
                                    r=[f"E{ci}", "amask"], w=[f"E{ci}"])
                            if kb < 3:
                                S.add("dve", lambda e, ci=ci, c0=c0, WN=WN, kb=kb: e.tensor_scalar(
                                    Et[ci][:, c0:WN], Et[ci][:, c0:WN], msel_t[:, kb:kb + 1], None, ALU.mult),
                                    r=[f"E{ci}", "msel"], w=[f"E{ci}"])
                            S.add("act", lambda e, ci=ci, c0=c0, WN=WN: e.activation(spb[ci][:, c0:WN], Et[ci][:, c0:WN], AF.Ln, bias=1.0),
                                  r=[f"E{ci}"], w=[f"sp{ci}"])
                        pcs = []
                        for ci in range(2):
                            pc, pck = bank()
                            pcs.append((pc, pck))
                            S.add("pe", lambda e, pc=pc, ci=ci, c0=c0, WN=WN: e.matmul(
                                pc[:, c0:WN], UIb, spb[ci][:, c0:WN], start=True, stop=False), r=["UIb", f"sp{ci}"], w=[pck])
                            S.add("pe", lambda e, pc=pc, ci=ci, c0=c0, WN=WN: e.matmul(
                                pc[:, c0:WN], onesb[0:1, :], Trow[ci][0:1, c0:WN], start=False, stop=True), r=["onesb", f"Trow{ci}"], w=[pck])
                            S.add("act", lambda e, pc=pc, ci=ci, c0=c0, WN=WN: e.copy(Trow[ci][0:1, c0:WN], pc[0:1, c0:WN]),
                                  r=[pck], w=[f"Trow{ci}"])
                            S.add("act", lambda e, pc=pc, ci=ci, c0=c0, WN=WN: e.activation(expc[ci][:, c0:WN], pc[:, c0:WN], AF.Exp, scale=-1.0),
                                  r=[pck], w=[f"expc{ci}"])
                            S.add("dve", lambda e, ci=ci, c0=c0, WN=WN: e.tensor_tensor(
                                wbf[ci][:, c0:WN], Et[ci][:, c0:WN], expc[ci][:, c0:WN], ALU.mult),
                                r=[f"E{ci}", f"expc{ci}"], w=[f"w{ci}"])
                        for ci in range(2):
                            py, pyk = pys[ci]
                            newj = (kb - 3) // 4 if (kb - 3) % 4 == 0 and j0 <= (kb - 3) // 4 < j0 + W else None
                            last = (kb == 0)
                            if newj is not None:
                                cn = (newj - j0) * 128
                                S.add("pe", lambda e, py=py, ci=ci, kb=kb, cn=cn, last=last: e.matmul(
                                    py[:, cn:cn + 128], vh[ci][:, kb, :], wbf[ci][:, cn:cn + 128], start=False, stop=last),
                                    r=[f"vh{ci}", f"w{ci}"], w=[pyk])
                                c1 = cn + 128
                            else:
                                c1 = c0
                            if c1 < WN:
                                S.add("pe", lambda e, py=py, ci=ci, kb=kb, c1=c1, WN=WN, last=last: e.matmul(
                                    py[:, c1:WN], vh[ci][:, kb, :], wbf[ci][:, c1:WN], start=False, stop=last),
                                    r=[f"vh{ci}", f"w{ci}"], w=[pyk])
                    for ci in range(2):
                        hd = hp * 2 + ci
                        py, pyk = pys[ci]
                        S.add("act", lambda e, py=py, hd=hd, WN=WN, j0=j0: e.copy(ybT[:, hd, j0 * 128:j0 * 128 + WN], py[:, 0:WN]),
                              r=[pyk], w=[("ybT", hd)])
            cnt["npg"] = 6
            if dbg:
                S.add("pool", lambda e: e.dma_start(out=dbg_t["yb"][:, :, 0:NT], in_=ybT[:, :, 0:NT]),
                      r=[("ybT", hd) for hd in range(16)], dma=True)

        S.barrier()
        yaT = at(32, BF16, [16, 1024])
        mT = at(96, BF16, [16, 1024])
        AR.off = R1 + 128 * 1024
        yab = AR.alloc(BF16, [128, D])
        sg = [AR.alloc(F32, [128, 512]) for _ in range(2)]
        tt = [AR.alloc(F32, [128, 512]) for _ in range(2)]
        NTB = (NT + 511) // 512
        if "C" in phases:
            for j in range(NJ):
                S.add("sp", lambda e, j=j: e.dma_start(out=yab, in_=ya_d[j]), r=[("ya_d", j)], w=["yab"], dma=True)
                transpose_rows(yab, "yab", yaT, lambda half, j=j: ("yaT", j, half), j * 128)
            yaT_keys = [("yaT", j, half) for j in range(NJ) for half in range(2)]
            hTo_keys = [("hTo", j, half) for j in range(NJ) for half in range(2)]
            ybT_keys = [("ybT", hd) for hd in range(16)]
            srcs = [(w_branch_a, 0, yaT, yaT_keys), (w_branch_b, 0, ybT, ybT_keys),
                    (w_in, C_GA, hTo, hTo_keys), (w_in, C_GB, hTo, hTo_keys)]
            for piece in range(4):
                for tb in range(NTB):
                    n = min(512, NT - tb * 512)
                    for sub in range(4):
                        pass
                for pair in range(2):
                    res = []
                    wa = wload(srcs[pair][0][:, srcs[pair][1] + piece * 512:srcs[pair][1] + (piece + 1) * 512])
                    wg = wload(srcs[2 + pair][0][:, srcs[2 + pair][1] + piece * 512:srcs[2 + pair][1] + (piece + 1) * 512])
                    for sub in range(4):
                        nb = piece * 4 + sub
                        for tb in range(NTB):
                            n = min(512, NT - tb * 512)
                            pa, pak = bank()
                            pgt, pgk = bank()
                            act_in, act_keys = srcs[pair][2], srcs[pair][3]
                            for fc in range(16):
                                S.add("pe", lambda e, pa=pa, fc=fc, sub=sub, tb=tb, n=n, wa=wa, act_in=act_in: e.matmul(
                                    pa[:, 0:n], wa[0][:, fc, sub * 128:(sub + 1) * 128], act_in[:, fc, tb * 512:tb * 512 + n],
                                    start=(fc == 0), stop=(fc == 15)), r=act_keys + [wa[1]], w=[pak])
                            for fc in range(16):
                                S.add("pe", lambda e, pgt=pgt, fc=fc, sub=sub, tb=tb, n=n, wg=wg: e.matmul(
                                    pgt[:, 0:n], wg[0][:, fc, sub * 128:(sub + 1) * 128], hTo[:, fc, tb * 512:tb * 512 + n],
                                    start=(fc == 0), stop=(fc == 15)), r=hTo_keys + [wg[1]], w=[pgk])
                            si = cnt.setdefault("sg", 0) % 2
                            cnt["sg"] += 1
                            S.add("act", lambda e, pgt=pgt, si=si, n=n: e.activation(sg[si][:, 0:n], pgt[:, 0:n], AF.Sigmoid),
                                  r=[pgk], w=[f"sg{si}"])
                            mv = mT[:, nb, tb * 512:tb * 512 + n]
                            if pair == 0:
                                S.add("dve", lambda e, pa=pa, si=si, n=n, mv=mv: e.tensor_tensor(mv, pa[:, 0:n], sg[si][:, 0:n], ALU.mult),
                                      r=[pak, f"sg{si}"], w=[("mT", nb, tb)])
                            else:
                                S.add("dve", lambda e, pa=pa, si=si, n=n: e.tensor_tensor(tt[si][:, 0:n], pa[:, 0:n], sg[si][:, 0:n], ALU.mult),
                                      r=[pak, f"sg{si}"], w=[f"tt{si}"])
                                S.add("dve", lambda e, si=si, n=n, mv=mv: e.tensor_tensor(mv, mv, tt[si][:, 0:n], ALU.add),
                                      r=[f"tt{si}", ("mT", nb, tb)], w=[("mT", nb, tb)])
        S.barrier()
        x1 = at(0, F32, [8, D])
        if "C" in phases:
            mT_keys = [("mT", nb, tb) for nb in range(16) for tb in range(NTB)]
            for j in range(NJ):
                S.add("sp", lambda e, j=j: e.dma_start(out=x1[:, j, :], in_=x_own[j * 128:(j + 1) * 128, :]),
                      w=[("x1", j)], dma=True)
            for fb in range(4):
                wb, wk = wload(w_out[:, fb * 512:(fb + 1) * 512])
                for j in range(NJ):
                    pg, pk = bank()
                    for fc in range(16):
                        S.add("pe", lambda e, pg=pg, fc=fc, j=j, wb=wb: e.matmul(
                            pg, mT[:, fc, j * 128:(j + 1) * 128], wb[:, fc, :], start=(fc == 0), stop=(fc == 15)),
                            r=mT_keys + [wk], w=[pk])
                    xv = x1[:, j, fb * 512:(fb + 1) * 512]
                    S.add("dve", lambda e, pg=pg, xv=xv: e.tensor_tensor(xv, xv, pg, ALU.add), r=[pk, ("x1", j)], w=[("x1", j)])
            if dbg:
                for j in range(NJ):
                    S.add("sp", lambda e, j=j: e.dma_start(out=dbg_t["x1"][j * 128:(j + 1) * 128, :], in_=x1[:, j, :]),
                          r=[("x1", j)], dma=True)


        S.barrier()
        h2 = at(64, BF16, [8, D])
        h2T = at(96, BF16, [16, 1024])
        AR.off = R1 + 140 * 1024
        lg = AR.alloc(F32, [8, 32])
        top8 = AR.alloc(F32, [8, 8])
        mask = AR.alloc(F32, [8, 32])
        maskb = AR.alloc(BF16, [8, 32])
        Gt = AR.alloc(F32, [8, 32])
        pos = AR.alloc(F32, [8, 32])
        sm = AR.alloc(F32, [8, 4])
        wrb = AR.alloc(BF16, [16, 32])
        brb = AR.alloc(BF16, [128, 32])
        D_SMALL_END = AR.off
        posT = gain[1][:, 0:1024]
        GT = gain[1][:, 1024:2048]
        if "D" in phases:
            load_gain(0, g_ffn)
            S.add("pool", lambda e: e.dma_start(out=wrb, in_=w_router.rearrange("(c p) n -> p c n", p=128)), w=["wrb"], dma=True)
            S.add("pool", lambda e: e.dma_start(out=brb[0:1, :], in_=b_router.unsqueeze(0)), w=["brb"], dma=True)
            for j in range(NJ):
                xj = x1[:, j, :]
                S.add("act", lambda e, xj=xj, j=j: e.activation(junkB2, xj, AF.Square, accum_out=sm[:, j, 0:1]),
                      r=[("x1", j)], w=["junkB2", ("sm", j)])
                S.add("act", lambda e, j=j: e.activation(sm[:, j, 0:1], sm[:, j, 0:1], AF.Ln, bias=EPS, scale=1.0 / D), r=[("sm", j)], w=[("sm", j)])
                S.add("act", lambda e, j=j: e.activation(sm[:, j, 0:1], sm[:, j, 0:1], AF.Exp, scale=-0.5), r=[("sm", j)], w=[("sm", j)])
                S.add("dve", lambda e, xj=xj, j=j: e.scalar_tensor_tensor(h2[:, j, :], xj, sm[:, j, 0:1], gain[0], ALU.mult, ALU.mult),
                      r=[("x1", j), ("sm", j), "gain0"], w=[("h2", j)])
                transpose_rows(h2[:, j, :], ("h2", j), h2T, lambda half, j=j: ("h2T", j, half), j * 128)
            for j in range(NJ):
                pg, pk = bank()
                for fc in range(16):
                    S.add("pe", lambda e, pg=pg, fc=fc, j=j: e.matmul(pg[:, 0:32], h2T[:, fc, j * 128:(j + 1) * 128], wrb[:, fc, :],
                                                                  start=(fc == 0), stop=False),
                          r=[("h2T", j, fc // 8), "wrb"], w=[pk])
                S.add("pe", lambda e, pg=pg: e.matmul(pg[:, 0:32], onesb[0:1, :], brb[0:1, :], start=False, stop=True),
                      r=["onesb", "brb"], w=[pk])
                S.add("act", lambda e, pg=pg, j=j: e.copy(lg[:, j, :], pg[:, 0:32]), r=[pk], w=[("lg", j)])
                S.add("dve", lambda e, j=j: e.max(out=top8[:, j, :], in_=lg[:, j, :]), r=[("lg", j)], w=[("top8", j)])
                S.add("dve", lambda e, j=j: e.tensor_scalar(mask[:, j, :], lg[:, j, :], top8[:, j, 3:4], None, ALU.is_ge),
                      r=[("lg", j), ("top8", j)], w=[("mask", j)])
                S.add("dve", lambda e, j=j: e.tensor_scalar(sm[:, j, 1:2], top8[:, j, 0:1], -1.0, None, ALU.mult),
                      r=[("top8", j)], w=[("smb", j)])
                S.add("act", lambda e, j=j: e.activation(Gt[:, j, :], lg[:, j, :], AF.Exp, bias=sm[:, j, 1:2]),
                      r=[("lg", j), ("smb", j)], w=[("Gt", j)])
                S.add("dve", lambda e, j=j: e.tensor_tensor(Gt[:, j, :], Gt[:, j, :], mask[:, j, :], ALU.mult),
                      r=[("Gt", j), ("mask", j)], w=[("Gt", j)])
                S.add("dve", lambda e, j=j: e.reduce_sum(sm[:, j, 2:3], Gt[:, j, :], axis=AX.X), r=[("Gt", j)], w=[("sms", j)])
                S.add("dve", lambda e, j=j: e.reciprocal(sm[:, j, 2:3], sm[:, j, 2:3]), r=[("sms", j)], w=[("sms", j)])
                S.add("dve", lambda e, j=j: e.tensor_scalar(Gt[:, j, :], Gt[:, j, :], sm[:, j, 2:3], None, ALU.mult),
                      r=[("Gt", j), ("sms", j)], w=[("Gt", j)])
                S.add("dve", lambda e, j=j: e.tensor_copy(maskb[:, j, :], mask[:, j, :]), r=[("mask", j)], w=[("maskb", j)])
            for j in range(NJ):
                pg, pk = bank()
                for j2 in range(j):
                    S.add("pe", lambda e, pg=pg, j2=j2: e.matmul(pg[:, 0:32], onesb, maskb[:, j2, :], start=(j2 == 0), stop=False),
                          r=["onesb", ("maskb", j2)], w=[pk])
                S.add("pe", lambda e, pg=pg, j=j: e.matmul(pg[:, 0:32], SLTb, maskb[:, j, :], start=(j == 0), stop=True),
                      r=["SLTb", ("maskb", j)], w=[pk])
                S.add("act", lambda e, pg=pg, j=j: e.copy(pos[:, j, :], pg[:, 0:32]), r=[pk], w=[("pos", j)])
        S.barrier()
        ident32 = at(96, F32, [128, 128])
        XeT = AR.alloc(BF16, [16, CAP])
        actT = AR.alloc(BF16, [16, CAP])
        bgr_off = AR.off
        Oe = AR.alloc(BF16, [2, D])
        Sel = AR.alloc(BF16, [8, CAP])
        SelT = AR.alloc(BF16, [2, 1024])
        Gbs = AR.alloc(BF16, [128, 1024])
        bguT = AR.alloc(F32, [32, 32])
        oh = [AR.alloc(F32, [128, 128]) for _ in range(2)]
        glc = [AR.alloc(F32, [128, CAP]) for _ in range(2)]
        sgm = [AR.alloc(F32, [128, CAP]) for _ in range(2)]
        assert AR.off <= R1 + 140 * 1024, AR.off - R1
        _save = AR.off
        AR.off = bgr_off
        bgr = AR.alloc(F32, [128, 4096])
        AR.off = _save
        if "D" in phases:
            S.add("dve", lambda e: e.tensor_scalar(ident32, io_row[:, 0:128], pidx[:, 0:1], None, ALU.is_equal),
                  r=["io_row", "pidx"], w=["ident32"])
            for j in range(NJ):
                pg, pk = bank()
                S.add("pe", lambda e, pg=pg, j=j: e.transpose(pg[0:32, 0:128], pos[:, j, :], ident32), r=[("pos", j), "ident32"], w=[pk])
                S.add("pe", lambda e, pg=pg, j=j: e.transpose(pg[0:32, 128:256], Gt[:, j, :], ident32), r=[("Gt", j), "ident32"], w=[pk])
                S.add("act", lambda e, pg=pg, j=j: e.copy(posT[0:32, j * 128:(j + 1) * 128], pg[0:32, 0:128]), r=[pk], w=[("posT", j)])
                S.add("act", lambda e, pg=pg, j=j: e.copy(GT[0:32, j * 128:(j + 1) * 128], pg[0:32, 128:256]), r=[pk], w=[("GT", j)])
            S.add("sp", lambda e: e.dma_start(out=bgr[0:32, :], in_=b_gate_up[:, :]), w=["bgr"], dma=True)
            for half in range(2):
                pg, pk = bank()
                for c in range(16):
                    cc_ = half * 16 + c
                    S.add("pe", lambda e, pg=pg, c=c, cc_=cc_: e.transpose(pg[:, c * 32:(c + 1) * 32], bgr[0:32, cc_ * 128:(cc_ + 1) * 128],
                                                                       ident32[0:32, 0:32]), r=["bgr", "ident32"], w=[pk])
                S.add("act", lambda e, pg=pg, half=half: e.copy(bguT[:, half * 16:(half + 1) * 16, :],
                                                             pg.rearrange("p (c e) -> p c e", c=16)), r=[pk], w=[("bguT", half)])
            posT_keys = [("posT", j) for j in range(NJ)]
            GT_keys = [("GT", j) for j in range(NJ)]
            NTB = (NT + 511) // 512
            S.barrier()
            def moe_prep(ex):
                oi = ex % 2
                S.add("dve", lambda e, oi=oi, ex=ex: e.tensor_scalar(oh[oi][0:32, :], ones32[0:32, :], ident32[0:32, ex:ex + 1], None, ALU.mult),
                      r=["ones32", "ident32"], w=[f"oh{oi}"])
                for tb in range(NTB):
                    n = min(512, NT - tb * 512)
                    pgG, pgGk = bank()
                    S.add("pe", lambda e, pgG=pgG, oi=oi, tb=tb, n=n: e.matmul(pgG[:, 0:n], oh[oi][0:32, :], GT[0:32, tb * 512:tb * 512 + n],
                                                                          start=True, stop=True), r=[f"oh{oi}"] + GT_keys, w=[pgGk])
                    S.add("act", lambda e, pgG=pgG, tb=tb, n=n: e.copy(Gbs[:, tb * 512:tb * 512 + n], pgG[:, 0:n]), r=[pgGk], w=[("Gbs", tb)])
                    pgP, pgPk = bank()
                    S.add("pe", lambda e, pgP=pgP, oi=oi, tb=tb, n=n: e.matmul(pgP[:, 0:n], oh[oi][0:32, :], posT[0:32, tb * 512:tb * 512 + n],
                                                                          start=True, stop=True), r=[f"oh{oi}"] + posT_keys, w=[pgPk])
                    for stt in range(2):
                        S.add("dve", lambda e, pgP=pgP, stt=stt, tb=tb, n=n: e.scalar_tensor_tensor(
                            SelTs[ex % 2][:, stt, tb * 512:tb * 512 + n], pgP[:, 0:n], pidx[:, stt:stt + 1], Gbs[:, tb * 512:tb * 512 + n],
                            ALU.is_equal, ALU.mult), r=[pgPk, "pidx", ("Gbs", tb)], w=[("SelT", ex % 2, stt, tb)])
                for j in range(NJ):
                    S.add("dve", lambda e, j=j, ex=ex: e.tensor_scalar(
                        Sel[:, j, :], io_row[:, 0:CAP], pos[:, j, ex:ex + 1], mask[:, j, ex:ex + 1], ALU.is_equal, ALU.mult),
                        r=["io_row", ("pos", j), ("mask", j)], w=[("Sel", j)])
            def moe_gather(ex):
                for fc in range(16):
                    pg, pk = bank()
                    for j in range(NJ):
                        S.add("pe", lambda e, pg=pg, fc=fc, j=j: e.matmul(pg[:, 0:CAP], h2[:, j, fc * 128:(fc + 1) * 128], Sel[:, j, :],
                                                                      start=(j == 0), stop=(j == NJ - 1)),
                              r=[("h2", j), ("Sel", j)], w=[pk])
                    S.add("act", lambda e, pg=pg, fc=fc: e.copy(XeT[:, fc, :], pg[:, 0:CAP]), r=[pk], w=[("XeT", fc)])
            def moe_gu(ex, p_lo, p_hi):
                for piece in range(p_lo, p_hi):
                    wb, wk = wload(w_gate_up[ex][:, piece * 512:(piece + 1) * 512])
                    for sub in range(4):
                        nci = piece * 4 + sub
                        pg, pk = bank()
                        for fc in range(16):
                            S.add("pe", lambda e, pg=pg, fc=fc, sub=sub, wb=wb: e.matmul(
                                pg[:, 0:CAP], wb[:, fc, sub * 128:(sub + 1) * 128], XeT[:, fc, :], start=(fc == 0), stop=(fc == 15)),
                                r=XeT_keys + [wk], w=[pk])
                        gi = nci % 2
                        bias_ap = bguT[:, nci, ex:ex + 1]
                        if nci < 16:
                            S.add("dve", lambda e, pg=pg, gi=gi, bias_ap=bias_ap: e.tensor_scalar(
                                glc[gi], pg[:, 0:CAP], bias_ap, 7.0, ALU.add, ALU.min), r=[pk, ("bguT", nci // 16)], w=[f"glc{gi}"])
                            S.add("act", lambda e, gi=gi: e.activation(sgm[gi], glc[gi], AF.Sigmoid, scale=1.702), r=[f"glc{gi}"], w=[f"sgm{gi}"])
                            S.add("dve", lambda e, gi=gi, nci=nci: e.tensor_tensor(actT[:, nci, :], glc[gi], sgm[gi], ALU.mult),
                                  r=[f"glc{gi}", f"sgm{gi}"], w=[("actT", nci)])
                        else:
                            m_ = nci - 16
                            S.add("dve", lambda e, pg=pg, gi=gi, bias_ap=bias_ap: e.tensor_scalar(
                                glc[gi], pg[:, 0:CAP], bias_ap, 7.0, ALU.add, ALU.min), r=[pk, ("bguT", nci // 16)], w=[f"glc{gi}"])
                            S.add("dve", lambda e, gi=gi: e.tensor_scalar(sgm[gi], glc[gi], -7.0, 1.0, ALU.max, ALU.add),
                                  r=[f"glc{gi}"], w=[f"sgm{gi}"])
                            S.add("dve", lambda e, gi=gi, m_=m_: e.tensor_tensor(actT[:, m_, :], actT[:, m_, :], sgm[gi], ALU.mult),
                                  r=[("actT", m_), f"sgm{gi}"], w=[("actT", m_)])
            def moe_down(ex):
                for fb in range(4):
                    wb, wk = wload(w_down[ex][:, fb * 512:(fb + 1) * 512])
                    for stt in range(2):
                        pg, pk = bank()
                        for mc in range(16):
                            S.add("pe", lambda e, pg=pg, mc=mc, stt=stt, wb=wb: e.matmul(
                                pg, actT[:, mc, stt * 128:(stt + 1) * 128], wb[:, mc, :], start=(mc == 0), stop=(mc == 15)),
                                r=actT_keys + [wk], w=[pk])
                        S.add("act", lambda e, pg=pg, stt=stt, fb=fb: e.copy(Oe[:, stt, fb * 512:(fb + 1) * 512], pg), r=[pk], w=[("Oe", stt, fb)])
            def moe_scatter(ex):
                for j in range(NJ):
                    for fb in range(4):
                        pg, pk = bank()
                        for stt in range(2):
                            S.add("pe", lambda e, pg=pg, stt=stt, j=j, fb=fb: e.matmul(
                                pg, SelTs[ex % 2][:, stt, j * 128:(j + 1) * 128], Oe[:, stt, fb * 512:(fb + 1) * 512], start=(stt == 0), stop=(stt == 1)),
                                r=[("SelT", ex % 2, stt, j // 4), ("Oe", stt, fb)], w=[pk])
                        xv = x1[:, j, fb * 512:(fb + 1) * 512]
                        S.add("dve", lambda e, pg=pg, xv=xv: e.tensor_tensor(xv, xv, pg, ALU.add), r=[pk, ("x1", j)], w=[("x1", j)])

            XeT_keys = [("XeT", fc) for fc in range(16)]
            actT_keys = [("actT", m_) for m_ in range(16)]
            SelTs = [SelT, gain[0].bitcast(BF16)[:, 0:2048].rearrange("p (a b) -> p a b", a=2)]
            moe_prep(0)
            moe_gather(0)
            for ex in range(NE):
                moe_gu(ex, 0, 3)
                if ex > 0:
                    moe_scatter(ex - 1)
                moe_gu(ex, 3, 8)
                if ex + 1 < NE:
                    moe_prep(ex + 1)
                moe_down(ex)
                if ex + 1 < NE:
                    moe_gather(ex + 1)
            moe_scatter(NE - 1)
            S.barrier()
            S.add("sp", lambda e: e.dma_start(out=bgr[0:32, 0:D], in_=b_down[:, :]), w=["bgr"], dma=True)
            for j in range(NJ):
                for fb in range(4):
                    pg, pk = bank()
                    S.add("pe", lambda e, pg=pg, j=j, fb=fb: e.matmul(pg, GT[0:32, j * 128:(j + 1) * 128], bgr[0:32, fb * 512:(fb + 1) * 512],
                                                                    start=True, stop=True), r=[("GT", j), "bgr"], w=[pk])
                    xv = x1[:, j, fb * 512:(fb + 1) * 512]
                    S.add("dve", lambda e, pg=pg, xv=xv: e.tensor_tensor(xv, xv, pg, ALU.add), r=[pk, ("x1", j)], w=[("x1", j)])
            if dbg:
                for j in range(NJ):
                    S.add("sp", lambda e, j=j: e.dma_start(out=dbg_t["x2"][j * 128:(j + 1) * 128, :], in_=x1[:, j, :]),
                          r=[("x1", j)], dma=True)

        S.barrier()
        h3T = at(64, BF16, [16, 1024])
        AR.off = R1 + 96 * 1024
        h3b = AR.alloc(BF16, [128, D])
        pT_ = AR.alloc(BF16, [2, 1024])
        pin = AR.alloc(F32, [128, 256])
        pinb = AR.alloc(BF16, [128, 256])
        u = AR.alloc(F32, [128, D])
        u4 = AR.alloc(BF16, [4, D])
        sgE = [AR.alloc(F32, [128, 512]) for _ in range(2)]
        obuf = AR.alloc(F32, [128, D])
        smE = AR.alloc(F32, [8, 4])
        if "E" in phases:
            load_gain(0, g_ple)
            for j in range(NJ):
                xj = x1[:, j, :]
                S.add("act", lambda e, xj=xj, j=j: e.activation(junkB2, xj, AF.Square, accum_out=smE[:, j, 0:1]),
                      r=[("x1", j)], w=["junkB2", ("smE", j)])
                S.add("act", lambda e, j=j: e.activation(smE[:, j, 0:1], smE[:, j, 0:1], AF.Ln, bias=EPS, scale=1.0 / D), r=[("smE", j)], w=[("smE", j)])
                S.add("act", lambda e, j=j: e.activation(smE[:, j, 0:1], smE[:, j, 0:1], AF.Exp, scale=-0.5), r=[("smE", j)], w=[("smE", j)])
                S.add("dve", lambda e, xj=xj, j=j: e.scalar_tensor_tensor(h3b, xj, smE[:, j, 0:1], gain[0], ALU.mult, ALU.mult),
                      r=[("x1", j), ("smE", j), "gain0"], w=["h3b"])
                transpose_rows(h3b, "h3b", h3T, lambda half, j=j: ("h3T", j, half), j * 128)
                S.add("sp", lambda e, j=j: e.dma_start(out=pin, in_=p_own[j * 128:(j + 1) * 128, :]), w=["pin"], dma=True)
                S.add("dve", lambda e: e.tensor_copy(pinb, pin), r=["pin"], w=["pinb"])
                pt, ptk = tbank()
                for k in range(2):
                    S.add("pe", lambda e, pt=pt, k=k: e.transpose(pt[:, k * 128:(k + 1) * 128], pinb[:, k * 128:(k + 1) * 128], ident),
                          r=["pinb", "ident"], w=[ptk])
                S.add("act", lambda e, pt=pt, j=j: e.copy(pT_[:, :, j * 128:(j + 1) * 128], pt[:, 0:256].rearrange("p (a b) -> p a b", a=2)),
                      r=[ptk], w=[("pT", j)])
            load_gain(0, g_ple_post)
            load_gain(1, g_final)
            for hf in range((NJ + 3) // 4):
              tiles = list(range(hf * 4, min(NJ, hf * 4 + 4)))
              for fb in range(4):
                wb, wk = wload(w_ple_gate[:, fb * 512:(fb + 1) * 512])
                wp, wpk = wload(w_ple_proj[:, fb * 512:(fb + 1) * 512], rows=256)
                for j in tiles:
                    pg, pk = bank()
                    for fc in range(16):
                        S.add("pe", lambda e, pg=pg, fc=fc, j=j, wb=wb: e.matmul(pg, h3T[:, fc, j * 128:(j + 1) * 128], wb[:, fc, :],
                                                                             start=(fc == 0), stop=(fc == 15)),
                              r=[("h3T", j, fc // 8), wk], w=[pk])
                    pp_, ppk = bank()
                    for k in range(2):
                        S.add("pe", lambda e, pp_=pp_, k=k, j=j, wp=wp: e.matmul(pp_, pT_[:, k, j * 128:(j + 1) * 128], wp[:, k, :],
                                                                             start=(k == 0), stop=(k == 1)),
                              r=[("pT", j), wpk], w=[ppk])
                    si = j % 2
                    S.add("act", lambda e, pg=pg, si=si: e.activation(sgE[si], pg, AF.Sigmoid), r=[pk], w=[f"sgE{si}"])
                    S.add("dve", lambda e, pp_=pp_, si=si, fb=fb, j=j: e.tensor_tensor(u4[:, j % 4, fb * 512:(fb + 1) * 512], pp_, sgE[si], ALU.mult),
                          r=[ppk, f"sgE{si}"], w=[("u4", j % 4, fb)])
              for j in tiles:
                ukeys = [("u4", j % 4, fb) for fb in range(4)]
                uj = u4[:, j % 4, :]
                S.add("act", lambda e, j=j, uj=uj: e.activation(junkB2, uj, AF.Square, accum_out=smE[:, j, 1:2]), r=ukeys, w=["junkB2", ("smE1", j)])
                S.add("act", lambda e, j=j: e.activation(smE[:, j, 1:2], smE[:, j, 1:2], AF.Ln, bias=EPS, scale=1.0 / D), r=[("smE1", j)], w=[("smE1", j)])
                S.add("act", lambda e, j=j: e.activation(smE[:, j, 1:2], smE[:, j, 1:2], AF.Exp, scale=-0.5), r=[("smE1", j)], w=[("smE1", j)])
                S.add("dve", lambda e, j=j, uj=uj: e.scalar_tensor_tensor(u, uj, smE[:, j, 1:2], gain[0], ALU.mult, ALU.mult),
                      r=ukeys + [("smE1", j), "gain0"], w=["u32"])
                xj = x1[:, j, :]
                S.add("dve", lambda e, xj=xj: e.tensor_tensor(xj, xj, u, ALU.add), r=["u32", ("x1", j)], w=[("x1", j)])
                S.add("act", lambda e, xj=xj, j=j: e.activation(junkB2, xj, AF.Square, accum_out=smE[:, j, 2:3]), r=[("x1", j)], w=["junkB2", ("smE2", j)])
                S.add("act", lambda e, j=j: e.activation(smE[:, j, 2:3], smE[:, j, 2:3], AF.Ln, bias=EPS, scale=1.0 / D), r=[("smE2", j)], w=[("smE2", j)])
                S.add("act", lambda e, j=j: e.activation(smE[:, j, 2:3], smE[:, j, 2:3], AF.Exp, scale=-0.5), r=[("smE2", j)], w=[("smE2", j)])
                S.add("dve", lambda e, xj=xj, j=j: e.scalar_tensor_tensor(obuf, xj, smE[:, j, 2:3], gain[1], ALU.mult, ALU.mult),
                      r=[("x1", j), ("smE2", j), "gain1"], w=["obuf"])
                S.add("sp", lambda e, j=j: e.dma_start(out=out[j * 128:(j + 1) * 128, :], in_=obuf), r=["obuf"], dma=True)
                out_dmas.append(len(S.ops) - 1)

        S.barrier()
        outs = [i for i, o in enumerate(S.ops) if o["dma"]]
        S.wait_all("sp", outs[-32:] + out_dmas)
        S.emit()
    return nc


def make_inputs(inputs, core):
    b, q = core // 4, core % 4
    x = np.asarray(inputs["x"], dtype=np.float32)
    p = np.asarray(inputs["p"], dtype=np.float32)
    own = np.concatenate([np.arange((4 * j + q) * 128, (4 * j + q + 1) * 128) for j in range(8)])
    m = {}
    pad = 3 - q
    xa = np.zeros((SEQ, D), np.float32)
    xa[pad * 128:] = x[b][:SEQ - pad * 128]
    m["x_all"] = xa
    m["x_own"] = np.ascontiguousarray(x[b][own])
    m["p_own"] = np.ascontiguousarray(p[0, b][own])
    ms = np.zeros((128, 4), np.float32)
    for kb in range(4):
        ms[:, kb] = 1.0 if kb >= pad else 0.0
    m["msel"] = ms
    am = np.zeros((128, 4, 128), np.float32)
    am[:, 3, :] = (np.arange(128)[:, None] < np.arange(128)[None, :]).astype(np.float32)
    m["amask"] = am
    for k in ["w_in", "conv_w", "conv_b", "dt_bias", "a_log", "d_skip", "ssd_norm_w", "w_branch_a", "w_branch_b",
              "w_out", "g_mix", "g_ffn", "w_router", "b_router", "w_gate_up", "b_gate_up", "w_down", "b_down",
              "g_ple", "w_ple_gate", "w_ple_proj", "g_ple_post"]:
        m[k] = np.ascontiguousarray(np.asarray(inputs[k], dtype=np.float32)[0])
    m["g_final"] = np.ascontiguousarray(np.asarray(inputs["g_final"], dtype=np.float32))
    return m, own


def kernel(**inputs):
    nc = build()
    in_maps = []
    owns = []
    for c in range(8):
        m, own = make_inputs(inputs, c)
        in_maps.append(m)
        owns.append(own)
    res = run_bass_kernel_spmd(nc, in_maps, core_ids=list(range(8)))
    outp = np.zeros((2, SEQ, D), np.float32)
    for c in range(8):
        outp[c // 4, owns[c]] = res.results[c]["out"]
    return outp
```

```python
import numpy as np
from contextlib import ExitStack
import concourse.bass as bass
import concourse.mybir as mybir
from concourse.bass_utils import run_bass_kernel_spmd

F32 = mybir.dt.float32
BF16 = mybir.dt.bfloat16
AF = mybir.ActivationFunctionType
ALU = mybir.AluOpType
AX = mybir.AxisListType

ENGS = ("pe", "act", "dve", "pool", "sp")
NDMASEM = 8
D = 2048
SEQ = 4096
NE = 32
CAP = 256
EPS = 1e-6
IN_DIM = 15392
C_Z, C_XBC, C_DT, C_Q, C_K, C_V, C_GA, C_GB = 0, 2048, 5120, 5152, 7200, 9248, 11296, 13344


class Sched:
    def __init__(self, nc, stack):
        self.nc = nc
        self.ops = []
        self.last_w = {}
        self.rd_eng = {}
        self.rd_dma = {}
        self.stack = stack

    def add(self, eng, fn, r=(), w=(), dma=False, prio=None):
        oid = len(self.ops)
        deps = set()
        for k in r:
            d = self.last_w.get(k)
            if d is not None:
                deps.add(d)
        for k in w:
            d = self.last_w.get(k)
            if d is not None:
                deps.add(d)
            for d in self.rd_eng.get(k, {}).values():
                deps.add(d)
            for d in self.rd_dma.get(k, ()):
                deps.add(d)
        for k in w:
            self.last_w[k] = oid
            self.rd_eng[k] = {}
            self.rd_dma[k] = []
        for k in r:
            if dma:
                self.rd_dma.setdefault(k, []).append(oid)
            else:
                self.rd_eng.setdefault(k, {})[eng] = oid
        deps.discard(oid)
        self.ops.append(dict(eng=eng, fn=fn, deps=deps, dma=dma, prio=(oid if prio is None else prio)))
        return oid

    def wait_all(self, eng, ids):
        self.ops.append(dict(eng=eng, fn=None, deps=set(ids), dma=False, prio=len(self.ops)))

    def barrier(self):
        self.nbar = getattr(self, "nbar", 0) + 1
        last = {}
        dmas = []
        for i, o in enumerate(self.ops):
            if o["fn"] is None:
                continue
            if o["dma"]:
                dmas.append(i)
            else:
                last[o["eng"]] = i
        ids = list(last.values()) + dmas[-64:]
        for e in ENGS:
            self.wait_all(e, ids)

    def emit(self):
        nc = self.nc
        ops = self.ops

        def skip(p, o):
            return (not p["dma"]) and (not o["dma"]) and p["eng"] == o["eng"] and p["eng"] == "pe"

        need = [False] * len(ops)
        for o in ops:
            for d in o["deps"]:
                if not skip(ops[d], o):
                    need[d] = True
        per_eng = {e: [] for e in ENGS}
        for i, o in enumerate(ops):
            per_eng[o["eng"]].append(i)
        for e in ENGS:
            per_eng[e].sort(key=lambda i: (ops[i]["prio"], i))
        cnt = {e: 0 for e in ENGS}
        dcnt = {e: 0 for e in ENGS}
        for e in ENGS:
            for i in per_eng[e]:
                o = ops[i]
                o["sig"] = None
                if o["fn"] is None:
                    continue
                if o["dma"]:
                    n = dcnt[e]
                    dcnt[e] += 1
                    o["sig"] = ("d", e, n % NDMASEM, 16 * (n // NDMASEM + 1))
                elif need[i]:
                    cnt[e] += 1
                    o["sig"] = ("c", e, 0, cnt[e])
        sems = {}
        for e in ENGS:
            sems[("c", e, 0)] = self.stack.enter_context(nc.semaphore(f"c_{e}"))
            if dcnt[e] > 0:
                for k in range(NDMASEM):
                    sems[("d", e, k)] = self.stack.enter_context(nc.semaphore(f"d_{e}_{k}"))

        def run_engine(ename, handle):
            known = {}
            for i in per_eng[ename]:
                o = ops[i]
                waits = {}
                for d in o["deps"]:
                    p = ops[d]
                    if skip(p, o):
                        continue
                    s = p["sig"]
                    if waits.get(s[:3], 0) < s[3]:
                        waits[s[:3]] = s[3]
                for key, v in waits.items():
                    if known.get(key, 0) >= v:
                        continue
                    known[key] = v
                    handle.wait_ge(sems[key], v)
                if o["fn"] is None:
                    continue
                ins = o["fn"](handle)
                s = o["sig"]
                if s is not None:
                    ins.then_inc(sems[s[:3]], 16 if s[0] == "d" else 1)

        block = self.stack.enter_context(nc.Block())

        @block.tensor
        def _(e):
            run_engine("pe", e)

        @block.scalar
        def _(e):
            run_engine("act", e)

        @block.vector
        def _(e):
            run_engine("dve", e)

        @block.gpsimd
        def _(e):
            run_engine("pool", e)

        @block.sync
        def _(e):
            run_engine("sp", e)


class Arena:
    def __init__(self, t, nbytes):
        self.t = t
        self.nbytes = nbytes
        self.off = 0

    def alloc(self, dt, shape):
        shape = list(shape)
        if shape[0] == 128 and len(shape) >= 2:
            shape = shape[1:]
        n = int(np.prod(shape))
        sz = n * (4 if dt == F32 else 2)
        sz = (sz + 31) // 32 * 32
        assert self.off + sz <= self.nbytes, ("arena overflow", self.off, sz)
        v = self.t[:, self.off // 4:(self.off + sz) // 4]
        self.off += sz
        if dt != F32:
            v = v.bitcast(dt)
        v = v[:, 0:n]
        if len(shape) == 2:
            v = v.rearrange("p (a b) -> p a b", a=shape[0])
        elif len(shape) == 3:
            v = v.rearrange("p (a b c) -> p a b c", a=shape[0], b=shape[1])
        return v


def build(NG=8, dbg=False, phases="ABCDE"):
    nc = bass.Bass("TRN2", target_bir_lowering=False)
    dram_in = lambda n, s: nc.dram_tensor(n, list(s), F32, kind="ExternalInput").ap()
    x_all = dram_in("x_all", [SEQ, D])
    x_own = dram_in("x_own", [1024, D])
    p_own = dram_in("p_own", [1024, 256])
    msel = dram_in("msel", [128, 4])
    amask = dram_in("amask", [128, 4, 128])
    w_in = dram_in("w_in", [D, IN_DIM])
    conv_w = dram_in("conv_w", [4, 3072])
    conv_b = dram_in("conv_b", [3072])
    dt_bias = dram_in("dt_bias", [32])
    a_log = dram_in("a_log", [32])
    d_skip = dram_in("d_skip", [32])
    ssd_norm_w = dram_in("ssd_norm_w", [D])
    w_branch_a = dram_in("w_branch_a", [D, D])
    w_branch_b = dram_in("w_branch_b", [D, D])
    w_out = dram_in("w_out", [D, D])
    g_mix = dram_in("g_mix", [D])
    g_ffn = dram_in("g_ffn", [D])
    w_router = dram_in("w_router", [D, NE])
    b_router = dram_in("b_router", [NE])
    w_gate_up = dram_in("w_gate_up", [NE, D, 2 * D])
    b_gate_up = dram_in("b_gate_up", [NE, 2 * D])
    w_down = dram_in("w_down", [NE, D, D])
    b_down = dram_in("b_down", [NE, D])
    g_ple = dram_in("g_ple", [D])
    w_ple_gate = dram_in("w_ple_gate", [D, D])
    w_ple_proj = dram_in("w_ple_proj", [256, D])
    g_ple_post = dram_in("g_ple_post", [D])
    g_final = dram_in("g_final", [D])
    out = nc.dram_tensor("out", [1024, D], F32, kind="ExternalOutput").ap()
    kT_d = nc.dram_tensor("kT_d", [16, 128, SEQ], BF16).ap()
    v_d = nc.dram_tensor("v_d", [16, 128, 32, 128], BF16).ap()
    ya_d = nc.dram_tensor("ya_d", [8, 128, D], BF16).ap()
    dbg_t = {}
    if dbg:
        dbg_t["ya"] = nc.dram_tensor("dbg_ya", [NG, 128, D], F32, kind="ExternalOutput").ap()
        dbg_t["yb"] = nc.dram_tensor("dbg_yb", [128, 16, 1024], F32, kind="ExternalOutput").ap()
        dbg_t["x1"] = nc.dram_tensor("dbg_x1", [1024, D], F32, kind="ExternalOutput").ap()
        dbg_t["q"] = nc.dram_tensor("dbg_q", [128, 16, 1024], F32, kind="ExternalOutput").ap()
        dbg_t["x2"] = nc.dram_tensor("dbg_x2", [1024, D], F32, kind="ExternalOutput").ap()

    st = ExitStack()
    with st:
        S = Sched(nc, st)
        ARN = 207 * 1024
        arena_t = st.enter_context(nc.sbuf_tensor("arena", [128, ARN // 4], F32))
        AR = Arena(arena_t, ARN)
        pgs = [st.enter_context(nc.psum_tensor(f"pg{i}", [128, 512], F32))[:] for i in range(6)]
        pts = [st.enter_context(nc.psum_tensor(f"pt{i}", [128, 1024], BF16))[:] for i in range(2)]
        cnt = {"pg": 0, "pt": 0, "wb": 0, "npg": 6}

        def bank():
            i = cnt["pg"] % cnt["npg"]
            cnt["pg"] += 1
            return pgs[i], f"pg{i}"

        def tbank():
            i = cnt["pt"] % 2
            cnt["pt"] += 1
            return pts[i], f"pt{i}"

        out_dmas = []

        ident = AR.alloc(BF16, [128, 128])
        io_row = AR.alloc(F32, [128, 256])
        pidx = AR.alloc(F32, [128, 2])
        UI = AR.alloc(F32, [128, 128])
        SL = AR.alloc(F32, [128, 128])
        ones32 = AR.alloc(F32, [128, 128])
        UIb = AR.alloc(BF16, [128, 128])
        onesb = AR.alloc(BF16, [128, 128])
        SLTb = AR.alloc(BF16, [128, 128])
        junkB2 = AR.alloc(BF16, [128, D])
        msel_t = AR.alloc(F32, [128, 4])
        amask_t = AR.alloc(F32, [128, 4, 128])
        gain = [AR.alloc(F32, [128, D]) for _ in range(2)]
        CONST_END = AR.off

        S.add("pool", lambda e: e.iota(io_row, pattern=[[1, 256]], base=0, channel_multiplier=0,
                                       allow_small_or_imprecise_dtypes=True), w=["io_row"])
        S.add("pool", lambda e: e.iota(pidx[:, 0:1], pattern=[[0, 1]], base=0, channel_multiplier=1,
                                       allow_small_or_imprecise_dtypes=True), w=["pidx"])
        S.add("pool", lambda e: e.iota(pidx[:, 1:2], pattern=[[0, 1]], base=128, channel_multiplier=1,
                                       allow_small_or_imprecise_dtypes=True), w=["pidx"])
        S.add("dve", lambda e: e.tensor_scalar(ones32, io_row[:, 0:128], pidx[:, 0:1], None, ALU.is_equal),
              r=["io_row", "pidx"], w=["ones32"])
        S.add("dve", lambda e: e.tensor_copy(ident, ones32), r=["ones32"], w=["ident"])
        S.add("dve", lambda e: e.tensor_scalar(UI, io_row[:, 0:128], pidx[:, 0:1], None, ALU.is_ge),
              r=["io_row", "pidx"], w=["UI"])
        S.add("dve", lambda e: e.tensor_scalar(SL, io_row[:, 0:128], pidx[:, 0:1], None, ALU.is_lt),
              r=["io_row", "pidx"], w=["SL"])
        S.add("dve", lambda e: e.tensor_scalar(UIb, io_row[:, 0:128], pidx[:, 0:1], None, ALU.is_le),
              r=["io_row", "pidx"], w=["UIb"])
        S.add("dve", lambda e: e.tensor_scalar(SLTb, io_row[:, 0:128], pidx[:, 0:1], None, ALU.is_gt),
              r=["io_row", "pidx"], w=["SLTb"])
        S.add("pool", lambda e: e.memset(ones32, 1.0), r=["ident"], w=["ones32"])
        S.add("pool", lambda e: e.memset(onesb, 1.0), w=["onesb"])
        S.add("sp", lambda e: e.dma_start(out=msel_t, in_=msel[:, :]), w=["msel"], dma=True)
        S.add("sp", lambda e: e.dma_start(out=amask_t, in_=amask[:, :, :]), w=["amask"], dma=True)

        def load_gain(slot, src):
            S.add("sp", lambda e: e.dma_start(out=gain[slot], in_=src.partition_broadcast(128)),
                  w=[f"gain{slot}"], dma=True)

        NWB = 2
        wbs = [AR.alloc(BF16, [16, 512]) for _ in range(NWB)]
        WB_END = AR.off

        def wload(src2d, ncols=512, rows=D):
            i = cnt["wb"] % NWB
            cnt["wb"] += 1
            nch = rows // 128
            dst = wbs[i][:, 0:nch, 0:ncols]
            pos_now = len(S.ops)
            pr = cnt.get("wmark")
            S.add("pool", lambda e: e.dma_start(out=dst, in_=src2d.rearrange("(c p) n -> p c n", p=128)),
                  w=[f"wb{i}"], dma=True)
            cnt["wmark"] = pos_now
            return wbs[i], f"wb{i}"

        def rmsnorm_rows(xt, xkey, gslot, outb, outkey, ss, sskey, junk, junkkey):
            S.add("act", lambda e: e.activation(junk, xt, AF.Square, accum_out=ss), r=[xkey], w=[junkkey, sskey])
            S.add("act", lambda e: e.activation(ss, ss, AF.Ln, bias=EPS, scale=1.0 / D), r=[sskey], w=[sskey])
            S.add("act", lambda e: e.activation(ss, ss, AF.Exp, scale=-0.5), r=[sskey], w=[sskey])
            S.add("dve", lambda e: e.scalar_tensor_tensor(outb, xt, ss, gain[gslot], ALU.mult, ALU.mult),
                  r=[xkey, sskey, f"gain{gslot}"], w=[outkey])

        def transpose_rows(srcb, srckey, dstT, dstkey_fn, tok0, ntok=128, nfc=16):
            for half in range(nfc // 8):
                pt, ptk = tbank()
                for k in range(8):
                    fc = half * 8 + k
                    S.add("pe", lambda e, fc=fc, k=k, pt=pt: e.transpose(pt[:, k * 128:(k + 1) * 128],
                                                                        srcb[:, fc * 128:(fc + 1) * 128], ident),
                          r=[srckey, "ident"], w=[ptk])
                S.add("act", lambda e, half=half, pt=pt: e.copy(
                    dstT[:, half * 8:(half + 1) * 8, tok0:tok0 + ntok],
                    pt.rearrange("p (a b) -> p a b", a=8)),
                    r=[ptk], w=[dstkey_fn(half)])

        A0 = AR.off
        xt = AR.alloc(F32, [128, D])
        hb = AR.alloc(BF16, [128, D])
        hT = AR.alloc(BF16, [16, 512])
        zs = AR.alloc(BF16, [4, D])
        xtm = AR.alloc(BF16, [4, D])
        Btm = AR.alloc(BF16, [4, 512])
        BT = AR.alloc(BF16, [4, 512])
        CT = AR.alloc(BF16, [4, 512])
        raw = [AR.alloc(F32, [128, 515]) for _ in range(2)]
        ctmp = [AR.alloc(F32, [128, 512]) for _ in range(2)]
        xcb = [AR.alloc(BF16, [128, 512]) for _ in range(2)]
        halo = AR.alloc(F32, [24, 3])
        convw_t = AR.alloc(F32, [4, 24])
        convb_t = AR.alloc(F32, [128, 24])
        wdt = AR.alloc(BF16, [16, 32])
        dtb_b = AR.alloc(F32, [128, 32])
        negA_b = AR.alloc(F32, [128, 32])
        dskip_b = AR.alloc(F32, [128, 32])
        ss = AR.alloc(F32, [128, 8])
        dt_all = AR.alloc(F32, [4, 32])
        a_all = AR.alloc(F32, [4, 32])
        dtr = AR.alloc(F32, [128, 32])
        acs = AR.alloc(F32, [128, 64])
        eacs = AR.alloc(F32, [128, 32])
        dst = AR.alloc(F32, [128, 32])
        cdec = AR.alloc(F32, [128, 32])
        Xdt = AR.alloc(BF16, [128, D])
        Xds = AR.alloc(BF16, [128, D])
        CBm = AR.alloc(F32, [4, 128])
        lh = [AR.alloc(F32, [8, 128]) for _ in range(2)]
        dec = [AR.alloc(F32, [4, 128]) for _ in range(2)]
        Mb = [AR.alloc(BF16, [4, 128]) for _ in range(2)]
        yo = [AR.alloc(F32, [128, 512]) for _ in range(2)]
        yy = [AR.alloc(F32, [128, 512]) for _ in range(2)]
        ssg = AR.alloc(F32, [128, 4])
        rm = AR.alloc(F32, [128, 4])
        state = AR.alloc(F32, [128, D])
        prevb = AR.alloc(BF16, [128, D])
        kst = [AR.alloc(BF16, [128, 512]) for _ in range(2)]
        vst = [AR.alloc(BF16, [128, 512]) for _ in range(2)]
        ya_cur = AR.alloc(BF16, [128, D])
        A_END = AR.off

        if "A" in phases:
            load_gain(0, g_mix)
            load_gain(1, ssd_norm_w)
            for k in range(4):
                S.add("sp", lambda e, k=k: e.dma_start(out=convw_t[:, k, :], in_=conv_w[k].rearrange("(c p) -> p c", p=128),
                                                       allow_slow_non_contiguous=True), w=[("convw", k)], dma=True)
            S.add("sp", lambda e: e.dma_start(out=convb_t, in_=conv_b.rearrange("(c p) -> p c", p=128),
                                              allow_slow_non_contiguous=True), w=["convb"], dma=True)
            S.add("pool", lambda e: e.dma_start(out=wdt, in_=w_in[:, C_DT:C_DT + 32].rearrange("(c p) n -> p c n", p=128)),
                  w=["wdt"], dma=True)
            S.add("sp", lambda e: e.dma_start(out=dtb_b, in_=dt_bias.partition_broadcast(128)), w=["dtb"], dma=True)
            S.add("sp", lambda e: e.dma_start(out=negA_b, in_=a_log.partition_broadcast(128)), w=["negA"], dma=True)
            S.add("sp", lambda e: e.dma_start(out=dskip_b, in_=d_skip.partition_broadcast(128)), w=["dskip"], dma=True)
            S.add("act", lambda e: e.activation(negA_b, negA_b, AF.Exp), r=["negA"], w=["negA"])
            S.add("dve", lambda e: e.tensor_scalar(negA_b, negA_b, -1.0, None, ALU.mult), r=["negA"], w=["negA"])
            S.add("pool", lambda e: e.memset(halo, 0.0), w=["halo"])
            S.add("pool", lambda e: e.memset(state, 0.0), w=["state"])
            S.add("pool", lambda e: e.memset(prevb, 0.0), w=["prevb"])

            own_flag = [False]

            def SY(*a_, **k_):
                if own_flag[0]:
                    S.add(*a_, **k_)

            for G in range(NG):
                for cc in range(4):
                    c = 4 * G + cc
                    S.add("sp", lambda e, c=c: e.dma_start(out=xt, in_=x_all[c * 128:(c + 1) * 128, :]), w=["xt"], dma=True)
                    rmsnorm_rows(xt, "xt", 0, hb, "hb", ss[:, 0:1], "ss0", junkB2, "junkB2")
                    transpose_rows(hb, "hb", hT, lambda half, cc=cc: ("hT", cc, half), cc * 128)
                hTkeys = [("hT", cc, half) for cc in range(4) for half in range(2)]
                for jb in range(8):
                    col0 = (C_Z + jb * 512) if jb < 4 else (C_V + (jb - 4) * 512)
                    wb, wk = wload(w_in[:, col0:col0 + 512])
                    for cc in ((3,) if jb < 4 else range(4)):
                        c = 4 * G + cc
                        pg, pk = bank()
                        for fc in range(16):
                            S.add("pe", lambda e, pg=pg, fc=fc, cc=cc, wb=wb: e.matmul(
                                pg, hT[:, fc, cc * 128:(cc + 1) * 128], wb[:, fc, :], start=(fc == 0), stop=(fc == 15)),
                                r=[("hT", cc, fc // 8), wk], w=[pk])
                        if jb < 4:
                            S.add("act", lambda e, pg=pg, cc=cc, jb=jb: e.activation(
                                zs[:, cc, jb * 512:(jb + 1) * 512], pg, AF.Silu), r=[pk], w=[("zs", cc, jb)])
                        else:
                            vi = cnt.setdefault("vst", 0) % 2
                            cnt["vst"] += 1
                            S.add("act", lambda e, pg=pg, vi=vi: e.copy(vst[vi], pg), r=[pk], w=[f"vst{vi}"])
                            S.add("sp", lambda e, vi=vi, c=c, jb=jb: e.dma_start(
                                out=v_d[(jb - 4) * 4:(jb - 3) * 4, :, c, :].rearrange("h p d -> p h d"),
                                in_=vst[vi].rearrange("p (h d) -> p h d", h=4)),
                                r=[f"vst{vi}"], w=[("v_d", c)], dma=True)
                for cc in range(4):
                    pg, pk = bank()
                    for fc in range(16):
                        S.add("pe", lambda e, pg=pg, fc=fc, cc=cc: e.matmul(
                            pg[:, 0:32], hT[:, fc, cc * 128:(cc + 1) * 128], wdt[:, fc, :], start=(fc == 0), stop=(fc == 15)),
                            r=[("hT", cc, fc // 8), "wdt"], w=[pk])
                    S.add("dve", lambda e, pg=pg: e.tensor_tensor(dtr, pg[:, 0:32], dtb_b, ALU.add), r=[pk, "dtb"], w=["dtr"])
                    S.add("act", lambda e: e.activation(dtr, dtr, AF.Exp), r=["dtr"], w=["dtr"])
                    S.add("act", lambda e, cc=cc: e.activation(dt_all[:, cc, :], dtr, AF.Ln, bias=1.0), r=["dtr"], w=[("dt", cc)])
                    S.add("dve", lambda e, cc=cc: e.tensor_tensor(a_all[:, cc, :], dt_all[:, cc, :], negA_b, ALU.mult),
                          r=[("dt", cc), "negA"], w=[("a", cc)])
                for piece in range(10):
                    col0 = (C_XBC + piece * 512) if piece < 6 else (C_K + (piece - 6) * 512)
                    wb, wk = wload(w_in[:, col0:col0 + 512])
                    for sub in range(4):
                        i = piece * 4 + sub
                        pg, pk = bank()
                        for fc in range(16):
                            S.add("pe", lambda e, pg=pg, fc=fc, sub=sub, wb=wb: e.matmul(
                                pg, wb[:, fc, sub * 128:(sub + 1) * 128], hT[:, fc, :], start=(fc == 0), stop=(fc == 15)),
                                r=hTkeys + [wk], w=[pk])
                        if i < 24:
                            ri = i % 2
                            rw, ct, xc = raw[ri], ctmp[ri], xcb[ri]
                            S.add("act", lambda e, pg=pg, rw=rw: e.copy(rw[:, 3:515], pg), r=[pk], w=[f"raw{ri}"])
                            S.add("act", lambda e, rw=rw, i=i: e.copy(rw[:, 0:3], halo[:, i, :]),
                                  r=[("halo", i)], w=[f"rawh{ri}"])
                            rk = [f"raw{ri}", f"rawh{ri}"] + [("convw", k) for k in range(4)]
                            S.add("dve", lambda e, rw=rw, ct=ct, i=i: e.tensor_scalar(
                                ct, rw[:, 0:512], convw_t[:, 0, i:i + 1], None, ALU.mult), r=rk, w=[f"ct{ri}"])
                            for k in (1, 2, 3):
                                S.add("dve", lambda e, rw=rw, ct=ct, i=i, k=k: e.scalar_tensor_tensor(
                                    ct, rw[:, k:512 + k], convw_t[:, k, i:i + 1], ct, ALU.mult, ALU.add),
                                    r=rk + [f"ct{ri}"], w=[f"ct{ri}"])
                            S.add("act", lambda e, rw=rw, i=i: e.copy(halo[:, i, :], rw[:, 512:515]),
                                  r=[f"raw{ri}"], w=[("halo", i)])
                            if i < 16:
                                S.add("act", lambda e, ct=ct, xc=xc, i=i: e.activation(xc, ct, AF.Silu, bias=convb_t[:, i:i + 1]),
                                      r=[f"ct{ri}", "convb"], w=[f"xc{ri}"])
                                src, sk = xc, f"xc{ri}"
                            elif i < 20:
                                g = i - 16
                                S.add("act", lambda e, ct=ct, g=g, i=i: e.activation(BT[:, g, :], ct, AF.Silu, bias=convb_t[:, i:i + 1]),
                                      r=[f"ct{ri}", "convb"], w=[("BT", g)])
                                src, sk = BT[:, g, :], ("BT", g)
                            else:
                                g = i - 20
                                S.add("act", lambda e, ct=ct, g=g, i=i: e.activation(CT[:, g, :], ct, AF.Silu, bias=convb_t[:, i:i + 1]),
                                      r=[f"ct{ri}", "convb"], w=[("CT", g)])
                                src = None
                            if src is not None:
                                pt, ptk = tbank()
                                for cc in range(4):
                                    S.add("pe", lambda e, pt=pt, cc=cc, src=src: e.transpose(
                                        pt[:, cc * 128:(cc + 1) * 128], src[:, cc * 128:(cc + 1) * 128], ident),
                                        r=[sk, "ident"], w=[ptk])
                                if i < 16:
                                    S.add("act", lambda e, pt=pt, i=i: e.copy(
                                        xtm[:, :, i * 128:(i + 1) * 128], pt[:, 0:512].rearrange("p (a b) -> p a b", a=4)),
                                        r=[ptk], w=[("xtm", i)])
                                else:
                                    S.add("act", lambda e, pt=pt, g=g: e.copy(
                                        Btm[:, :, g * 128:(g + 1) * 128], pt[:, 0:512].rearrange("p (a b) -> p a b", a=4)),
                                        r=[ptk], w=[("Btm", g)])
                        else:
                            hd = i - 24
                            ki = hd % 2
                            S.add("act", lambda e, pg=pg, ki=ki: e.copy(kst[ki], pg), r=[pk], w=[f"kst{ki}"])
                            S.add("sp", lambda e, ki=ki, hd=hd, G=G: e.dma_start(
                                out=kT_d[hd, :, G * 512:(G + 1) * 512], in_=kst[ki]),
                                r=[f"kst{ki}"], w=[("kT_d", hd)], dma=True)
                xtm_keys = [("xtm", i) for i in range(16)]
                for cc in range(4):
                    own_flag[0] = (cc == 3)
                    pg, pk = bank()
                    S.add("pe", lambda e, pg=pg, cc=cc: e.matmul(pg[:, 0:32], UI, a_all[:, cc, :], start=True, stop=True),
                          r=[("a", cc), "UI"], w=[pk])
                    S.add("pe", lambda e, pg=pg, cc=cc: e.matmul(pg[:, 32:64], ones32, a_all[:, cc, :], start=True, stop=True),
                          r=[("a", cc), "ones32"], w=[pk])
                    S.add("act", lambda e, pg=pg: e.copy(acs, pg[:, 0:64]), r=[pk], w=["acs"])
                    SY("act", lambda e: e.activation(eacs, acs[:, 0:32], AF.Exp), r=["acs"], w=["eacs"])
                    S.add("act", lambda e: e.activation(cdec, acs[:, 32:64], AF.Exp), r=["acs"], w=["cdec"])
                    S.add("dve", lambda e: e.tensor_tensor(dst, acs[:, 32:64], acs[:, 0:32], ALU.subtract), r=["acs"], w=["dst"])
                    S.add("act", lambda e: e.activation(dst, dst, AF.Exp), r=["dst"], w=["dst"])
                    if G == 0 and cc < 3:
                        S.add("dve", lambda e, cc=cc: e.tensor_scalar(dst, dst, msel_t[:, cc:cc + 1], None, ALU.mult),
                              r=["dst", "msel"], w=["dst"])
                    x3 = xtm[:, cc, :].rearrange("p (h d) -> p h d", h=32)
                    S.add("dve", lambda e, cc=cc, x3=x3: e.tensor_tensor(
                        Xdt.rearrange("p (h d) -> p h d", h=32), x3,
                        dt_all[:, cc, :].unsqueeze(2).to_broadcast([128, 32, 64]), ALU.mult),
                        r=xtm_keys + [("dt", cc)], w=["Xdt"])
                    S.add("dve", lambda e: e.tensor_tensor(
                        Xds.rearrange("p (h d) -> p h d", h=32), Xdt.rearrange("p (h d) -> p h d", h=32),
                        dst.unsqueeze(2).to_broadcast([128, 32, 64]), ALU.mult), r=["Xdt", "dst"], w=["Xds"])
                    pg, pk = bank()
                    for g in range(4):
                        SY("pe", lambda e, pg=pg, g=g, cc=cc: e.matmul(
                            pg[:, g * 128:(g + 1) * 128], BT[:, g, cc * 128:(cc + 1) * 128], CT[:, g, cc * 128:(cc + 1) * 128],
                            start=True, stop=True), r=[("BT", g), ("CT", g)], w=[pk])
                    SY("dve", lambda e, pg=pg: e.tensor_tensor(
                        CBm, pg.rearrange("p (g l) -> p g l", g=4), UI.unsqueeze(1).to_broadcast([128, 4, 128]), ALU.mult),
                        r=[pk, "UI"], w=["CBm"])
                    for g in range(4):
                        gi = g % 2
                        SY("dve", lambda e, g=g, gi=gi, cc=cc: e.tensor_tensor(
                            lh[gi], SL.unsqueeze(1).to_broadcast([128, 8, 128]),
                            a_all[:, cc, g * 8:(g + 1) * 8].unsqueeze(2).to_broadcast([128, 8, 128]), ALU.mult),
                            r=["SL", ("a", cc)], w=[f"lh{gi}"])
                        pY, pYk = bank()
                        for half in range(2):
                            pseg, psk = bank()
                            for j in range(4):
                                SY("pe", lambda e, pseg=pseg, j=j, gi=gi, half=half: e.matmul(
                                    pseg[:, j * 128:(j + 1) * 128], lh[gi][:, half * 4 + j, :], UI, start=True, stop=True),
                                    r=[f"lh{gi}", "UI"], w=[psk])
                            SY("act", lambda e, pseg=pseg, half=half: e.activation(
                                dec[half], pseg.rearrange("p (j l) -> p j l", j=4), AF.Exp), r=[psk], w=[f"dec{half}"])
                            SY("dve", lambda e, half=half, g=g: e.tensor_tensor(
                                Mb[half], dec[half], CBm[:, g:g + 1, :].to_broadcast([128, 4, 128]), ALU.mult),
                                r=[f"dec{half}", "CBm"], w=[f"Mb{half}"])
                            for j in range(4):
                                h = g * 8 + half * 4 + j
                                SY("pe", lambda e, pY=pY, half=half, j=j, h=h: e.matmul(
                                    pY[:, (half * 4 + j) * 64:(half * 4 + j + 1) * 64], Mb[half][:, j, :],
                                    Xdt[:, h * 64:(h + 1) * 64], start=True, stop=True),
                                    r=[f"Mb{half}", "Xdt"], w=[pYk])
                        pYo, pYok = bank()
                        SY("pe", lambda e, pYo=pYo, g=g, cc=cc: e.matmul(
                            pYo, CT[:, g, cc * 128:(cc + 1) * 128], prevb[:, g * 512:(g + 1) * 512], start=True, stop=True),
                            r=[("CT", g), ("prevb", g)], w=[pYok])
                        SY("act", lambda e, pYo=pYo, gi=gi: e.copy(yo[gi], pYo), r=[pYok], w=[f"yo{gi}"])
                        SY("dve", lambda e, gi=gi, g=g: e.tensor_tensor(
                            yo[gi].rearrange("p (h d) -> p h d", h=8), yo[gi].rearrange("p (h d) -> p h d", h=8),
                            eacs[:, g * 8:(g + 1) * 8].unsqueeze(2).to_broadcast([128, 8, 64]), ALU.mult),
                            r=[f"yo{gi}", "eacs"], w=[f"yo{gi}"])
                        SY("dve", lambda e, gi=gi, pY=pY: e.tensor_tensor(yy[gi], pY, yo[gi], ALU.add),
                              r=[pYk, f"yo{gi}"], w=[f"yy{gi}"])
                        SY("dve", lambda e, gi=gi, g=g, cc=cc: e.tensor_tensor(
                            yo[gi].rearrange("p (h d) -> p h d", h=8),
                            xtm[:, cc, g * 512:(g + 1) * 512].rearrange("p (h d) -> p h d", h=8),
                            dskip_b[:, g * 8:(g + 1) * 8].unsqueeze(2).to_broadcast([128, 8, 64]), ALU.mult),
                            r=xtm_keys + ["dskip", f"yy{gi}"], w=[f"yo{gi}"])
                        SY("dve", lambda e, gi=gi: e.tensor_tensor(yy[gi], yy[gi], yo[gi], ALU.add),
                              r=[f"yy{gi}", f"yo{gi}"], w=[f"yy{gi}"])
                        SY("dve", lambda e, gi=gi, g=g, cc=cc: e.tensor_tensor(
                            yy[gi], yy[gi], zs[:, cc, g * 512:(g + 1) * 512], ALU.mult),
                            r=[f"yy{gi}", ("zs", cc, g)], w=[f"yy{gi}"])
                        SY("act", lambda e, gi=gi, g=g: e.activation(yo[gi], yy[gi], AF.Square, accum_out=ssg[:, g:g + 1]),
                              r=[f"yy{gi}"], w=[f"yo{gi}", ("ssg", g)])
                        SY("act", lambda e, g=g: e.activation(rm[:, g:g + 1], ssg[:, g:g + 1], AF.Ln, bias=EPS, scale=1.0 / 512),
                              r=[("ssg", g)], w=[("rm", g)])
                        SY("act", lambda e, g=g: e.activation(rm[:, g:g + 1], rm[:, g:g + 1], AF.Exp, scale=-0.5),
                              r=[("rm", g)], w=[("rm", g)])
                        SY("dve", lambda e, gi=gi, g=g: e.tensor_tensor(
                            yy[gi], yy[gi], gain[1][:, g * 512:(g + 1) * 512], ALU.mult),
                            r=[f"yy{gi}", "gain1"], w=[f"yy{gi}"])
                        dstv = ya_cur[:, g * 512:(g + 1) * 512]
                        if True:
                            SY("dve", lambda e, dstv=dstv, g=g, gi=gi: e.tensor_scalar(
                                dstv, yy[gi], rm[:, g:g + 1], None, ALU.mult),
                                r=[f"yy{gi}", ("rm", g)], w=[("ya", g)])
                        else:
                            SY("dve", lambda e, dstv=dstv, g=g, gi=gi: e.scalar_tensor_tensor(
                                dstv, yy[gi], rm[:, g:g + 1], dstv, ALU.mult, ALU.add),
                                r=[f"yy{gi}", ("rm", g), ("ya", g)], w=[("ya", g)])
                        pSt, pStk = bank()
                        S.add("pe", lambda e, pSt=pSt, g=g, cc=cc: e.matmul(
                            pSt, Btm[:, cc, g * 128:(g + 1) * 128], Xds[:, g * 512:(g + 1) * 512], start=True, stop=True),
                            r=[("Btm", g), "Xds"], w=[pStk])
                        sv = state[:, g * 512:(g + 1) * 512]
                        S.add("dve", lambda e, sv=sv, g=g: e.tensor_tensor(
                            sv.rearrange("p (h d) -> p h d", h=8), sv.rearrange("p (h d) -> p h d", h=8),
                            cdec[:, g * 8:(g + 1) * 8].unsqueeze(2).to_broadcast([128, 8, 64]), ALU.mult),
                            r=[("state", g), "cdec"], w=[("state", g)])
                        S.add("dve", lambda e, sv=sv, pSt=pSt: e.tensor_tensor(sv, sv, pSt, ALU.add),
                              r=[("state", g), pStk], w=[("state", g)])
                        S.add("act", lambda e, sv=sv, g=g: e.copy(prevb[:, g * 512:(g + 1) * 512], sv),
                              r=[("state", g)], w=[("prevb", g)])
                S.add("sp", lambda e, G=G: e.dma_start(out=ya_d[G], in_=ya_cur),
                      r=[("ya", g) for g in range(4)], w=[("ya_d", G)], dma=True)
                if dbg:
                    S.add("pool", lambda e, G=G: e.dma_start(out=dbg_t["ya"][G], in_=ya_cur),
                          r=[("ya", g) for g in range(4)], dma=True)
                    out_dmas.append(len(S.ops) - 1)


        NJ = NG
        NT = NJ * 128
        R1 = WB_END

        def at(off_kb, dt, shape):
            AR.off = R1 + off_kb * 1024
            return AR.alloc(dt, shape)

        S.barrier()
        hTo = at(0, BF16, [16, 1024])
        qT = at(32, BF16, [16, 1024])
        ybT = at(64, BF16, [16, 1024])
        kTh = [at(96 + 8 * i, BF16, [128, SEQ]) for i in range(2)]
        vh = [at(112 + 8 * i, BF16, [32, 128]) for i in range(2)]
        xo = at(64, F32, [128, D])
        hbo = at(72, BF16, [128, D])
        junkB = at(76, BF16, [128, D])
        AR.off = R1 + 128 * 1024
        ssb = AR.alloc(F32, [128, 8])
        Et = [AR.alloc(F32, [128, 512]) for _ in range(2)]
        spb = [AR.alloc(BF16, [128, 512]) for _ in range(2)]
        expc = [AR.alloc(F32, [128, 512]) for _ in range(2)]
        wbf = [AR.alloc(BF16, [128, 512]) for _ in range(2)]
        Trow = [AR.alloc(BF16, [128, 512]) for _ in range(2)]

        def own_hT(gslot, src_tiles_fn, dstT, keyname):
            for j in range(NJ):
                src_tiles_fn(j)
                rmsnorm_rows(xo, "xo", gslot, hbo, "hbo", ssb[:, 0:1], "ssb0", junkB, "junkB")
                transpose_rows(hbo, "hbo", dstT, lambda half, j=j: (keyname, j, half), j * 128)

        if "B" in phases:
            load_gain(0, g_mix)
            own_hT(0, lambda j: S.add("sp", lambda e: e.dma_start(out=xo, in_=x_own[j * 128:(j + 1) * 128, :]),
                                      w=["xo"], dma=True), hTo, "hTo")
            hTo_keys = [("hTo", j, half) for j in range(NJ) for half in range(2)]
            NTB = (NT + 511) // 512
            for piece in range(4):
                wb, wk = wload(w_in[:, C_Q + piece * 512:C_Q + (piece + 1) * 512])
                for sub in range(4):
                    hd = piece * 4 + sub
                    for tb in range(NTB):
                        n = min(512, NT - tb * 512)
                        pg, pk = bank()
                        for fc in range(16):
                            S.add("pe", lambda e, pg=pg, fc=fc, sub=sub, wb=wb, tb=tb, n=n: e.matmul(
                                pg[:, 0:n], wb[:, fc, sub * 128:(sub + 1) * 128], hTo[:, fc, tb * 512:tb * 512 + n],
                                start=(fc == 0), stop=(fc == 15)), r=hTo_keys + [wk], w=[pk])
                        S.add("act", lambda e, pg=pg, hd=hd, tb=tb, n=n: e.mul(
                            qT[:, hd, tb * 512:tb * 512 + n], pg[:, 0:n], float(128 ** -0.5)), r=[pk], w=[("qT", hd)])

            if dbg:
                S.add("pool", lambda e: e.dma_start(out=dbg_t["q"][:, :, 0:NT], in_=qT[:, :, 0:NT]),
                      r=[("qT", hd) for hd in range(16)], dma=True)
            NKB = 4 * NJ
            cnt["npg"] = 4
            for hp in range(8):
                for ci in range(2):
                    hd = hp * 2 + ci
                    S.add("sp", lambda e, hd=hd, ci=ci: e.dma_start(out=kTh[ci][:, 0:NKB * 128], in_=kT_d[hd, :, 0:NKB * 128]),
                          r=[("kT_d", hd)], w=[f"kTh{ci}"], dma=True)
                    S.add("sp", lambda e, hd=hd, ci=ci: e.dma_start(
                        out=vh[ci][:, 0:NKB, :], in_=v_d[hd, :, 0:NKB, :]),
                        r=[("v_d", c) for c in range(NKB)], w=[f"vh{ci}"], dma=True)
                for Q in range((NJ + 3) // 4):
                    j0 = 4 * Q
                    W = min(4, NJ - j0)
                    WN = W * 128
                    pys = []
                    for ci in range(2):
                        S.add("dve", lambda e, ci=ci: e.memset(Trow[ci], 0.0), w=[f"Trow{ci}"])
                        pys.append((pgs[4 + ci], f"pg{4 + ci}"))
                        S.add("pe", lambda e, ci=ci, WN=WN: e.matmul(pgs[4 + ci][:, 0:WN], onesb[0:1, :], Trow[ci][0:1, 0:WN], start=True, stop=False),
                              r=["onesb", f"Trow{ci}"], w=[f"pg{4 + ci}"])
                    kmax = 4 * (j0 + W - 1) + 3
                    for kb in range(kmax, -1, -1):
                        jmin = max(j0, (kb - 3 + 3) // 4)
                        c0 = (jmin - j0) * 128
                        jm = kb // 4
                        pzs = []
                        for ci in range(2):
                            hd = hp * 2 + ci
                            pz, pzk = bank()
                            pzs.append((pz, pzk))
                            S.add("pe", lambda e, pz=pz, ci=ci, hd=hd, kb=kb, c0=c0, WN=WN, j0=j0: e.matmul(
                                pz[:, c0:WN], kTh[ci][:, kb * 128:(kb + 1) * 128], qT[:, hd, j0 * 128 + c0:j0 * 128 + WN],
                                start=True, stop=True), r=[f"kTh{ci}", ("qT", hd)], w=[pzk])
                            S.add("act", lambda e, pz=pz, ci=ci, c0=c0, WN=WN: e.activation(Et[ci][:, c0:WN], pz[:, c0:WN], AF.Exp),
                                  r=[pzk], w=[f"E{ci}"])
                            if kb % 4 == 3 and j0 <= jm < j0 + W:
                                cm = (jm - j0) * 128
                                S.add("dve", lambda e, ci=ci, cm=cm: e.tensor_tensor(
                                    Et[ci][:, cm:cm + 128], Et[ci][:, cm:cm + 128], amask_t[:, 3, :], ALU.mult),
                                    r=[f"E{ci}", "amask"], w=[f"E{ci}"])
                            if kb < 3:
                                S.add("dve", lambda e, ci=ci, c0=c0, WN=WN, kb=kb: e.tensor_scalar(
                                    Et[ci][:, c0:WN], Et[ci][:, c0:WN], msel_t[:, kb:kb + 1], None, ALU.mult),
                                    r=[f"E{ci}", "msel"], w=[f"E{ci}"])
                            S.add("act", lambda e, ci=ci, c0=c0, WN=WN: e.activation(spb[ci][:, c0:WN], Et[ci][:, c0:WN], AF.Ln, bias=1.0),
                                  r=[f"E{ci}"], w=[f"sp{ci}"])
                        pcs = []
                        for ci in range(2):
                            pc, pck = bank()
                            pcs.append((pc, pck))
                            S.add("pe", lambda e, pc=pc, ci=ci, c0=c0, WN=WN: e.matmul(
                                pc[:, c0:WN], UIb, spb[ci][:, c0:WN], start=True, stop=False), r=["UIb", f"sp{ci}"], w=[pck])
                            S.add("pe", lambda e, pc=pc, ci=ci, c0=c0, WN=WN: e.matmul(
                                pc[:, c0:WN], onesb[0:1, :], Trow[ci][0:1, c0:WN], start=False, stop=True), r=["onesb", f"Trow{ci}"], w=[pck])
                            S.add("act", lambda e, pc=pc, ci=ci, c0=c0, WN=WN: e.copy(Trow[ci][0:1, c0:WN], pc[0:1, c0:WN]),
                                  r=[pck], w=[f"Trow{ci}"])
                            S.add("act", lambda e, pc=pc, ci=ci, c0=c0, WN=WN: e.activation(expc[ci][:, c0:WN], pc[:, c0:WN], AF.Exp, scale=-1.0),
                                  r=[pck], w=[f"expc{ci}"])
                            S.add("dve", lambda e, ci=ci, c0=c0, WN=WN: e.tensor_tensor(
                                wbf[ci][:, c0:WN], Et[ci][:, c0:WN], expc[ci][:, c0:WN], ALU.mult),
                                r=[f"E{ci}", f"expc{ci}"], w=[f"w{ci}"])
                        for ci in range(2):
                            py, pyk = pys[ci]
                            newj = (kb - 3) // 4 if (kb - 3) % 4 == 0 and j0 <= (kb - 3) // 4 < j0 + W else None
                            last = (kb == 0)
                            if newj is not None:
                                cn = (newj - j0) * 128
                                S.add("pe", lambda e, py=py, ci=ci, kb=kb, cn=cn, last=last: e.matmul(
                                    py[:, cn:cn + 128], vh[ci][:, kb, :], wbf[ci][:, cn:cn + 128], start=False, stop=last),
                                    r=[f"vh{ci}", f"w{ci}"], w=[pyk])
                                c1 = cn + 128
                            else:
                                c1 = c0
                            if c1 < WN:
                                S.add("pe", lambda e, py=py, ci=ci, kb=kb, c1=c1, WN=WN, last=last: e.matmul(
                                    py[:, c1:WN], vh[ci][:, kb, :], wbf[ci][:, c1:WN], start=False, stop=last),
                                    r=[f"vh{ci}", f"w{ci}"], w=[pyk])
                    for ci in range(2):
                        hd = hp * 2 + ci
                        py, pyk = pys[ci]
                        S.add("act", lambda e, py=py, hd=hd, WN=WN, j0=j0: e.copy(ybT[:, hd, j0 * 128:j0 * 128 + WN], py[:, 0:WN]),
                              r=[pyk], w=[("ybT", hd)])
            cnt["npg"] = 6
            if dbg:
                S.add("pool", lambda e: e.dma_start(out=dbg_t["yb"][:, :, 0:NT], in_=ybT[:, :, 0:NT]),
                      r=[("ybT", hd) for hd in range(16)], dma=True)

        S.barrier()
        yaT = at(32, BF16, [16, 1024])
        mT = at(96, BF16, [16, 1024])
        AR.off = R1 + 128 * 1024
        yab = AR.alloc(BF16, [128, D])
        sg = [AR.alloc(F32, [128, 512]) for _ in range(2)]
        tt = [AR.alloc(F32, [128, 512]) for _ in range(2)]
        NTB = (NT + 511) // 512
        if "C" in phases:
            for j in range(NJ):
                S.add("sp", lambda e, j=j: e.dma_start(out=yab, in_=ya_d[j]), r=[("ya_d", j)], w=["yab"], dma=True)
                transpose_rows(yab, "yab", yaT, lambda half, j=j: ("yaT", j, half), j * 128)
            yaT_keys = [("yaT", j, half) for j in range(NJ) for half in range(2)]
            hTo_keys = [("hTo", j, half) for j in range(NJ) for half in range(2)]
            ybT_keys = [("ybT", hd) for hd in range(16)]
            srcs = [(w_branch_a, 0, yaT, yaT_keys), (w_branch_b, 0, ybT, ybT_keys),
                    (w_in, C_GA, hTo, hTo_keys), (w_in, C_GB, hTo, hTo_keys)]
            for piece in range(4):
                for tb in range(NTB):
                    n = min(512, NT - tb * 512)
                    for sub in range(4):
                        pass
                for pair in range(2):
                    res = []
                    wa = wload(srcs[pair][0][:, srcs[pair][1] + piece * 512:srcs[pair][1] + (piece + 1) * 512])
                    wg = wload(srcs[2 + pair][0][:, srcs[2 + pair][1] + piece * 512:srcs[2 + pair][1] + (piece + 1) * 512])
                    for sub in range(4):
                        nb = piece * 4 + sub
                        for tb in range(NTB):
                            n = min(512, NT - tb * 512)
                            pa, pak = bank()
                            pgt, pgk = bank()
                            act_in, act_keys = srcs[pair][2], srcs[pair][3]
                            for fc in range(16):
                                S.add("pe", lambda e, pa=pa, fc=fc, sub=sub, tb=tb, n=n, wa=wa, act_in=act_in: e.matmul(
                                    pa[:, 0:n], wa[0][:, fc, sub * 128:(sub + 1) * 128], act_in[:, fc, tb * 512:tb * 512 + n],
                                    start=(fc == 0), stop=(fc == 15)), r=act_keys + [wa[1]], w=[pak])
                            for fc in range(16):
                                S.add("pe", lambda e, pgt=pgt, fc=fc, sub=sub, tb=tb, n=n, wg=wg: e.matmul(
                                    pgt[:, 0:n], wg[0][:, fc, sub * 128:(sub + 1) * 128], hTo[:, fc, tb * 512:tb * 512 + n],
                                    start=(fc == 0), stop=(fc == 15)), r=hTo_keys + [wg[1]], w=[pgk])
                            si = cnt.setdefault("sg", 0) % 2
                            cnt["sg"] += 1
                            S.add("act", lambda e, pgt=pgt, si=si, n=n: e.activation(sg[si][:, 0:n], pgt[:, 0:n], AF.Sigmoid),
                                  r=[pgk], w=[f"sg{si}"])
                            mv = mT[:, nb, tb * 512:tb * 512 + n]
                            if pair == 0:
                                S.add("dve", lambda e, pa=pa, si=si, n=n, mv=mv: e.tensor_tensor(mv, pa[:, 0:n], sg[si][:, 0:n], ALU.mult),
                                      r=[pak, f"sg{si}"], w=[("mT", nb, tb)])
                            else:
                                S.add("dve", lambda e, pa=pa, si=si, n=n: e.tensor_tensor(tt[si][:, 0:n], pa[:, 0:n], sg[si][:, 0:n], ALU.mult),
                                      r=[pak, f"sg{si}"], w=[f"tt{si}"])
                                S.add("dve", lambda e, si=si, n=n, mv=mv: e.tensor_tensor(mv, mv, tt[si][:, 0:n], ALU.add),
                                      r=[f"tt{si}", ("mT", nb, tb)], w=[("mT", nb, tb)])
        S.barrier()
        x1 = at(0, F32, [8, D])
        if "C" in phases:
            mT_keys = [("mT", nb, tb) for nb in range(16) for tb in range(NTB)]
            for j in range(NJ):
                S.add("sp", lambda e, j=j: e.dma_start(out=x1[:, j, :], in_=x_own[j * 128:(j + 1) * 128, :]),
                      w=[("x1", j)], dma=True)
            for fb in range(4):
                wb, wk = wload(w_out[:, fb * 512:(fb + 1) * 512])
                for j in range(NJ):
                    pg, pk = bank()
                    for fc in range(16):
                        S.add("pe", lambda e, pg=pg, fc=fc, j=j, wb=wb: e.matmul(
                            pg, mT[:, fc, j * 128:(j + 1) * 128], wb[:, fc, :], start=(fc == 0), stop=(fc == 15)),
                            r=mT_keys + [wk], w=[pk])
                    xv = x1[:, j, fb * 512:(fb + 1) * 512]
                    S.add("dve", lambda e, pg=pg, xv=xv: e.tensor_tensor(xv, xv, pg, ALU.add), r=[pk, ("x1", j)], w=[("x1", j)])
            if dbg:
                for j in range(NJ):
                    S.add("sp", lambda e, j=j: e.dma_start(out=dbg_t["x1"][j * 128:(j + 1) * 128, :], in_=x1[:, j, :]),
                          r=[("x1", j)], dma=True)


        S.barrier()
        h2 = at(64, BF16, [8, D])
        h2T = at(96, BF16, [16, 1024])
        AR.off = R1 + 140 * 1024
        lg = AR.alloc(F32, [8, 32])
        top8 = AR.alloc(F32, [8, 8])
        mask = AR.alloc(F32, [8, 32])
        maskb = AR.alloc(BF16, [8, 32])
        Gt = AR.alloc(F32, [8, 32])
        pos = AR.alloc(F32, [8, 32])
        sm = AR.alloc(F32, [8, 4])
        wrb = AR.alloc(BF16, [16, 32])
        brb = AR.alloc(BF16, [128, 32])
        D_SMALL_END = AR.off
        posT = gain[1][:, 0:1024]
        GT = gain[1][:, 1024:2048]
        if "D" in phases:
            load_gain(0, g_ffn)
            S.add("pool", lambda e: e.dma_start(out=wrb, in_=w_router.rearrange("(c p) n -> p c n", p=128)), w=["wrb"], dma=True)
            S.add("pool", lambda e: e.dma_start(out=brb[0:1, :], in_=b_router.unsqueeze(0)), w=["brb"], dma=True)
            for j in range(NJ):
                xj = x1[:, j, :]
                S.add("act", lambda e, xj=xj, j=j: e.activation(junkB2, xj, AF.Square, accum_out=sm[:, j, 0:1]),
                      r=[("x1", j)], w=["junkB2", ("sm", j)])
                S.add("act", lambda e, j=j: e.activation(sm[:, j, 0:1], sm[:, j, 0:1], AF.Ln, bias=EPS, scale=1.0 / D), r=[("sm", j)], w=[("sm", j)])
                S.add("act", lambda e, j=j: e.activation(sm[:, j, 0:1], sm[:, j, 0:1], AF.Exp, scale=-0.5), r=[("sm", j)], w=[("sm", j)])
                S.add("dve", lambda e, xj=xj, j=j: e.scalar_tensor_tensor(h2[:, j, :], xj, sm[:, j, 0:1], gain[0], ALU.mult, ALU.mult),
                      r=[("x1", j), ("sm", j), "gain0"], w=[("h2", j)])
                transpose_rows(h2[:, j, :], ("h2", j), h2T, lambda half, j=j: ("h2T", j, half), j * 128)
            for j in range(NJ):
                pg, pk = bank()
                for fc in range(16):
                    S.add("pe", lambda e, pg=pg, fc=fc, j=j: e.matmul(pg[:, 0:32], h2T[:, fc, j * 128:(j + 1) * 128], wrb[:, fc, :],
                                                                  start=(fc == 0), stop=False),
                          r=[("h2T", j, fc // 8), "wrb"], w=[pk])
                S.add("pe", lambda e, pg=pg: e.matmul(pg[:, 0:32], onesb[0:1, :], brb[0:1, :], start=False, stop=True),
                      r=["onesb", "brb"], w=[pk])
                S.add("act", lambda e, pg=pg, j=j: e.copy(lg[:, j, :], pg[:, 0:32]), r=[pk], w=[("lg", j)])
                S.add("dve", lambda e, j=j: e.max(out=top8[:, j, :], in_=lg[:, j, :]), r=[("lg", j)], w=[("top8", j)])
                S.add("dve", lambda e, j=j: e.tensor_scalar(mask[:, j, :], lg[:, j, :], top8[:, j, 3:4], None, ALU.is_ge),
                      r=[("lg", j), ("top8", j)], w=[("mask", j)])
                S.add("dve", lambda e, j=j: e.tensor_scalar(sm[:, j, 1:2], top8[:, j, 0:1], -1.0, None, ALU.mult),
                      r=[("top8", j)], w=[("smb", j)])
                S.add("act", lambda e, j=j: e.activation(Gt[:, j, :], lg[:, j, :], AF.Exp, bias=sm[:, j, 1:2]),
                      r=[("lg", j), ("smb", j)], w=[("Gt", j)])
                S.add("dve", lambda e, j=j: e.tensor_tensor(Gt[:, j, :], Gt[:, j, :], mask[:, j, :], ALU.mult),
                      r=[("Gt", j), ("mask", j)], w=[("Gt", j)])
                S.add("dve", lambda e, j=j: e.reduce_sum(sm[:, j, 2:3], Gt[:, j, :], axis=AX.X), r=[("Gt", j)], w=[("sms", j)])
                S.add("dve", lambda e, j=j: e.reciprocal(sm[:, j, 2:3], sm[:, j, 2:3]), r=[("sms", j)], w=[("sms", j)])
                S.add("dve", lambda e, j=j: e.tensor_scalar(Gt[:, j, :], Gt[:, j, :], sm[:, j, 2:3], None, ALU.mult),
                      r=[("Gt", j), ("sms", j)], w=[("Gt", j)])
                S.add("dve", lambda e, j=j: e.tensor_copy(maskb[:, j, :], mask[:, j, :]), r=[("mask", j)], w=[("maskb", j)])
            for j in range(NJ):
                pg, pk = bank()
                for j2 in range(j):
                    S.add("pe", lambda e, pg=pg, j2=j2: e.matmul(pg[:, 0:32], onesb, maskb[:, j2, :], start=(j2 == 0), stop=False),
                          r=["onesb", ("maskb", j2)], w=[pk])
                S.add("pe", lambda e, pg=pg, j=j: e.matmul(pg[:, 0:32], SLTb, maskb[:, j, :], start=(j == 0), stop=True),
                      r=["SLTb", ("maskb", j)], w=[pk])
                S.add("act", lambda e, pg=pg, j=j: e.copy(pos[:, j, :], pg[:, 0:32]), r=[pk], w=[("pos", j)])
        S.barrier()
        ident32 = at(96, F32, [128, 128])
        XeT = AR.alloc(BF16, [16, CAP])
        actT = AR.alloc(BF16, [16, CAP])
        bgr_off = AR.off
        Oe = AR.alloc(BF16, [2, D])
        Sel = AR.alloc(BF16, [8, CAP])
        SelT = AR.alloc(BF16, [2, 1024])
        Gbs = AR.alloc(BF16, [128, 1024])
        bguT = AR.alloc(F32, [32, 32])
        oh = [AR.alloc(F32, [128, 128]) for _ in range(2)]
        glc = [AR.alloc(F32, [128, CAP]) for _ in range(2)]
        sgm = [AR.alloc(F32, [128, CAP]) for _ in range(2)]
        assert AR.off <= R1 + 140 * 1024, AR.off - R1
        _save = AR.off
        AR.off = bgr_off
        bgr = AR.alloc(F32, [128, 4096])
        AR.off = _save
        if "D" in phases:
            S.add("dve", lambda e: e.tensor_scalar(ident32, io_row[:, 0:128], pidx[:, 0:1], None, ALU.is_equal),
                  r=["io_row", "pidx"], w=["ident32"])
            for j in range(NJ):
                pg, pk = bank()
                S.add("pe", lambda e, pg=pg, j=j: e.transpose(pg[0:32, 0:128], pos[:, j, :], ident32), r=[("pos", j), "ident32"], w=[pk])
                S.add("pe", lambda e, pg=pg, j=j: e.transpose(pg[0:32, 128:256], Gt[:, j, :], ident32), r=[("Gt", j), "ident32"], w=[pk])
                S.add("act", lambda e, pg=pg, j=j: e.copy(posT[0:32, j * 128:(j + 1) * 128], pg[0:32, 0:128]), r=[pk], w=[("posT", j)])
                S.add("act", lambda e, pg=pg, j=j: e.copy(GT[0:32, j * 128:(j + 1) * 128], pg[0:32, 128:256]), r=[pk], w=[("GT", j)])
            S.add("sp", lambda e: e.dma_start(out=bgr[0:32, :], in_=b_gate_up[:, :]), w=["bgr"], dma=True)
            for half in range(2):
                pg, pk = bank()
                for c in range(16):
                    cc_ = half * 16 + c
                    S.add("pe", lambda e, pg=pg, c=c, cc_=cc_: e.transpose(pg[:, c * 32:(c + 1) * 32], bgr[0:32, cc_ * 128:(cc_ + 1) * 128],
                                                                       ident32[0:32, 0:32]), r=["bgr", "ident32"], w=[pk])
                S.add("act", lambda e, pg=pg, half=half: e.copy(bguT[:, half * 16:(half + 1) * 16, :],
                                                             pg.rearrange("p (c e) -> p c e", c=16)), r=[pk], w=[("bguT", half)])
            posT_keys = [("posT", j) for j in range(NJ)]
            GT_keys = [("GT", j) for j in range(NJ)]
            NTB = (NT + 511) // 512
            S.barrier()
            def moe_prep(ex):
                oi = ex % 2
                S.add("dve", lambda e, oi=oi, ex=ex: e.tensor_scalar(oh[oi][0:32, :], ones32[0:32, :], ident32[0:32, ex:ex + 1], None, ALU.mult),
                      r=["ones32", "ident32"], w=[f"oh{oi}"])
                for tb in range(NTB):
                    n = min(512, NT - tb * 512)
                    pgG, pgGk = bank()
                    S.add("pe", lambda e, pgG=pgG, oi=oi, tb=tb, n=n: e.matmul(pgG[:, 0:n], oh[oi][0:32, :], GT[0:32, tb * 512:tb * 512 + n],
                                                                          start=True, stop=True), r=[f"oh{oi}"] + GT_keys, w=[pgGk])
                    S.add("act", lambda e, pgG=pgG, tb=tb, n=n: e.copy(Gbs[:, tb * 512:tb * 512 + n], pgG[:, 0:n]), r=[pgGk], w=[("Gbs", tb)])
                    pgP, pgPk = bank()
                    S.add("pe", lambda e, pgP=pgP, oi=oi, tb=tb, n=n: e.matmul(pgP[:, 0:n], oh[oi][0:32, :], posT[0:32, tb * 512:tb * 512 + n],
                                                                          start=True, stop=True), r=[f"oh{oi}"] + posT_keys, w=[pgPk])
                    for stt in range(2):
                        S.add("dve", lambda e, pgP=pgP, stt=stt, tb=tb, n=n: e.scalar_tensor_tensor(
                            SelTs[ex % 2][:, stt, tb * 512:tb * 512 + n], pgP[:, 0:n], pidx[:, stt:stt + 1], Gbs[:, tb * 512:tb * 512 + n],
                            ALU.is_equal, ALU.mult), r=[pgPk, "pidx", ("Gbs", tb)], w=[("SelT", ex % 2, stt, tb)])
                for j in range(NJ):
                    S.add("dve", lambda e, j=j, ex=ex: e.tensor_scalar(
                        Sel[:, j, :], io_row[:, 0:CAP], pos[:, j, ex:ex + 1], mask[:, j, ex:ex + 1], ALU.is_equal, ALU.mult),
                        r=["io_row", ("pos", j), ("mask", j)], w=[("Sel", j)])
            def moe_gather(ex, fcs):
                for fc in fcs:
                    pg, pk = bank()
                    for j in range(NJ):
                        S.add("pe", lambda e, pg=pg, fc=fc, j=j: e.matmul(pg[:, 0:CAP], h2[:, j, fc * 128:(fc + 1) * 128], Sel[:, j, :],
                                                                      start=(j == 0), stop=(j == NJ - 1)),
                              r=[("h2", j), ("Sel", j)], w=[pk])
                    S.add("act", lambda e, pg=pg, fc=fc: e.copy(XeT[:, fc, :], pg[:, 0:CAP]), r=[pk], w=[("XeT", fc)])
            def moe_gu(ex, p_lo, p_hi):
                for piece in range(p_lo, p_hi):
                    wb, wk = wload(w_gate_up[ex][:, piece * 512:(piece + 1) * 512])
                    for sub in range(4):
                        nci = piece * 4 + sub
                        pg, pk = bank()
                        for fc in range(16):
                            S.add("pe", lambda e, pg=pg, fc=fc, sub=sub, wb=wb: e.matmul(
                                pg[:, 0:CAP], wb[:, fc, sub * 128:(sub + 1) * 128], XeT[:, fc, :], start=(fc == 0), stop=(fc == 15)),
                                r=XeT_keys + [wk], w=[pk])
                        gi = nci % 2
                        bias_ap = bguT[:, nci, ex:ex + 1]
                        if nci < 16:
                            S.add("dve", lambda e, pg=pg, gi=gi, bias_ap=bias_ap: e.tensor_scalar(
                                glc[gi], pg[:, 0:CAP], bias_ap, 7.0, ALU.add, ALU.min), r=[pk, ("bguT", nci // 16)], w=[f"glc{gi}"])
                            S.add("act", lambda e, gi=gi: e.activation(sgm[gi], glc[gi], AF.Sigmoid, scale=1.702), r=[f"glc{gi}"], w=[f"sgm{gi}"])
                            S.add("dve", lambda e, gi=gi, nci=nci: e.tensor_tensor(actT[:, nci, :], glc[gi], sgm[gi], ALU.mult),
                                  r=[f"glc{gi}", f"sgm{gi}"], w=[("actT", nci)])
                        else:
                            m_ = nci - 16
                            S.add("dve", lambda e, pg=pg, gi=gi, bias_ap=bias_ap: e.tensor_scalar(
                                glc[gi], pg[:, 0:CAP], bias_ap, 7.0, ALU.add, ALU.min), r=[pk, ("bguT", nci // 16)], w=[f"glc{gi}"])
                            S.add("dve", lambda e, gi=gi: e.tensor_scalar(sgm[gi], glc[gi], -7.0, 1.0, ALU.max, ALU.add),
                                  r=[f"glc{gi}"], w=[f"sgm{gi}"])
                            S.add("dve", lambda e, gi=gi, m_=m_: e.tensor_tensor(actT[:, m_, :], actT[:, m_, :], sgm[gi], ALU.mult),
                                  r=[("actT", m_), f"sgm{gi}"], w=[("actT", m_)])
            def moe_down(ex, fbs):
                for fb in fbs:
                    wb, wk = wload(w_down[ex][:, fb * 512:(fb + 1) * 512])
                    for stt in range(2):
                        pg, pk = bank()
                        for mc in range(16):
                            S.add("pe", lambda e, pg=pg, mc=mc, stt=stt, wb=wb: e.matmul(
                                pg, actT[:, mc, stt * 128:(stt + 1) * 128], wb[:, mc, :], start=(mc == 0), stop=(mc == 15)),
                                r=actT_keys + [wk], w=[pk])
                        S.add("act", lambda e, pg=pg, stt=stt, fb=fb: e.copy(Oe[:, stt, fb * 512:(fb + 1) * 512], pg), r=[pk], w=[("Oe", stt, fb)])
            def moe_scatter(ex, tiles):
                for (j, fb) in tiles:
                    if True:
                        pg, pk = bank()
                        for stt in range(2):
                            S.add("pe", lambda e, pg=pg, stt=stt, j=j, fb=fb: e.matmul(
                                pg, SelTs[ex % 2][:, stt, j * 128:(j + 1) * 128], Oe[:, stt, fb * 512:(fb + 1) * 512], start=(stt == 0), stop=(stt == 1)),
                                r=[("SelT", ex % 2, stt, j // 4), ("Oe", stt, fb)], w=[pk])
                        xv = x1[:, j, fb * 512:(fb + 1) * 512]
                        S.add("dve", lambda e, pg=pg, xv=xv: e.tensor_tensor(xv, xv, pg, ALU.add), r=[pk, ("x1", j)], w=[("x1", j)])

            XeT_keys = [("XeT", fc) for fc in range(16)]
            actT_keys = [("actT", m_) for m_ in range(16)]
            SelTs = [SelT, gain[0].bitcast(BF16)[:, 0:2048].rearrange("p (a b) -> p a b", a=2)]
            sc_tiles = [(j, fb) for j in range(NJ) for fb in range(4)]
            nsc = (len(sc_tiles) + 7) // 8
            moe_prep(0)
            moe_gather(0, range(16))
            for ex in range(NE):
                for k in range(8):
                    moe_gu(ex, k, k + 1)
                    if ex > 0:
                        moe_scatter(ex - 1, sc_tiles[k * nsc:(k + 1) * nsc])
                if ex + 1 < NE:
                    moe_prep(ex + 1)
                for fb in range(4):
                    moe_down(ex, [fb])
                    if ex + 1 < NE:
                        moe_gather(ex + 1, range(4 * fb, 4 * fb + 4))
            moe_scatter(NE - 1, sc_tiles)
            S.barrier()
            S.add("sp", lambda e: e.dma_start(out=bgr[0:32, 0:D], in_=b_down[:, :]), w=["bgr"], dma=True)
            for j in range(NJ):
                for fb in range(4):
                    pg, pk = bank()
                    S.add("pe", lambda e, pg=pg, j=j, fb=fb: e.matmul(pg, GT[0:32, j * 128:(j + 1) * 128], bgr[0:32, fb * 512:(fb + 1) * 512],
                                                                    start=True, stop=True), r=[("GT", j), "bgr"], w=[pk])
                    xv = x1[:, j, fb * 512:(fb + 1) * 512]
                    S.add("dve", lambda e, pg=pg, xv=xv: e.tensor_tensor(xv, xv, pg, ALU.add), r=[pk, ("x1", j)], w=[("x1", j)])
            if dbg:
                for j in range(NJ):
                    S.add("sp", lambda e, j=j: e.dma_start(out=dbg_t["x2"][j * 128:(j + 1) * 128, :], in_=x1[:, j, :]),
                          r=[("x1", j)], dma=True)

        S.barrier()
        h3T = at(64, BF16, [16, 1024])
        AR.off = R1 + 96 * 1024
        h3b = AR.alloc(BF16, [128, D])
        pT_ = AR.alloc(BF16, [2, 1024])
        pin = AR.alloc(F32, [128, 256])
        pinb = AR.alloc(BF16, [128, 256])
        u = AR.alloc(F32, [128, D])
        sgE = [AR.alloc(F32, [128, 512]) for _ in range(2)]
        obuf = AR.alloc(F32, [128, D])
        smE = AR.alloc(F32, [8, 4])
        if "E" in phases:
            load_gain(0, g_ple)
            for j in range(NJ):
                xj = x1[:, j, :]
                S.add("act", lambda e, xj=xj, j=j: e.activation(junkB2, xj, AF.Square, accum_out=smE[:, j, 0:1]),
                      r=[("x1", j)], w=["junkB2", ("smE", j)])
                S.add("act", lambda e, j=j: e.activation(smE[:, j, 0:1], smE[:, j, 0:1], AF.Ln, bias=EPS, scale=1.0 / D), r=[("smE", j)], w=[("smE", j)])
                S.add("act", lambda e, j=j: e.activation(smE[:, j, 0:1], smE[:, j, 0:1], AF.Exp, scale=-0.5), r=[("smE", j)], w=[("smE", j)])
                S.add("dve", lambda e, xj=xj, j=j: e.scalar_tensor_tensor(h3b, xj, smE[:, j, 0:1], gain[0], ALU.mult, ALU.mult),
                      r=[("x1", j), ("smE", j), "gain0"], w=["h3b"])
                transpose_rows(h3b, "h3b", h3T, lambda half, j=j: ("h3T", j, half), j * 128)
                S.add("sp", lambda e, j=j: e.dma_start(out=pin, in_=p_own[j * 128:(j + 1) * 128, :]), w=["pin"], dma=True)
                S.add("dve", lambda e: e.tensor_copy(pinb, pin), r=["pin"], w=["pinb"])
                pt, ptk = tbank()
                for k in range(2):
                    S.add("pe", lambda e, pt=pt, k=k: e.transpose(pt[:, k * 128:(k + 1) * 128], pinb[:, k * 128:(k + 1) * 128], ident),
                          r=["pinb", "ident"], w=[ptk])
                S.add("act", lambda e, pt=pt, j=j: e.copy(pT_[:, :, j * 128:(j + 1) * 128], pt[:, 0:256].rearrange("p (a b) -> p a b", a=2)),
                      r=[ptk], w=[("pT", j)])
            load_gain(0, g_ple_post)
            load_gain(1, g_final)
            for j in range(NJ):
                for fb in range(4):
                    wb, wk = wload(w_ple_gate[:, fb * 512:(fb + 1) * 512])
                    wp, wpk = wload(w_ple_proj[:, fb * 512:(fb + 1) * 512], rows=256)
                    pg, pk = bank()
                    for fc in range(16):
                        S.add("pe", lambda e, pg=pg, fc=fc, j=j, wb=wb: e.matmul(pg, h3T[:, fc, j * 128:(j + 1) * 128], wb[:, fc, :],
                                                                             start=(fc == 0), stop=(fc == 15)),
                              r=[("h3T", j, fc // 8), wk], w=[pk])
                    pp_, ppk = bank()
                    for k in range(2):
                        S.add("pe", lambda e, pp_=pp_, k=k, j=j, wp=wp: e.matmul(pp_, pT_[:, k, j * 128:(j + 1) * 128], wp[:, k, :],
                                                                             start=(k == 0), stop=(k == 1)),
                              r=[("pT", j), wpk], w=[ppk])
                    si = fb % 2
                    S.add("act", lambda e, pg=pg, si=si: e.activation(sgE[si], pg, AF.Sigmoid), r=[pk], w=[f"sgE{si}"])
                    S.add("dve", lambda e, pp_=pp_, si=si, fb=fb: e.tensor_tensor(u[:, fb * 512:(fb + 1) * 512], pp_, sgE[si], ALU.mult),
                          r=[ppk, f"sgE{si}"], w=[("u", fb)])
                ukeys = [("u", fb) for fb in range(4)]
                S.add("act", lambda e, j=j: e.activation(junkB2, u, AF.Square, accum_out=smE[:, j, 1:2]), r=ukeys, w=["junkB2", ("smE1", j)])
                S.add("act", lambda e, j=j: e.activation(smE[:, j, 1:2], smE[:, j, 1:2], AF.Ln, bias=EPS, scale=1.0 / D), r=[("smE1", j)], w=[("smE1", j)])
                S.add("act", lambda e, j=j: e.activation(smE[:, j, 1:2], smE[:, j, 1:2], AF.Exp, scale=-0.5), r=[("smE1", j)], w=[("smE1", j)])
                S.add("dve", lambda e, j=j: e.scalar_tensor_tensor(u, u, smE[:, j, 1:2], gain[0], ALU.mult, ALU.mult),
                      r=ukeys + [("smE1", j), "gain0"], w=ukeys)
                xj = x1[:, j, :]
                S.add("dve", lambda e, xj=xj: e.tensor_tensor(xj, xj, u, ALU.add), r=ukeys + [("x1", j)], w=[("x1", j)])
                S.add("act", lambda e, xj=xj, j=j: e.activation(junkB2, xj, AF.Square, accum_out=smE[:, j, 2:3]), r=[("x1", j)], w=["junkB2", ("smE2", j)])
                S.add("act", lambda e, j=j: e.activation(smE[:, j, 2:3], smE[:, j, 2:3], AF.Ln, bias=EPS, scale=1.0 / D), r=[("smE2", j)], w=[("smE2", j)])
                S.add("act", lambda e, j=j: e.activation(smE[:, j, 2:3], smE[:, j, 2:3], AF.Exp, scale=-0.5), r=[("smE2", j)], w=[("smE2", j)])
                S.add("dve", lambda e, xj=xj, j=j: e.scalar_tensor_tensor(obuf, xj, smE[:, j, 2:3], gain[1], ALU.mult, ALU.mult),
                      r=[("x1", j), ("smE2", j), "gain1"], w=["obuf"])
                S.add("sp", lambda e, j=j: e.dma_start(out=out[j * 128:(j + 1) * 128, :], in_=obuf), r=["obuf"], dma=True)
                out_dmas.append(len(S.ops) - 1)

        S.barrier()
        outs = [i for i, o in enumerate(S.ops) if o["dma"]]
        S.wait_all("sp", outs[-32:] + out_dmas)
        S.emit()
    return nc


def make_inputs(inputs, core):
    b, q = core // 4, core % 4
    x = np.asarray(inputs["x"], dtype=np.float32)
    p = np.asarray(inputs["p"], dtype=np.float32)
    own = np.concatenate([np.arange((4 * j + q) * 128, (4 * j + q + 1) * 128) for j in range(8)])
    m = {}
    pad = 3 - q
    xa = np.zeros((SEQ, D), np.float32)
    xa[pad * 128:] = x[b][:SEQ - pad * 128]
    m["x_all"] = xa
    m["x_own"] = np.ascontiguousarray(x[b][own])
    m["p_own"] = np.ascontiguousarray(p[0, b][own])
    ms = np.zeros((128, 4), np.float32)
    for kb in range(4):
        ms[:, kb] = 1.0 if kb >= pad else 0.0
    m["msel"] = ms
    am = np.zeros((128, 4, 128), np.float32)
    am[:, 3, :] = (np.arange(128)[:, None] < np.arange(128)[None, :]).astype(np.float32)
    m["amask"] = am
    for k in ["w_in", "conv_w", "conv_b", "dt_bias", "a_log", "d_skip", "ssd_norm_w", "w_branch_a", "w_branch_b",
              "w_out", "g_mix", "g_ffn", "w_router", "b_router", "w_gate_up", "b_gate_up", "w_down", "b_down",
              "g_ple", "w_ple_gate", "w_ple_proj", "g_ple_post"]:
        m[k] = np.ascontiguousarray(np.asarray(inputs[k], dtype=np.float32)[0])
    m["g_final"] = np.ascontiguousarray(np.asarray(inputs["g_final"], dtype=np.float32))
    return m, own


def kernel(**inputs):
    nc = build()
    in_maps = []
    owns = []
    for c in range(8):
        m, own = make_inputs(inputs, c)
        in_maps.append(m)
        owns.append(own)
    res = run_bass_kernel_spmd(nc, in_maps, core_ids=list(range(8)))
    outp = np.zeros((2, SEQ, D), np.float32)
    for c in range(8):
        outp[c // 4, owns[c]] = res.results[c]["out"]
    return outp
```

```python
import numpy as np
from contextlib import ExitStack
import concourse.bass as bass
import concourse.mybir as mybir
from concourse.bass_utils import run_bass_kernel_spmd

F32 = mybir.dt.float32
BF16 = mybir.dt.bfloat16
AF = mybir.ActivationFunctionType
ALU = mybir.AluOpType
AX = mybir.AxisListType

ENGS = ("pe", "act", "dve", "pool", "sp")
NDMASEM = 8
D = 2048
SEQ = 4096
NE = 32
CAP = 256
EPS = 1e-6
IN_DIM = 15392
C_Z, C_XBC, C_DT, C_Q, C_K, C_V, C_GA, C_GB = 0, 2048, 5120, 5152, 7200, 9248, 11296, 13344


class Sched:
    def __init__(self, nc, stack):
        self.nc = nc
        self.ops = []
        self.last_w = {}
        self.rd_eng = {}
        self.rd_dma = {}
        self.stack = stack

    def add(self, eng, fn, r=(), w=(), dma=False, prio=None):
        oid = len(self.ops)
        deps = set()
        for k in r:
            d = self.last_w.get(k)
            if d is not None:
                deps.add(d)
        for k in w:
            d = self.last_w.get(k)
            if d is not None:
                deps.add(d)
            for d in self.rd_eng.get(k, {}).values():
                deps.add(d)
            for d in self.rd_dma.get(k, ()):
                deps.add(d)
        for k in w:
            self.last_w[k] = oid
            self.rd_eng[k] = {}
            self.rd_dma[k] = []
        for k in r:
            if dma:
                self.rd_dma.setdefault(k, []).append(oid)
            else:
                self.rd_eng.setdefault(k, {})[eng] = oid
        deps.discard(oid)
        self.ops.append(dict(eng=eng, fn=fn, deps=deps, dma=dma, prio=(oid if prio is None else prio)))
        return oid

    def wait_all(self, eng, ids):
        self.ops.append(dict(eng=eng, fn=None, deps=set(ids), dma=False, prio=len(self.ops)))

    def barrier(self):
        self.nbar = getattr(self, "nbar", 0) + 1
        last = {}
        dmas = []
        for i, o in enumerate(self.ops):
            if o["fn"] is None:
                continue
            if o["dma"]:
                dmas.append(i)
            else:
                last[o["eng"]] = i
        ids = list(last.values()) + dmas[-64:]
        for e in ENGS:
            self.wait_all(e, ids)

    def emit(self):
        nc = self.nc
        ops = self.ops

        def skip(p, o):
            return (not p["dma"]) and (not o["dma"]) and p["eng"] == o["eng"] and p["eng"] == "pe"

        need = [False] * len(ops)
        for o in ops:
            for d in o["deps"]:
                if not skip(ops[d], o):
                    need[d] = True
        per_eng = {e: [] for e in ENGS}
        for i, o in enumerate(ops):
            per_eng[o["eng"]].append(i)
        for e in ENGS:
            per_eng[e].sort(key=lambda i: (ops[i]["prio"], i))
        cnt = {e: 0 for e in ENGS}
        dcnt = {e: 0 for e in ENGS}
        for e in ENGS:
            for i in per_eng[e]:
                o = ops[i]
                o["sig"] = None
                if o["fn"] is None:
                    continue
                if o["dma"]:
                    n = dcnt[e]
                    dcnt[e] += 1
                    o["sig"] = ("d", e, n % NDMASEM, 16 * (n // NDMASEM + 1))
                elif need[i]:
                    cnt[e] += 1
                    o["sig"] = ("c", e, 0, cnt[e])
        sems = {}
        for e in ENGS:
            sems[("c", e, 0)] = self.stack.enter_context(nc.semaphore(f"c_{e}"))
            if dcnt[e] > 0:
                for k in range(NDMASEM):
                    sems[("d", e, k)] = self.stack.enter_context(nc.semaphore(f"d_{e}_{k}"))

        def run_engine(ename, handle):
            known = {}
            for i in per_eng[ename]:
                o = ops[i]
                waits = {}
                for d in o["deps"]:
                    p = ops[d]
                    if skip(p, o):
                        continue
                    s = p["sig"]
                    if waits.get(s[:3], 0) < s[3]:
                        waits[s[:3]] = s[3]
                for key, v in waits.items():
                    if known.get(key, 0) >= v:
                        continue
                    known[key] = v
                    handle.wait_ge(sems[key], v)
                if o["fn"] is None:
                    continue
                ins = o["fn"](handle)
                s = o["sig"]
                if s is not None:
                    ins.then_inc(sems[s[:3]], 16 if s[0] == "d" else 1)

        block = self.stack.enter_context(nc.Block())

        @block.tensor
        def _(e):
            run_engine("pe", e)

        @block.scalar
        def _(e):
            run_engine("act", e)

        @block.vector
        def _(e):
            run_engine("dve", e)

        @block.gpsimd
        def _(e):
            run_engine("pool", e)

        @block.sync
        def _(e):
            run_engine("sp", e)


class Arena:
    def __init__(self, t, nbytes):
        self.t = t
        self.nbytes = nbytes
        self.off = 0

    def alloc(self, dt, shape):
        shape = list(shape)
        if shape[0] == 128 and len(shape) >= 2:
            shape = shape[1:]
        n = int(np.prod(shape))
        sz = n * (4 if dt == F32 else 2)
        sz = (sz + 31) // 32 * 32
        assert self.off + sz <= self.nbytes, ("arena overflow", self.off, sz)
        v = self.t[:, self.off // 4:(self.off + sz) // 4]
        self.off += sz
        if dt != F32:
            v = v.bitcast(dt)
        v = v[:, 0:n]
        if len(shape) == 2:
            v = v.rearrange("p (a b) -> p a b", a=shape[0])
        elif len(shape) == 3:
            v = v.rearrange("p (a b c) -> p a b c", a=shape[0], b=shape[1])
        return v


def build(NG=8, dbg=False, phases="ABCDE"):
    nc = bass.Bass("TRN2", target_bir_lowering=False)
    dram_in = lambda n, s: nc.dram_tensor(n, list(s), F32, kind="ExternalInput").ap()
    x_all = dram_in("x_all", [SEQ, D])
    x_own = dram_in("x_own", [1024, D])
    p_own = dram_in("p_own", [1024, 256])
    msel = dram_in("msel", [128, 4])
    amask = dram_in("amask", [128, 4, 128])
    w_in = dram_in("w_in", [D, IN_DIM])
    conv_w = dram_in("conv_w", [4, 3072])
    conv_b = dram_in("conv_b", [3072])
    dt_bias = dram_in("dt_bias", [32])
    a_log = dram_in("a_log", [32])
    d_skip = dram_in("d_skip", [32])
    ssd_norm_w = dram_in("ssd_norm_w", [D])
    w_branch_a = dram_in("w_branch_a", [D, D])
    w_branch_b = dram_in("w_branch_b", [D, D])
    w_out = dram_in("w_out", [D, D])
    g_mix = dram_in("g_mix", [D])
    g_ffn = dram_in("g_ffn", [D])
    w_router = dram_in("w_router", [D, NE])
    b_router = dram_in("b_router", [NE])
    w_gate_up = dram_in("w_gate_up", [NE, D, 2 * D])
    b_gate_up = dram_in("b_gate_up", [NE, 2 * D])
    w_down = dram_in("w_down", [NE, D, D])
    b_down = dram_in("b_down", [NE, D])
    g_ple = dram_in("g_ple", [D])
    w_ple_gate = dram_in("w_ple_gate", [D, D])
    w_ple_proj = dram_in("w_ple_proj", [256, D])
    g_ple_post = dram_in("g_ple_post", [D])
    g_final = dram_in("g_final", [D])
    out = nc.dram_tensor("out", [1024, D], F32, kind="ExternalOutput").ap()
    kT_d = nc.dram_tensor("kT_d", [16, 128, SEQ], BF16).ap()
    v_d = nc.dram_tensor("v_d", [16, 128, 32, 128], BF16).ap()
    ya_d = nc.dram_tensor("ya_d", [8, 128, D], BF16).ap()
    dbg_t = {}
    if dbg:
        dbg_t["ya"] = nc.dram_tensor("dbg_ya", [NG, 128, D], F32, kind="ExternalOutput").ap()
        dbg_t["yb"] = nc.dram_tensor("dbg_yb", [128, 16, 1024], F32, kind="ExternalOutput").ap()
        dbg_t["x1"] = nc.dram_tensor("dbg_x1", [1024, D], F32, kind="ExternalOutput").ap()
        dbg_t["q"] = nc.dram_tensor("dbg_q", [128, 16, 1024], F32, kind="ExternalOutput").ap()
        dbg_t["x2"] = nc.dram_tensor("dbg_x2", [1024, D], F32, kind="ExternalOutput").ap()

    st = ExitStack()
    with st:
        S = Sched(nc, st)
        ARN = 207 * 1024
        arena_t = st.enter_context(nc.sbuf_tensor("arena", [128, ARN // 4], F32))
        AR = Arena(arena_t, ARN)
        pgs = [st.enter_context(nc.psum_tensor(f"pg{i}", [128, 512], F32))[:] for i in range(6)]
        pts = [st.enter_context(nc.psum_tensor(f"pt{i}", [128, 1024], BF16))[:] for i in range(2)]
        cnt = {"pg": 0, "pt": 0, "wb": 0, "npg": 6}

        def bank():
            i = cnt["pg"] % cnt["npg"]
            cnt["pg"] += 1
            return pgs[i], f"pg{i}"

        def tbank():
            i = cnt["pt"] % 2
            cnt["pt"] += 1
            return pts[i], f"pt{i}"

        out_dmas = []

        ident = AR.alloc(BF16, [128, 128])
        io_row = AR.alloc(F32, [128, 256])
        pidx = AR.alloc(F32, [128, 2])
        UI = AR.alloc(F32, [128, 128])
        SL = AR.alloc(F32, [128, 128])
        ones32 = AR.alloc(F32, [128, 128])
        UIb = AR.alloc(BF16, [128, 128])
        onesb = AR.alloc(BF16, [128, 128])
        SLTb = AR.alloc(BF16, [128, 128])
        junkB2 = AR.alloc(BF16, [128, D])
        msel_t = AR.alloc(F32, [128, 4])
        amask_t = AR.alloc(F32, [128, 4, 128])
        gain = [AR.alloc(F32, [128, D]) for _ in range(2)]
        CONST_END = AR.off

        S.add("pool", lambda e: e.iota(io_row, pattern=[[1, 256]], base=0, channel_multiplier=0,
                                       allow_small_or_imprecise_dtypes=True), w=["io_row"])
        S.add("pool", lambda e: e.iota(pidx[:, 0:1], pattern=[[0, 1]], base=0, channel_multiplier=1,
                                       allow_small_or_imprecise_dtypes=True), w=["pidx"])
        S.add("pool", lambda e: e.iota(pidx[:, 1:2], pattern=[[0, 1]], base=128, channel_multiplier=1,
                                       allow_small_or_imprecise_dtypes=True), w=["pidx"])
        S.add("dve", lambda e: e.tensor_scalar(ones32, io_row[:, 0:128], pidx[:, 0:1], None, ALU.is_equal),
              r=["io_row", "pidx"], w=["ones32"])
        S.add("dve", lambda e: e.tensor_copy(ident, ones32), r=["ones32"], w=["ident"])
        S.add("dve", lambda e: e.tensor_scalar(UI, io_row[:, 0:128], pidx[:, 0:1], None, ALU.is_ge),
              r=["io_row", "pidx"], w=["UI"])
        S.add("dve", lambda e: e.tensor_scalar(SL, io_row[:, 0:128], pidx[:, 0:1], None, ALU.is_lt),
              r=["io_row", "pidx"], w=["SL"])
        S.add("dve", lambda e: e.tensor_scalar(UIb, io_row[:, 0:128], pidx[:, 0:1], None, ALU.is_le),
              r=["io_row", "pidx"], w=["UIb"])
        S.add("dve", lambda e: e.tensor_scalar(SLTb, io_row[:, 0:128], pidx[:, 0:1], None, ALU.is_gt),
              r=["io_row", "pidx"], w=["SLTb"])
        S.add("pool", lambda e: e.memset(ones32, 1.0), r=["ident"], w=["ones32"])
        S.add("pool", lambda e: e.memset(onesb, 1.0), w=["onesb"])
        S.add("sp", lambda e: e.dma_start(out=msel_t, in_=msel[:, :]), w=["msel"], dma=True)
        S.add("sp", lambda e: e.dma_start(out=amask_t, in_=amask[:, :, :]), w=["amask"], dma=True)

        def load_gain(slot, src):
            S.add("sp", lambda e: e.dma_start(out=gain[slot], in_=src.partition_broadcast(128)),
                  w=[f"gain{slot}"], dma=True)

        NWB = 2
        wbs = [AR.alloc(BF16, [16, 512]) for _ in range(NWB)]
        WB_END = AR.off

        def wload(src2d, ncols=512, rows=D):
            i = cnt["wb"] % NWB
            cnt["wb"] += 1
            nch = rows // 128
            dst = wbs[i][:, 0:nch, 0:ncols]
            pos_now = len(S.ops)
            pr = cnt.get("wmark")
            S.add("pool", lambda e: e.dma_start(out=dst, in_=src2d.rearrange("(c p) n -> p c n", p=128)),
                  w=[f"wb{i}"], dma=True)
            cnt["wmark"] = pos_now
            return wbs[i], f"wb{i}"

        def rmsnorm_rows(xt, xkey, gslot, outb, outkey, ss, sskey, junk, junkkey):
            S.add("act", lambda e: e.activation(junk, xt, AF.Square, accum_out=ss), r=[xkey], w=[junkkey, sskey])
            S.add("act", lambda e: e.activation(ss, ss, AF.Ln, bias=EPS, scale=1.0 / D), r=[sskey], w=[sskey])
            S.add("act", lambda e: e.activation(ss, ss, AF.Exp, scale=-0.5), r=[sskey], w=[sskey])
            S.add("dve", lambda e: e.scalar_tensor_tensor(outb, xt, ss, gain[gslot], ALU.mult, ALU.mult),
                  r=[xkey, sskey, f"gain{gslot}"], w=[outkey])

        def transpose_rows(srcb, srckey, dstT, dstkey_fn, tok0, ntok=128, nfc=16):
            for half in range(nfc // 8):
                pt, ptk = tbank()
                for k in range(8):
                    fc = half * 8 + k
                    S.add("pe", lambda e, fc=fc, k=k, pt=pt: e.transpose(pt[:, k * 128:(k + 1) * 128],
                                                                        srcb[:, fc * 128:(fc + 1) * 128], ident),
                          r=[srckey, "ident"], w=[ptk])
                S.add("act", lambda e, half=half, pt=pt: e.copy(
                    dstT[:, half * 8:(half + 1) * 8, tok0:tok0 + ntok],
                    pt.rearrange("p (a b) -> p a b", a=8)),
                    r=[ptk], w=[dstkey_fn(half)])

        A0 = AR.off
        xt = AR.alloc(F32, [128, D])
        hb = AR.alloc(BF16, [128, D])
        hT = AR.alloc(BF16, [16, 512])
        zs = AR.alloc(BF16, [4, D])
        xtm = AR.alloc(BF16, [4, D])
        Btm = AR.alloc(BF16, [4, 512])
        BT = AR.alloc(BF16, [4, 512])
        CT = AR.alloc(BF16, [4, 512])
        raw = [AR.alloc(F32, [128, 515]) for _ in range(2)]
        ctmp = [AR.alloc(F32, [128, 512]) for _ in range(2)]
        xcb = [AR.alloc(BF16, [128, 512]) for _ in range(2)]
        halo = AR.alloc(F32, [24, 3])
        convw_t = AR.alloc(F32, [4, 24])
        convb_t = AR.alloc(F32, [128, 24])
        wdt = AR.alloc(BF16, [16, 32])
        dtb_b = AR.alloc(F32, [128, 32])
        negA_b = AR.alloc(F32, [128, 32])
        dskip_b = AR.alloc(F32, [128, 32])
        ss = AR.alloc(F32, [128, 8])
        dt_all = AR.alloc(F32, [4, 32])
        a_all = AR.alloc(F32, [4, 32])
        dtr = AR.alloc(F32, [128, 32])
        acs = AR.alloc(F32, [128, 64])
        eacs = AR.alloc(F32, [128, 32])
        dst = AR.alloc(F32, [128, 32])
        cdec = AR.alloc(F32, [128, 32])
        Xdt = AR.alloc(BF16, [128, D])
        Xds = AR.alloc(BF16, [128, D])
        CBm = AR.alloc(F32, [4, 128])
        lh = [AR.alloc(F32, [8, 128]) for _ in range(2)]
        dec = [AR.alloc(F32, [4, 128]) for _ in range(2)]
        Mb = [AR.alloc(BF16, [4, 128]) for _ in range(2)]
        yo = [AR.alloc(F32, [128, 512]) for _ in range(2)]
        yy = [AR.alloc(F32, [128, 512]) for _ in range(2)]
        ssg = AR.alloc(F32, [128, 4])
        rm = AR.alloc(F32, [128, 4])
        state = AR.alloc(F32, [128, D])
        prevb = AR.alloc(BF16, [128, D])
        kst = [AR.alloc(BF16, [128, 512]) for _ in range(2)]
        vst = [AR.alloc(BF16, [128, 512]) for _ in range(2)]
        ya_cur = AR.alloc(BF16, [128, D])
        A_END = AR.off

        if "A" in phases:
            load_gain(0, g_mix)
            load_gain(1, ssd_norm_w)
            for k in range(4):
                S.add("sp", lambda e, k=k: e.dma_start(out=convw_t[:, k, :], in_=conv_w[k].rearrange("(c p) -> p c", p=128),
                                                       allow_slow_non_contiguous=True), w=[("convw", k)], dma=True)
            S.add("sp", lambda e: e.dma_start(out=convb_t, in_=conv_b.rearrange("(c p) -> p c", p=128),
                                              allow_slow_non_contiguous=True), w=["convb"], dma=True)
            S.add("pool", lambda e: e.dma_start(out=wdt, in_=w_in[:, C_DT:C_DT + 32].rearrange("(c p) n -> p c n", p=128)),
                  w=["wdt"], dma=True)
            S.add("sp", lambda e: e.dma_start(out=dtb_b, in_=dt_bias.partition_broadcast(128)), w=["dtb"], dma=True)
            S.add("sp", lambda e: e.dma_start(out=negA_b, in_=a_log.partition_broadcast(128)), w=["negA"], dma=True)
            S.add("sp", lambda e: e.dma_start(out=dskip_b, in_=d_skip.partition_broadcast(128)), w=["dskip"], dma=True)
            S.add("act", lambda e: e.activation(negA_b, negA_b, AF.Exp), r=["negA"], w=["negA"])
            S.add("dve", lambda e: e.tensor_scalar(negA_b, negA_b, -1.0, None, ALU.mult), r=["negA"], w=["negA"])
            S.add("pool", lambda e: e.memset(halo, 0.0), w=["halo"])
            S.add("pool", lambda e: e.memset(state, 0.0), w=["state"])
            S.add("pool", lambda e: e.memset(prevb, 0.0), w=["prevb"])

            own_flag = [False]

            def SY(*a_, **k_):
                if own_flag[0]:
                    S.add(*a_, **k_)

            def emit_A1(G):
                for cc in range(4):
                    c = 4 * G + cc
                    S.add("sp", lambda e, c=c: e.dma_start(out=xt, in_=x_all[c * 128:(c + 1) * 128, :]), w=["xt"], dma=True)
                    rmsnorm_rows(xt, "xt", 0, hb, "hb", ss[:, 0:1], "ss0", junkB2, "junkB2")
                    transpose_rows(hb, "hb", hT, lambda half, cc=cc: ("hT", cc, half), cc * 128)

            hTkeys = [("hT", cc, half) for cc in range(4) for half in range(2)]
            for G in range(NG):
                if G == 0:
                    emit_A1(0)
                for jb in range(8):
                    col0 = (C_Z + jb * 512) if jb < 4 else (C_V + (jb - 4) * 512)
                    wb, wk = wload(w_in[:, col0:col0 + 512])
                    for cc in ((3,) if jb < 4 else range(4)):
                        c = 4 * G + cc
                        pg, pk = bank()
                        for fc in range(16):
                            S.add("pe", lambda e, pg=pg, fc=fc, cc=cc, wb=wb: e.matmul(
                                pg, hT[:, fc, cc * 128:(cc + 1) * 128], wb[:, fc, :], start=(fc == 0), stop=(fc == 15)),
                                r=[("hT", cc, fc // 8), wk], w=[pk])
                        if jb < 4:
                            S.add("act", lambda e, pg=pg, cc=cc, jb=jb: e.activation(
                                zs[:, cc, jb * 512:(jb + 1) * 512], pg, AF.Silu), r=[pk], w=[("zs", cc, jb)])
                        else:
                            vi = cnt.setdefault("vst", 0) % 2
                            cnt["vst"] += 1
                            S.add("act", lambda e, pg=pg, vi=vi: e.copy(vst[vi], pg), r=[pk], w=[f"vst{vi}"])
                            S.add("sp", lambda e, vi=vi, c=c, jb=jb: e.dma_start(
                                out=v_d[(jb - 4) * 4:(jb - 3) * 4, :, c, :].rearrange("h p d -> p h d"),
                                in_=vst[vi].rearrange("p (h d) -> p h d", h=4)),
                                r=[f"vst{vi}"], w=[("v_d", c)], dma=True)
                for cc in range(4):
                    pg, pk = bank()
                    for fc in range(16):
                        S.add("pe", lambda e, pg=pg, fc=fc, cc=cc: e.matmul(
                            pg[:, 0:32], hT[:, fc, cc * 128:(cc + 1) * 128], wdt[:, fc, :], start=(fc == 0), stop=(fc == 15)),
                            r=[("hT", cc, fc // 8), "wdt"], w=[pk])
                    S.add("dve", lambda e, pg=pg: e.tensor_tensor(dtr, pg[:, 0:32], dtb_b, ALU.add), r=[pk, "dtb"], w=["dtr"])
                    S.add("act", lambda e: e.activation(dtr, dtr, AF.Exp), r=["dtr"], w=["dtr"])
                    S.add("act", lambda e, cc=cc: e.activation(dt_all[:, cc, :], dtr, AF.Ln, bias=1.0), r=["dtr"], w=[("dt", cc)])
                    S.add("dve", lambda e, cc=cc: e.tensor_tensor(a_all[:, cc, :], dt_all[:, cc, :], negA_b, ALU.mult),
                          r=[("dt", cc), "negA"], w=[("a", cc)])
                for piece in range(10):
                    col0 = (C_XBC + piece * 512) if piece < 6 else (C_K + (piece - 6) * 512)
                    wb, wk = wload(w_in[:, col0:col0 + 512])
                    for sub in range(4):
                        i = piece * 4 + sub
                        pg, pk = bank()
                        for fc in range(16):
                            S.add("pe", lambda e, pg=pg, fc=fc, sub=sub, wb=wb: e.matmul(
                                pg, wb[:, fc, sub * 128:(sub + 1) * 128], hT[:, fc, :], start=(fc == 0), stop=(fc == 15)),
                                r=hTkeys + [wk], w=[pk])
                        if i < 24:
                            ri = i % 2
                            rw, ct, xc = raw[ri], ctmp[ri], xcb[ri]
                            S.add("act", lambda e, pg=pg, rw=rw: e.copy(rw[:, 3:515], pg), r=[pk], w=[f"raw{ri}"])
                            S.add("act", lambda e, rw=rw, i=i: e.copy(rw[:, 0:3], halo[:, i, :]),
                                  r=[("halo", i)], w=[f"rawh{ri}"])
                            rk = [f"raw{ri}", f"rawh{ri}"] + [("convw", k) for k in range(4)]
                            S.add("dve", lambda e, rw=rw, ct=ct, i=i: e.tensor_scalar(
                                ct, rw[:, 0:512], convw_t[:, 0, i:i + 1], None, ALU.mult), r=rk, w=[f"ct{ri}"])
                            for k in (1, 2, 3):
                                S.add("dve", lambda e, rw=rw, ct=ct, i=i, k=k: e.scalar_tensor_tensor(
                                    ct, rw[:, k:512 + k], convw_t[:, k, i:i + 1], ct, ALU.mult, ALU.add),
                                    r=rk + [f"ct{ri}"], w=[f"ct{ri}"])
                            S.add("act", lambda e, rw=rw, i=i: e.copy(halo[:, i, :], rw[:, 512:515]),
                                  r=[f"raw{ri}"], w=[("halo", i)])
                            if i < 16:
                                S.add("act", lambda e, ct=ct, xc=xc, i=i: e.activation(xc, ct, AF.Silu, bias=convb_t[:, i:i + 1]),
                                      r=[f"ct{ri}", "convb"], w=[f"xc{ri}"])
                                src, sk = xc, f"xc{ri}"
                            elif i < 20:
                                g = i - 16
                                S.add("act", lambda e, ct=ct, g=g, i=i: e.activation(BT[:, g, :], ct, AF.Silu, bias=convb_t[:, i:i + 1]),
                                      r=[f"ct{ri}", "convb"], w=[("BT", g)])
                                src, sk = BT[:, g, :], ("BT", g)
                            else:
                                g = i - 20
                                S.add("act", lambda e, ct=ct, g=g, i=i: e.activation(CT[:, g, :], ct, AF.Silu, bias=convb_t[:, i:i + 1]),
                                      r=[f"ct{ri}", "convb"], w=[("CT", g)])
                                src = None
                            if src is not None:
                                pt, ptk = tbank()
                                for cc in range(4):
                                    S.add("pe", lambda e, pt=pt, cc=cc, src=src: e.transpose(
                                        pt[:, cc * 128:(cc + 1) * 128], src[:, cc * 128:(cc + 1) * 128], ident),
                                        r=[sk, "ident"], w=[ptk])
                                if i < 16:
                                    S.add("act", lambda e, pt=pt, i=i: e.copy(
                                        xtm[:, :, i * 128:(i + 1) * 128], pt[:, 0:512].rearrange("p (a b) -> p a b", a=4)),
                                        r=[ptk], w=[("xtm", i)])
                                else:
                                    S.add("act", lambda e, pt=pt, g=g: e.copy(
                                        Btm[:, :, g * 128:(g + 1) * 128], pt[:, 0:512].rearrange("p (a b) -> p a b", a=4)),
                                        r=[ptk], w=[("Btm", g)])
                        else:
                            hd = i - 24
                            ki = hd % 2
                            S.add("act", lambda e, pg=pg, ki=ki: e.copy(kst[ki], pg), r=[pk], w=[f"kst{ki}"])
                            S.add("sp", lambda e, ki=ki, hd=hd, G=G: e.dma_start(
                                out=kT_d[hd, :, G * 512:(G + 1) * 512], in_=kst[ki]),
                                r=[f"kst{ki}"], w=[("kT_d", hd)], dma=True)
                if G + 1 < NG:
                    emit_A1(G + 1)
                xtm_keys = [("xtm", i) for i in range(16)]
                for cc in range(4):
                    own_flag[0] = (cc == 3)
                    pg, pk = bank()
                    S.add("pe", lambda e, pg=pg, cc=cc: e.matmul(pg[:, 0:32], UI, a_all[:, cc, :], start=True, stop=True),
                          r=[("a", cc), "UI"], w=[pk])
                    S.add("pe", lambda e, pg=pg, cc=cc: e.matmul(pg[:, 32:64], ones32, a_all[:, cc, :], start=True, stop=True),
                          r=[("a", cc), "ones32"], w=[pk])
                    S.add("act", lambda e, pg=pg: e.copy(acs, pg[:, 0:64]), r=[pk], w=["acs"])
                    SY("act", lambda e: e.activation(eacs, acs[:, 0:32], AF.Exp), r=["acs"], w=["eacs"])
                    S.add("act", lambda e: e.activation(cdec, acs[:, 32:64], AF.Exp), r=["acs"], w=["cdec"])
                    S.add("dve", lambda e: e.tensor_tensor(dst, acs[:, 32:64], acs[:, 0:32], ALU.subtract), r=["acs"], w=["dst"])
                    S.add("act", lambda e: e.activation(dst, dst, AF.Exp), r=["dst"], w=["dst"])
                    if G == 0 and cc < 3:
                        S.add("dve", lambda e, cc=cc: e.tensor_scalar(dst, dst, msel_t[:, cc:cc + 1], None, ALU.mult),
                              r=["dst", "msel"], w=["dst"])
                    x3 = xtm[:, cc, :].rearrange("p (h d) -> p h d", h=32)
                    S.add("dve", lambda e, cc=cc, x3=x3: e.tensor_tensor(
                        Xdt.rearrange("p (h d) -> p h d", h=32), x3,
                        dt_all[:, cc, :].unsqueeze(2).to_broadcast([128, 32, 64]), ALU.mult),
                        r=xtm_keys + [("dt", cc)], w=["Xdt"])
                    S.add("dve", lambda e: e.tensor_tensor(
                        Xds.rearrange("p (h d) -> p h d", h=32), Xdt.rearrange("p (h d) -> p h d", h=32),
                        dst.unsqueeze(2).to_broadcast([128, 32, 64]), ALU.mult), r=["Xdt", "dst"], w=["Xds"])
                    pg, pk = bank()
                    for g in range(4):
                        SY("pe", lambda e, pg=pg, g=g, cc=cc: e.matmul(
                            pg[:, g * 128:(g + 1) * 128], BT[:, g, cc * 128:(cc + 1) * 128], CT[:, g, cc * 128:(cc + 1) * 128],
                            start=True, stop=True), r=[("BT", g), ("CT", g)], w=[pk])
                    SY("dve", lambda e, pg=pg: e.tensor_tensor(
                        CBm, pg.rearrange("p (g l) -> p g l", g=4), UI.unsqueeze(1).to_broadcast([128, 4, 128]), ALU.mult),
                        r=[pk, "UI"], w=["CBm"])
                    for g in range(4):
                        gi = g % 2
                        SY("dve", lambda e, g=g, gi=gi, cc=cc: e.tensor_tensor(
                            lh[gi], SL.unsqueeze(1).to_broadcast([128, 8, 128]),
                            a_all[:, cc, g * 8:(g + 1) * 8].unsqueeze(2).to_broadcast([128, 8, 128]), ALU.mult),
                            r=["SL", ("a", cc)], w=[f"lh{gi}"])
                        pY, pYk = bank()
                        for half in range(2):
                            pseg, psk = bank()
                            for j in range(4):
                                SY("pe", lambda e, pseg=pseg, j=j, gi=gi, half=half: e.matmul(
                                    pseg[:, j * 128:(j + 1) * 128], lh[gi][:, half * 4 + j, :], UI, start=True, stop=True),
                                    r=[f"lh{gi}", "UI"], w=[psk])
                            SY("act", lambda e, pseg=pseg, half=half: e.activation(
                                dec[half], pseg.rearrange("p (j l) -> p j l", j=4), AF.Exp), r=[psk], w=[f"dec{half}"])
                            SY("dve", lambda e, half=half, g=g: e.tensor_tensor(
                                Mb[half], dec[half], CBm[:, g:g + 1, :].to_broadcast([128, 4, 128]), ALU.mult),
                                r=[f"dec{half}", "CBm"], w=[f"Mb{half}"])
                            for j in range(4):
                                h = g * 8 + half * 4 + j
                                SY("pe", lambda e, pY=pY, half=half, j=j, h=h: e.matmul(
                                    pY[:, (half * 4 + j) * 64:(half * 4 + j + 1) * 64], Mb[half][:, j, :],
                                    Xdt[:, h * 64:(h + 1) * 64], start=True, stop=True),
                                    r=[f"Mb{half}", "Xdt"], w=[pYk])
                        pYo, pYok = bank()
                        SY("pe", lambda e, pYo=pYo, g=g, cc=cc: e.matmul(
                            pYo, CT[:, g, cc * 128:(cc + 1) * 128], prevb[:, g * 512:(g + 1) * 512], start=True, stop=True),
                            r=[("CT", g), ("prevb", g)], w=[pYok])
                        SY("act", lambda e, pYo=pYo, gi=gi: e.copy(yo[gi], pYo), r=[pYok], w=[f"yo{gi}"])
                        SY("dve", lambda e, gi=gi, g=g: e.tensor_tensor(
                            yo[gi].rearrange("p (h d) -> p h d", h=8), yo[gi].rearrange("p (h d) -> p h d", h=8),
                            eacs[:, g * 8:(g + 1) * 8].unsqueeze(2).to_broadcast([128, 8, 64]), ALU.mult),
                            r=[f"yo{gi}", "eacs"], w=[f"yo{gi}"])
                        SY("dve", lambda e, gi=gi, pY=pY: e.tensor_tensor(yy[gi], pY, yo[gi], ALU.add),
                              r=[pYk, f"yo{gi}"], w=[f"yy{gi}"])
                        SY("dve", lambda e, gi=gi, g=g, cc=cc: e.tensor_tensor(
                            yo[gi].rearrange("p (h d) -> p h d", h=8),
                            xtm[:, cc, g * 512:(g + 1) * 512].rearrange("p (h d) -> p h d", h=8),
                            dskip_b[:, g * 8:(g + 1) * 8].unsqueeze(2).to_broadcast([128, 8, 64]), ALU.mult),
                            r=xtm_keys + ["dskip", f"yy{gi}"], w=[f"yo{gi}"])
                        SY("dve", lambda e, gi=gi: e.tensor_tensor(yy[gi], yy[gi], yo[gi], ALU.add),
                              r=[f"yy{gi}", f"yo{gi}"], w=[f"yy{gi}"])
                        SY("dve", lambda e, gi=gi, g=g, cc=cc: e.tensor_tensor(
                            yy[gi], yy[gi], zs[:, cc, g * 512:(g + 1) * 512], ALU.mult),
                            r=[f"yy{gi}", ("zs", cc, g)], w=[f"yy{gi}"])
                        SY("act", lambda e, gi=gi, g=g: e.activation(yo[gi], yy[gi], AF.Square, accum_out=ssg[:, g:g + 1]),
                              r=[f"yy{gi}"], w=[f"yo{gi}", ("ssg", g)])
                        SY("act", lambda e, g=g: e.activation(rm[:, g:g + 1], ssg[:, g:g + 1], AF.Ln, bias=EPS, scale=1.0 / 512),
                              r=[("ssg", g)], w=[("rm", g)])
                        SY("act", lambda e, g=g: e.activation(rm[:, g:g + 1], rm[:, g:g + 1], AF.Exp, scale=-0.5),
                              r=[("rm", g)], w=[("rm", g)])
                        SY("dve", lambda e, gi=gi, g=g: e.tensor_tensor(
                            yy[gi], yy[gi], gain[1][:, g * 512:(g + 1) * 512], ALU.mult),
                            r=[f"yy{gi}", "gain1"], w=[f"yy{gi}"])
                        dstv = ya_cur[:, g * 512:(g + 1) * 512]
                        if True:
                            SY("dve", lambda e, dstv=dstv, g=g, gi=gi: e.tensor_scalar(
                                dstv, yy[gi], rm[:, g:g + 1], None, ALU.mult),
                                r=[f"yy{gi}", ("rm", g)], w=[("ya", g)])
                        else:
                            SY("dve", lambda e, dstv=dstv, g=g, gi=gi: e.scalar_tensor_tensor(
                                dstv, yy[gi], rm[:, g:g + 1], dstv, ALU.mult, ALU.add),
                                r=[f"yy{gi}", ("rm", g), ("ya", g)], w=[("ya", g)])
                        pSt, pStk = bank()
                        S.add("pe", lambda e, pSt=pSt, g=g, cc=cc: e.matmul(
                            pSt, Btm[:, cc, g * 128:(g + 1) * 128], Xds[:, g * 512:(g + 1) * 512], start=True, stop=True),
                            r=[("Btm", g), "Xds"], w=[pStk])
                        sv = state[:, g * 512:(g + 1) * 512]
                        S.add("dve", lambda e, sv=sv, g=g: e.tensor_tensor(
                            sv.rearrange("p (h d) -> p h d", h=8), sv.rearrange("p (h d) -> p h d", h=8),
                            cdec[:, g * 8:(g + 1) * 8].unsqueeze(2).to_broadcast([128, 8, 64]), ALU.mult),
                            r=[("state", g), "cdec"], w=[("state", g)])
                        S.add("dve", lambda e, sv=sv, pSt=pSt: e.tensor_tensor(sv, sv, pSt, ALU.add),
                              r=[("state", g), pStk], w=[("state", g)])
                        S.add("act", lambda e, sv=sv, g=g: e.copy(prevb[:, g * 512:(g + 1) * 512], sv),
                              r=[("state", g)], w=[("prevb", g)])
                S.add("sp", lambda e, G=G: e.dma_start(out=ya_d[G], in_=ya_cur),
                      r=[("ya", g) for g in range(4)], w=[("ya_d", G)], dma=True)
                if dbg:
                    S.add("pool", lambda e, G=G: e.dma_start(out=dbg_t["ya"][G], in_=ya_cur),
                          r=[("ya", g) for g in range(4)], dma=True)
                    out_dmas.append(len(S.ops) - 1)


        NJ = NG
        NT = NJ * 128
        R1 = WB_END

        def at(off_kb, dt, shape):
            AR.off = R1 + off_kb * 1024
            return AR.alloc(dt, shape)

        S.barrier()
        hTo = at(0, BF16, [16, 1024])
        qT = at(32, BF16, [16, 1024])
        ybT = at(64, BF16, [16, 1024])
        kTh = [at(96 + 8 * i, BF16, [128, SEQ]) for i in range(2)]
        vh = [at(112 + 8 * i, BF16, [32, 128]) for i in range(2)]
        xo = at(64, F32, [128, D])
        hbo = at(72, BF16, [128, D])
        junkB = at(76, BF16, [128, D])
        AR.off = R1 + 128 * 1024
        ssb = AR.alloc(F32, [128, 8])
        Et = [AR.alloc(F32, [128, 512]) for _ in range(2)]
        spb = [AR.alloc(BF16, [128, 512]) for _ in range(2)]
        expc = [AR.alloc(F32, [128, 512]) for _ in range(2)]
        wbf = [AR.alloc(BF16, [128, 512]) for _ in range(2)]
        Trow = [AR.alloc(BF16, [128, 512]) for _ in range(2)]

        def own_hT(gslot, src_tiles_fn, dstT, keyname):
            for j in range(NJ):
                src_tiles_fn(j)
                rmsnorm_rows(xo, "xo", gslot, hbo, "hbo", ssb[:, 0:1], "ssb0", junkB, "junkB")
                transpose_rows(hbo, "hbo", dstT, lambda half, j=j: (keyname, j, half), j * 128)

        if "B" in phases:
            load_gain(0, g_mix)
            own_hT(0, lambda j: S.add("sp", lambda e: e.dma_start(out=xo, in_=x_own[j * 128:(j + 1) * 128, :]),
                                      w=["xo"], dma=True), hTo, "hTo")
            hTo_keys = [("hTo", j, half) for j in range(NJ) for half in range(2)]
            NTB = (NT + 511) // 512
            for piece in range(4):
                wb, wk = wload(w_in[:, C_Q + piece * 512:C_Q + (piece + 1) * 512])
                for sub in range(4):
                    hd = piece * 4 + sub
                    for tb in range(NTB):
                        n = min(512, NT - tb * 512)
                        pg, pk = bank()
                        for fc in range(16):
                            S.add("pe", lambda e, pg=pg, fc=fc, sub=sub, wb=wb, tb=tb, n=n: e.matmul(
                                pg[:, 0:n], wb[:, fc, sub * 128:(sub + 1) * 128], hTo[:, fc, tb * 512:tb * 512 + n],
                                start=(fc == 0), stop=(fc == 15)), r=hTo_keys + [wk], w=[pk])
                        S.add("act", lambda e, pg=pg, hd=hd, tb=tb, n=n: e.mul(
                            qT[:, hd, tb * 512:tb * 512 + n], pg[:, 0:n], float(128 ** -0.5)), r=[pk], w=[("qT", hd)])

            if dbg:
                S.add("pool", lambda e: e.dma_start(out=dbg_t["q"][:, :, 0:NT], in_=qT[:, :, 0:NT]),
                      r=[("qT", hd) for hd in range(16)], dma=True)
            NKB = 4 * NJ
            cnt["npg"] = 4
            for hp in range(8):
                for ci in range(2):
                    hd = hp * 2 + ci
                    S.add("sp", lambda e, hd=hd, ci=ci: e.dma_start(out=kTh[ci][:, 0:NKB * 128], in_=kT_d[hd, :, 0:NKB * 128]),
                          r=[("kT_d", hd)], w=[f"kTh{ci}"], dma=True)
                    S.add("sp", lambda e, hd=hd, ci=ci: e.dma_start(
                        out=vh[ci][:, 0:NKB, :], in_=v_d[hd, :, 0:NKB, :]),
                        r=[("v_d", c) for c in range(NKB)], w=[f"vh{ci}"], dma=True)
                for Q in range((NJ + 3) // 4):
                    j0 = 4 * Q
                    W = min(4, NJ - j0)
                    WN = W * 128
                    pys = []
                    for ci in range(2):
                        S.add("dve", lambda e, ci=ci: e.memset(Trow[ci], 0.0), w=[f"Trow{ci}"])
                        pys.append((pgs[4 + ci], f"pg{4 + ci}"))
                        S.add("pe", lambda e, ci=ci, WN=WN: e.matmul(pgs[4 + ci][:, 0:WN], onesb[0:1, :], Trow[ci][0:1, 0:WN], start=True, stop=False),
                              r=["onesb", f"Trow{ci}"], w=[f"pg{4 + ci}"])
                    kmax = 4 * (j0 + W - 1) + 3
                    def emit_qk(kb, j0=j0, WN=WN, hp=hp):
                        c0 = (max(j0, kb // 4) - j0) * 128
                        res = []
                        for ci in range(2):
                            hd = hp * 2 + ci
                            pz, pzk = bank()
                            res.append((pz, pzk))
                            S.add("pe", lambda e, pz=pz, ci=ci, hd=hd, kb=kb, c0=c0, WN=WN, j0=j0: e.matmul(
                                pz[:, c0:WN], kTh[ci][:, kb * 128:(kb + 1) * 128], qT[:, hd, j0 * 128 + c0:j0 * 128 + WN],
                                start=True, stop=True), r=[f"kTh{ci}", ("qT", hd)], w=[pzk])
                        return res

                    nxt_pz = emit_qk(kmax)
                    for kb in range(kmax, -1, -1):
                        jmin = max(j0, (kb - 3 + 3) // 4)
                        c0 = (jmin - j0) * 128
                        jm = kb // 4
                        pzs = nxt_pz
                        for ci in range(2):
                            hd = hp * 2 + ci
                            pz, pzk = pzs[ci]
                            S.add("act", lambda e, pz=pz, ci=ci, c0=c0, WN=WN: e.activation(Et[ci][:, c0:WN], pz[:, c0:WN], AF.Exp),
                                  r=[pzk], w=[f"E{ci}"])
                            if kb % 4 == 3 and j0 <= jm < j0 + W:
                                cm = (jm - j0) * 128
                                S.add("dve", lambda e, ci=ci, cm=cm: e.tensor_tensor(
                                    Et[ci][:, cm:cm + 128], Et[ci][:, cm:cm + 128], amask_t[:, 3, :], ALU.mult),
                                    r=[f"E{ci}", "amask"], w=[f"E{ci}"])
                            if kb < 3:
                                S.add("dve", lambda e, ci=ci, c0=c0, WN=WN, kb=kb: e.tensor_scalar(
                                    Et[ci][:, c0:WN], Et[ci][:, c0:WN], msel_t[:, kb:kb + 1], None, ALU.mult),
                                    r=[f"E{ci}", "msel"], w=[f"E{ci}"])
                            S.add("act", lambda e, ci=ci, c0=c0, WN=WN: e.activation(spb[ci][:, c0:WN], Et[ci][:, c0:WN], AF.Ln, bias=1.0),
                                  r=[f"E{ci}"], w=[f"sp{ci}"])
                        pcs = []
                        for ci in range(2):
                            pc, pck = bank()
                            pcs.append((pc, pck))
                            S.add("pe", lambda e, pc=pc, ci=ci, c0=c0, WN=WN: e.matmul(
                                pc[:, c0:WN], UIb, spb[ci][:, c0:WN], start=True, stop=False), r=["UIb", f"sp{ci}"], w=[pck])
                            S.add("pe", lambda e, pc=pc, ci=ci, c0=c0, WN=WN: e.matmul(
                                pc[:, c0:WN], onesb[0:1, :], Trow[ci][0:1, c0:WN], start=False, stop=True), r=["onesb", f"Trow{ci}"], w=[pck])
                            S.add("act", lambda e, pc=pc, ci=ci, c0=c0, WN=WN: e.copy(Trow[ci][0:1, c0:WN], pc[0:1, c0:WN]),
                                  r=[pck], w=[f"Trow{ci}"])
                            S.add("act", lambda e, pc=pc, ci=ci, c0=c0, WN=WN: e.activation(expc[ci][:, c0:WN], pc[:, c0:WN], AF.Exp, scale=-1.0),
                                  r=[pck], w=[f"expc{ci}"])
                            S.add("dve", lambda e, ci=ci, c0=c0, WN=WN: e.tensor_tensor(
                                wbf[ci][:, c0:WN], Et[ci][:, c0:WN], expc[ci][:, c0:WN], ALU.mult),
                                r=[f"E{ci}", f"expc{ci}"], w=[f"w{ci}"])
                        nxt_pz = emit_qk(kb - 1) if kb > 0 else None
                        for ci in range(2):
                            py, pyk = pys[ci]
                            newj = (kb - 3) // 4 if (kb - 3) % 4 == 0 and j0 <= (kb - 3) // 4 < j0 + W else None
                            last = (kb == 0)
                            if newj is not None:
                                cn = (newj - j0) * 128
                                S.add("pe", lambda e, py=py, ci=ci, kb=kb, cn=cn, last=last: e.matmul(
                                    py[:, cn:cn + 128], vh[ci][:, kb, :], wbf[ci][:, cn:cn + 128], start=False, stop=last),
                                    r=[f"vh{ci}", f"w{ci}"], w=[pyk])
                                c1 = cn + 128
                            else:
                                c1 = c0
                            if c1 < WN:
                                S.add("pe", lambda e, py=py, ci=ci, kb=kb, c1=c1, WN=WN, last=last: e.matmul(
                                    py[:, c1:WN], vh[ci][:, kb, :], wbf[ci][:, c1:WN], start=False, stop=last),
                                    r=[f"vh{ci}", f"w{ci}"], w=[pyk])
                    for ci in range(2):
                        hd = hp * 2 + ci
                        py, pyk = pys[ci]
                        S.add("act", lambda e, py=py, hd=hd, WN=WN, j0=j0: e.copy(ybT[:, hd, j0 * 128:j0 * 128 + WN], py[:, 0:WN]),
                              r=[pyk], w=[("ybT", hd)])
            cnt["npg"] = 6
            if dbg:
                S.add("pool", lambda e: e.dma_start(out=dbg_t["yb"][:, :, 0:NT], in_=ybT[:, :, 0:NT]),
                      r=[("ybT", hd) for hd in range(16)], dma=True)

        S.barrier()
        yaT = at(32, BF16, [16, 1024])
        mT = at(96, BF16, [16, 1024])
        AR.off = R1 + 128 * 1024
        yab = AR.alloc(BF16, [128, D])
        sg = [AR.alloc(F32, [128, 512]) for _ in range(2)]
        tt = [AR.alloc(F32, [128, 512]) for _ in range(2)]
        NTB = (NT + 511) // 512
        if "C" in phases:
            for j in range(NJ):
                S.add("sp", lambda e, j=j: e.dma_start(out=yab, in_=ya_d[j]), r=[("ya_d", j)], w=["yab"], dma=True)
                transpose_rows(yab, "yab", yaT, lambda half, j=j: ("yaT", j, half), j * 128)
            yaT_keys = [("yaT", j, half) for j in range(NJ) for half in range(2)]
            hTo_keys = [("hTo", j, half) for j in range(NJ) for half in range(2)]
            ybT_keys = [("ybT", hd) for hd in range(16)]
            srcs = [(w_branch_a, 0, yaT, yaT_keys), (w_branch_b, 0, ybT, ybT_keys),
                    (w_in, C_GA, hTo, hTo_keys), (w_in, C_GB, hTo, hTo_keys)]
            for piece in range(4):
                for tb in range(NTB):
                    n = min(512, NT - tb * 512)
                    for sub in range(4):
                        pass
                for pair in range(2):
                    res = []
                    wa = wload(srcs[pair][0][:, srcs[pair][1] + piece * 512:srcs[pair][1] + (piece + 1) * 512])
                    wg = wload(srcs[2 + pair][0][:, srcs[2 + pair][1] + piece * 512:srcs[2 + pair][1] + (piece + 1) * 512])
                    for sub in range(4):
                        nb = piece * 4 + sub
                        for tb in range(NTB):
                            n = min(512, NT - tb * 512)
                            pa, pak = bank()
                            pgt, pgk = bank()
                            act_in, act_keys = srcs[pair][2], srcs[pair][3]
                            for fc in range(16):
                                S.add("pe", lambda e, pa=pa, fc=fc, sub=sub, tb=tb, n=n, wa=wa, act_in=act_in: e.matmul(
                                    pa[:, 0:n], wa[0][:, fc, sub * 128:(sub + 1) * 128], act_in[:, fc, tb * 512:tb * 512 + n],
                                    start=(fc == 0), stop=(fc == 15)), r=act_keys + [wa[1]], w=[pak])
                            for fc in range(16):
                                S.add("pe", lambda e, pgt=pgt, fc=fc, sub=sub, tb=tb, n=n, wg=wg: e.matmul(
                                    pgt[:, 0:n], wg[0][:, fc, sub * 128:(sub + 1) * 128], hTo[:, fc, tb * 512:tb * 512 + n],
                                    start=(fc == 0), stop=(fc == 15)), r=hTo_keys + [wg[1]], w=[pgk])
                            si = cnt.setdefault("sg", 0) % 2
                            cnt["sg"] += 1
                            S.add("act", lambda e, pgt=pgt, si=si, n=n: e.activation(sg[si][:, 0:n], pgt[:, 0:n], AF.Sigmoid),
                                  r=[pgk], w=[f"sg{si}"])
                            mv = mT[:, nb, tb * 512:tb * 512 + n]
                            if pair == 0:
                                S.add("dve", lambda e, pa=pa, si=si, n=n, mv=mv: e.tensor_tensor(mv, pa[:, 0:n], sg[si][:, 0:n], ALU.mult),
                                      r=[pak, f"sg{si}"], w=[("mT", nb, tb)])
                            else:
                                S.add("dve", lambda e, pa=pa, si=si, n=n: e.tensor_tensor(tt[si][:, 0:n], pa[:, 0:n], sg[si][:, 0:n], ALU.mult),
                                      r=[pak, f"sg{si}"], w=[f"tt{si}"])
                                S.add("dve", lambda e, si=si, n=n, mv=mv: e.tensor_tensor(mv, mv, tt[si][:, 0:n], ALU.add),
                                      r=[f"tt{si}", ("mT", nb, tb)], w=[("mT", nb, tb)])
        S.barrier()
        x1 = at(0, F32, [8, D])
        if "C" in phases:
            mT_keys = [("mT", nb, tb) for nb in range(16) for tb in range(NTB)]
            for j in range(NJ):
                S.add("sp", lambda e, j=j: e.dma_start(out=x1[:, j, :], in_=x_own[j * 128:(j + 1) * 128, :]),
                      w=[("x1", j)], dma=True)
            for fb in range(4):
                wb, wk = wload(w_out[:, fb * 512:(fb + 1) * 512])
                for j in range(NJ):
                    pg, pk = bank()
                    for fc in range(16):
                        S.add("pe", lambda e, pg=pg, fc=fc, j=j, wb=wb: e.matmul(
                            pg, mT[:, fc, j * 128:(j + 1) * 128], wb[:, fc, :], start=(fc == 0), stop=(fc == 15)),
                            r=mT_keys + [wk], w=[pk])
                    xv = x1[:, j, fb * 512:(fb + 1) * 512]
                    S.add("dve", lambda e, pg=pg, xv=xv: e.tensor_tensor(xv, xv, pg, ALU.add), r=[pk, ("x1", j)], w=[("x1", j)])
            if dbg:
                for j in range(NJ):
                    S.add("sp", lambda e, j=j: e.dma_start(out=dbg_t["x1"][j * 128:(j + 1) * 128, :], in_=x1[:, j, :]),
                          r=[("x1", j)], dma=True)


        S.barrier()
        h2 = at(64, BF16, [8, D])
        h2T = at(96, BF16, [16, 1024])
        AR.off = R1 + 140 * 1024
        lg = AR.alloc(F32, [8, 32])
        top8 = AR.alloc(F32, [8, 8])
        mask = AR.alloc(F32, [8, 32])
        maskb = AR.alloc(BF16, [8, 32])
        Gt = AR.alloc(F32, [8, 32])
        pos = AR.alloc(F32, [8, 32])
        sm = AR.alloc(F32, [8, 4])
        wrb = AR.alloc(BF16, [16, 32])
        brb = AR.alloc(BF16, [128, 32])
        D_SMALL_END = AR.off
        posT = gain[1][:, 0:1024]
        GT = gain[1][:, 1024:2048]
        if "D" in phases:
            load_gain(0, g_ffn)
            S.add("pool", lambda e: e.dma_start(out=wrb, in_=w_router.rearrange("(c p) n -> p c n", p=128)), w=["wrb"], dma=True)
            S.add("pool", lambda e: e.dma_start(out=brb[0:1, :], in_=b_router.unsqueeze(0)), w=["brb"], dma=True)
            for j in range(NJ):
                xj = x1[:, j, :]
                S.add("act", lambda e, xj=xj, j=j: e.activation(junkB2, xj, AF.Square, accum_out=sm[:, j, 0:1]),
                      r=[("x1", j)], w=["junkB2", ("sm", j)])
                S.add("act", lambda e, j=j: e.activation(sm[:, j, 0:1], sm[:, j, 0:1], AF.Ln, bias=EPS, scale=1.0 / D), r=[("sm", j)], w=[("sm", j)])
                S.add("act", lambda e, j=j: e.activation(sm[:, j, 0:1], sm[:, j, 0:1], AF.Exp, scale=-0.5), r=[("sm", j)], w=[("sm", j)])
                S.add("dve", lambda e, xj=xj, j=j: e.scalar_tensor_tensor(h2[:, j, :], xj, sm[:, j, 0:1], gain[0], ALU.mult, ALU.mult),
                      r=[("x1", j), ("sm", j), "gain0"], w=[("h2", j)])
                transpose_rows(h2[:, j, :], ("h2", j), h2T, lambda half, j=j: ("h2T", j, half), j * 128)
            for j in range(NJ):
                pg, pk = bank()
                for fc in range(16):
                    S.add("pe", lambda e, pg=pg, fc=fc, j=j: e.matmul(pg[:, 0:32], h2T[:, fc, j * 128:(j + 1) * 128], wrb[:, fc, :],
                                                                  start=(fc == 0), stop=False),
                          r=[("h2T", j, fc // 8), "wrb"], w=[pk])
                S.add("pe", lambda e, pg=pg: e.matmul(pg[:, 0:32], onesb[0:1, :], brb[0:1, :], start=False, stop=True),
                      r=["onesb", "brb"], w=[pk])
                S.add("act", lambda e, pg=pg, j=j: e.copy(lg[:, j, :], pg[:, 0:32]), r=[pk], w=[("lg", j)])
                S.add("dve", lambda e, j=j: e.max(out=top8[:, j, :], in_=lg[:, j, :]), r=[("lg", j)], w=[("top8", j)])
                S.add("dve", lambda e, j=j: e.tensor_scalar(mask[:, j, :], lg[:, j, :], top8[:, j, 3:4], None, ALU.is_ge),
                      r=[("lg", j), ("top8", j)], w=[("mask", j)])
                S.add("dve", lambda e, j=j: e.tensor_scalar(sm[:, j, 1:2], top8[:, j, 0:1], -1.0, None, ALU.mult),
                      r=[("top8", j)], w=[("smb", j)])
                S.add("act", lambda e, j=j: e.activation(Gt[:, j, :], lg[:, j, :], AF.Exp, bias=sm[:, j, 1:2]),
                      r=[("lg", j), ("smb", j)], w=[("Gt", j)])
                S.add("dve", lambda e, j=j: e.tensor_tensor(Gt[:, j, :], Gt[:, j, :], mask[:, j, :], ALU.mult),
                      r=[("Gt", j), ("mask", j)], w=[("Gt", j)])
                S.add("dve", lambda e, j=j: e.reduce_sum(sm[:, j, 2:3], Gt[:, j, :], axis=AX.X), r=[("Gt", j)], w=[("sms", j)])
                S.add("dve", lambda e, j=j: e.reciprocal(sm[:, j, 2:3], sm[:, j, 2:3]), r=[("sms", j)], w=[("sms", j)])
                S.add("dve", lambda e, j=j: e.tensor_scalar(Gt[:, j, :], Gt[:, j, :], sm[:, j, 2:3], None, ALU.mult),
                      r=[("Gt", j), ("sms", j)], w=[("Gt", j)])
                S.add("dve", lambda e, j=j: e.tensor_copy(maskb[:, j, :], mask[:, j, :]), r=[("mask", j)], w=[("maskb", j)])
            for j in range(NJ):
                pg, pk = bank()
                for j2 in range(j):
                    S.add("pe", lambda e, pg=pg, j2=j2: e.matmul(pg[:, 0:32], onesb, maskb[:, j2, :], start=(j2 == 0), stop=False),
                          r=["onesb", ("maskb", j2)], w=[pk])
                S.add("pe", lambda e, pg=pg, j=j: e.matmul(pg[:, 0:32], SLTb, maskb[:, j, :], start=(j == 0), stop=True),
                      r=["SLTb", ("maskb", j)], w=[pk])
                S.add("act", lambda e, pg=pg, j=j: e.copy(pos[:, j, :], pg[:, 0:32]), r=[pk], w=[("pos", j)])
        S.barrier()
        ident32 = at(96, F32, [128, 128])
        XeT = AR.alloc(BF16, [16, CAP])
        actT = AR.alloc(BF16, [16, CAP])
        bgr_off = AR.off
        Oe = AR.alloc(BF16, [2, D])
        Sel = AR.alloc(BF16, [8, CAP])
        SelT = AR.alloc(BF16, [2, 1024])
        Gbs = AR.alloc(BF16, [128, 1024])
        bguT = AR.alloc(F32, [32, 32])
        oh = [AR.alloc(F32, [128, 128]) for _ in range(2)]
        glc = [AR.alloc(F32, [128, CAP]) for _ in range(2)]
        sgm = [AR.alloc(F32, [128, CAP]) for _ in range(2)]
        assert AR.off <= R1 + 140 * 1024, AR.off - R1
        _save = AR.off
        AR.off = bgr_off
        bgr = AR.alloc(F32, [128, 4096])
        AR.off = _save
        if "D" in phases:
            S.add("dve", lambda e: e.tensor_scalar(ident32, io_row[:, 0:128], pidx[:, 0:1], None, ALU.is_equal),
                  r=["io_row", "pidx"], w=["ident32"])
            for j in range(NJ):
                pg, pk = bank()
                S.add("pe", lambda e, pg=pg, j=j: e.transpose(pg[0:32, 0:128], pos[:, j, :], ident32), r=[("pos", j), "ident32"], w=[pk])
                S.add("pe", lambda e, pg=pg, j=j: e.transpose(pg[0:32, 128:256], Gt[:, j, :], ident32), r=[("Gt", j), "ident32"], w=[pk])
                S.add("act", lambda e, pg=pg, j=j: e.copy(posT[0:32, j * 128:(j + 1) * 128], pg[0:32, 0:128]), r=[pk], w=[("posT", j)])
                S.add("act", lambda e, pg=pg, j=j: e.copy(GT[0:32, j * 128:(j + 1) * 128], pg[0:32, 128:256]), r=[pk], w=[("GT", j)])
            S.add("sp", lambda e: e.dma_start(out=bgr[0:32, :], in_=b_gate_up[:, :]), w=["bgr"], dma=True)
            for half in range(2):
                pg, pk = bank()
                for c in range(16):
                    cc_ = half * 16 + c
                    S.add("pe", lambda e, pg=pg, c=c, cc_=cc_: e.transpose(pg[:, c * 32:(c + 1) * 32], bgr[0:32, cc_ * 128:(cc_ + 1) * 128],
                                                                       ident32[0:32, 0:32]), r=["bgr", "ident32"], w=[pk])
                S.add("act", lambda e, pg=pg, half=half: e.copy(bguT[:, half * 16:(half + 1) * 16, :],
                                                             pg.rearrange("p (c e) -> p c e", c=16)), r=[pk], w=[("bguT", half)])
            posT_keys = [("posT", j) for j in range(NJ)]
            GT_keys = [("GT", j) for j in range(NJ)]
            NTB = (NT + 511) // 512
            S.barrier()
            def moe_prep(ex):
                oi = ex % 2
                S.add("dve", lambda e, oi=oi, ex=ex: e.tensor_scalar(oh[oi][0:32, :], ones32[0:32, :], ident32[0:32, ex:ex + 1], None, ALU.mult),
                      r=["ones32", "ident32"], w=[f"oh{oi}"])
                for tb in range(NTB):
                    n = min(512, NT - tb * 512)
                    pgG, pgGk = bank()
                    S.add("pe", lambda e, pgG=pgG, oi=oi, tb=tb, n=n: e.matmul(pgG[:, 0:n], oh[oi][0:32, :], GT[0:32, tb * 512:tb * 512 + n],
                                                                          start=True, stop=True), r=[f"oh{oi}"] + GT_keys, w=[pgGk])
                    S.add("act", lambda e, pgG=pgG, tb=tb, n=n: e.copy(Gbs[:, tb * 512:tb * 512 + n], pgG[:, 0:n]), r=[pgGk], w=[("Gbs", tb)])
                    pgP, pgPk = bank()
                    S.add("pe", lambda e, pgP=pgP, oi=oi, tb=tb, n=n: e.matmul(pgP[:, 0:n], oh[oi][0:32, :], posT[0:32, tb * 512:tb * 512 + n],
                                                                          start=True, stop=True), r=[f"oh{oi}"] + posT_keys, w=[pgPk])
                    for stt in range(2):
                        S.add("dve", lambda e, pgP=pgP, stt=stt, tb=tb, n=n: e.scalar_tensor_tensor(
                            SelTs[ex % 2][:, stt, tb * 512:tb * 512 + n], pgP[:, 0:n], pidx[:, stt:stt + 1], Gbs[:, tb * 512:tb * 512 + n],
                            ALU.is_equal, ALU.mult), r=[pgPk, "pidx", ("Gbs", tb)], w=[("SelT", ex % 2, stt, tb)])
                for j in range(NJ):
                    S.add("dve", lambda e, j=j, ex=ex: e.tensor_scalar(
                        Sel[:, j, :], io_row[:, 0:CAP], pos[:, j, ex:ex + 1], mask[:, j, ex:ex + 1], ALU.is_equal, ALU.mult),
                        r=["io_row", ("pos", j), ("mask", j)], w=[("Sel", j)])
            def moe_gather(ex, fcs):
                for fc in fcs:
                    pg, pk = bank()
                    for j in range(NJ):
                        S.add("pe", lambda e, pg=pg, fc=fc, j=j: e.matmul(pg[:, 0:CAP], h2[:, j, fc * 128:(fc + 1) * 128], Sel[:, j, :],
                                                                      start=(j == 0), stop=(j == NJ - 1)),
                              r=[("h2", j), ("Sel", j)], w=[pk])
                    S.add("act", lambda e, pg=pg, fc=fc: e.copy(XeT[:, fc, :], pg[:, 0:CAP]), r=[pk], w=[("XeT", fc)])
            def moe_gu(ex, p_lo, p_hi):
                for piece in range(p_lo, p_hi):
                    wb, wk = wload(w_gate_up[ex][:, piece * 512:(piece + 1) * 512])
                    for sub in range(4):
                        nci = piece * 4 + sub
                        pg, pk = bank()
                        for fc in range(16):
                            S.add("pe", lambda e, pg=pg, fc=fc, sub=sub, wb=wb: e.matmul(
                                pg[:, 0:CAP], wb[:, fc, sub * 128:(sub + 1) * 128], XeT[:, fc, :], start=(fc == 0), stop=(fc == 15)),
                                r=XeT_keys + [wk], w=[pk])
                        gi = nci % 2
                        bias_ap = bguT[:, nci, ex:ex + 1]
                        if nci < 16:
                            S.add("dve", lambda e, pg=pg, gi=gi, bias_ap=bias_ap: e.tensor_scalar(
                                glc[gi], pg[:, 0:CAP], bias_ap, 7.0, ALU.add, ALU.min), r=[pk, ("bguT", nci // 16)], w=[f"glc{gi}"])
                            S.add("act", lambda e, gi=gi: e.activation(sgm[gi], glc[gi], AF.Sigmoid, scale=1.702), r=[f"glc{gi}"], w=[f"sgm{gi}"])
                            S.add("dve", lambda e, gi=gi, nci=nci: e.tensor_tensor(actT[:, nci, :], glc[gi], sgm[gi], ALU.mult),
                                  r=[f"glc{gi}", f"sgm{gi}"], w=[("actT", nci)])
                        else:
                            m_ = nci - 16
                            S.add("dve", lambda e, pg=pg, gi=gi, bias_ap=bias_ap: e.tensor_scalar(
                                glc[gi], pg[:, 0:CAP], bias_ap, 7.0, ALU.add, ALU.min), r=[pk, ("bguT", nci // 16)], w=[f"glc{gi}"])
                            S.add("dve", lambda e, gi=gi: e.tensor_scalar(sgm[gi], glc[gi], -7.0, 1.0, ALU.max, ALU.add),
                                  r=[f"glc{gi}"], w=[f"sgm{gi}"])
                            S.add("dve", lambda e, gi=gi, m_=m_: e.tensor_tensor(actT[:, m_, :], actT[:, m_, :], sgm[gi], ALU.mult),
                                  r=[("actT", m_), f"sgm{gi}"], w=[("actT", m_)])
            def moe_down(ex, fbs):
                for fb in fbs:
                    wb, wk = wload(w_down[ex][:, fb * 512:(fb + 1) * 512])
                    for stt in range(2):
                        pg, pk = bank()
                        for mc in range(16):
                            S.add("pe", lambda e, pg=pg, mc=mc, stt=stt, wb=wb: e.matmul(
                                pg, actT[:, mc, stt * 128:(stt + 1) * 128], wb[:, mc, :], start=(mc == 0), stop=(mc == 15)),
                                r=actT_keys + [wk], w=[pk])
                        S.add("act", lambda e, pg=pg, stt=stt, fb=fb: e.copy(Oe[:, stt, fb * 512:(fb + 1) * 512], pg), r=[pk], w=[("Oe", stt, fb)])
            def moe_scatter(ex, tiles):
                for (j, fb) in tiles:
                    if True:
                        pg, pk = bank()
                        for stt in range(2):
                            S.add("pe", lambda e, pg=pg, stt=stt, j=j, fb=fb: e.matmul(
                                pg, SelTs[ex % 2][:, stt, j * 128:(j + 1) * 128], Oe[:, stt, fb * 512:(fb + 1) * 512], start=(stt == 0), stop=(stt == 1)),
                                r=[("SelT", ex % 2, stt, j // 4), ("Oe", stt, fb)], w=[pk])
                        xv = x1[:, j, fb * 512:(fb + 1) * 512]
                        S.add("dve", lambda e, pg=pg, xv=xv: e.tensor_tensor(xv, xv, pg, ALU.add), r=[pk, ("x1", j)], w=[("x1", j)])

            XeT_keys = [("XeT", fc) for fc in range(16)]
            actT_keys = [("actT", m_) for m_ in range(16)]
            SelTs = [SelT, gain[0].bitcast(BF16)[:, 0:2048].rearrange("p (a b) -> p a b", a=2)]
            sc_tiles = [(j, fb) for j in range(NJ) for fb in range(4)]
            nsc = (len(sc_tiles) + 7) // 8
            moe_prep(0)
            moe_gather(0, range(16))
            for ex in range(NE):
                for k in range(8):
                    moe_gu(ex, k, k + 1)
                    if ex > 0:
                        moe_scatter(ex - 1, sc_tiles[k * nsc:(k + 1) * nsc])
                if ex + 1 < NE:
                    moe_prep(ex + 1)
                for fb in range(4):
                    moe_down(ex, [fb])
                    if ex + 1 < NE:
                        moe_gather(ex + 1, range(4 * fb, 4 * fb + 4))
            moe_scatter(NE - 1, sc_tiles)
            S.barrier()
            S.add("sp", lambda e: e.dma_start(out=bgr[0:32, 0:D], in_=b_down[:, :]), w=["bgr"], dma=True)
            for j in range(NJ):
                for fb in range(4):
                    pg, pk = bank()
                    S.add("pe", lambda e, pg=pg, j=j, fb=fb: e.matmul(pg, GT[0:32, j * 128:(j + 1) * 128], bgr[0:32, fb * 512:(fb + 1) * 512],
                                                                    start=True, stop=True), r=[("GT", j), "bgr"], w=[pk])
                    xv = x1[:, j, fb * 512:(fb + 1) * 512]
                    S.add("dve", lambda e, pg=pg, xv=xv: e.tensor_tensor(xv, xv, pg, ALU.add), r=[pk, ("x1", j)], w=[("x1", j)])
            if dbg:
                for j in range(NJ):
                    S.add("sp", lambda e, j=j: e.dma_start(out=dbg_t["x2"][j * 128:(j + 1) * 128, :], in_=x1[:, j, :]),
                          r=[("x1", j)], dma=True)

        S.barrier()
        h3T = at(64, BF16, [16, 1024])
        AR.off = R1 + 96 * 1024
        h3b = AR.alloc(BF16, [128, D])
        pT_ = AR.alloc(BF16, [2, 1024])
        pin = AR.alloc(F32, [128, 256])
        pinb = AR.alloc(BF16, [128, 256])
        u = AR.alloc(F32, [128, D])
        sgE = [AR.alloc(F32, [128, 512]) for _ in range(2)]
        obuf = AR.alloc(F32, [128, D])
        smE = AR.alloc(F32, [8, 4])
        if "E" in phases:
            load_gain(0, g_ple)
            for j in range(NJ):
                xj = x1[:, j, :]
                S.add("act", lambda e, xj=xj, j=j: e.activation(junkB2, xj, AF.Square, accum_out=smE[:, j, 0:1]),
                      r=[("x1", j)], w=["junkB2", ("smE", j)])
                S.add("act", lambda e, j=j: e.activation(smE[:, j, 0:1], smE[:, j, 0:1], AF.Ln, bias=EPS, scale=1.0 / D), r=[("smE", j)], w=[("smE", j)])
                S.add("act", lambda e, j=j: e.activation(smE[:, j, 0:1], smE[:, j, 0:1], AF.Exp, scale=-0.5), r=[("smE", j)], w=[("smE", j)])
                S.add("dve", lambda e, xj=xj, j=j: e.scalar_tensor_tensor(h3b, xj, smE[:, j, 0:1], gain[0], ALU.mult, ALU.mult),
                      r=[("x1", j), ("smE", j), "gain0"], w=["h3b"])
                transpose_rows(h3b, "h3b", h3T, lambda half, j=j: ("h3T", j, half), j * 128)
                S.add("sp", lambda e, j=j: e.dma_start(out=pin, in_=p_own[j * 128:(j + 1) * 128, :]), w=["pin"], dma=True)
                S.add("dve", lambda e: e.tensor_copy(pinb, pin), r=["pin"], w=["pinb"])
                pt, ptk = tbank()
                for k in range(2):
                    S.add("pe", lambda e, pt=pt, k=k: e.transpose(pt[:, k * 128:(k + 1) * 128], pinb[:, k * 128:(k + 1) * 128], ident),
                          r=["pinb", "ident"], w=[ptk])
                S.add("act", lambda e, pt=pt, j=j: e.copy(pT_[:, :, j * 128:(j + 1) * 128], pt[:, 0:256].rearrange("p (a b) -> p a b", a=2)),
                      r=[ptk], w=[("pT", j)])
            load_gain(0, g_ple_post)
            load_gain(1, g_final)
            for j in range(NJ):
                for fb in range(4):
                    wb, wk = wload(w_ple_gate[:, fb * 512:(fb + 1) * 512])
                    wp, wpk = wload(w_ple_proj[:, fb * 512:(fb + 1) * 512], rows=256)
                    pg, pk = bank()
                    for fc in range(16):
                        S.add("pe", lambda e, pg=pg, fc=fc, j=j, wb=wb: e.matmul(pg, h3T[:, fc, j * 128:(j + 1) * 128], wb[:, fc, :],
                                                                             start=(fc == 0), stop=(fc == 15)),
                              r=[("h3T", j, fc // 8), wk], w=[pk])
                    pp_, ppk = bank()
                    for k in range(2):
                        S.add("pe", lambda e, pp_=pp_, k=k, j=j, wp=wp: e.matmul(pp_, pT_[:, k, j * 128:(j + 1) * 128], wp[:, k, :],
                                                                             start=(k == 0), stop=(k == 1)),
                              r=[("pT", j), wpk], w=[ppk])
                    si = fb % 2
                    S.add("act", lambda e, pg=pg, si=si: e.activation(sgE[si], pg, AF.Sigmoid), r=[pk], w=[f"sgE{si}"])
                    S.add("dve", lambda e, pp_=pp_, si=si, fb=fb: e.tensor_tensor(u[:, fb * 512:(fb + 1) * 512], pp_, sgE[si], ALU.mult),
                          r=[ppk, f"sgE{si}"], w=[("u", fb)])
                ukeys = [("u", fb) for fb in range(4)]
                S.add("act", lambda e, j=j: e.activation(junkB2, u, AF.Square, accum_out=smE[:, j, 1:2]), r=ukeys, w=["junkB2", ("smE1", j)])
                S.add("act", lambda e, j=j: e.activation(smE[:, j, 1:2], smE[:, j, 1:2], AF.Ln, bias=EPS, scale=1.0 / D), r=[("smE1", j)], w=[("smE1", j)])
                S.add("act", lambda e, j=j: e.activation(smE[:, j, 1:2], smE[:, j, 1:2], AF.Exp, scale=-0.5), r=[("smE1", j)], w=[("smE1", j)])
                S.add("dve", lambda e, j=j: e.scalar_tensor_tensor(u, u, smE[:, j, 1:2], gain[0], ALU.mult, ALU.mult),
                      r=ukeys + [("smE1", j), "gain0"], w=ukeys)
                xj = x1[:, j, :]
                S.add("dve", lambda e, xj=xj: e.tensor_tensor(xj, xj, u, ALU.add), r=ukeys + [("x1", j)], w=[("x1", j)])
                S.add("act", lambda e, xj=xj, j=j: e.activation(junkB2, xj, AF.Square, accum_out=smE[:, j, 2:3]), r=[("x1", j)], w=["junkB2", ("smE2", j)])
                S.add("act", lambda e, j=j: e.activation(smE[:, j, 2:3], smE[:, j, 2:3], AF.Ln, bias=EPS, scale=1.0 / D), r=[("smE2", j)], w=[("smE2", j)])
                S.add("act", lambda e, j=j: e.activation(smE[:, j, 2:3], smE[:, j, 2:3], AF.Exp, scale=-0.5), r=[("smE2", j)], w=[("smE2", j)])
                S.add("dve", lambda e, xj=xj, j=j: e.scalar_tensor_tensor(obuf, xj, smE[:, j, 2:3], gain[1], ALU.mult, ALU.mult),
                      r=[("x1", j), ("smE2", j), "gain1"], w=["obuf"])
                S.add("sp", lambda e, j=j: e.dma_start(out=out[j * 128:(j + 1) * 128, :], in_=obuf), r=["obuf"], dma=True)
                out_dmas.append(len(S.ops) - 1)

        S.barrier()
        outs = [i for i, o in enumerate(S.ops) if o["dma"]]
        S.wait_all("sp", outs[-32:] + out_dmas)
        S.emit()
    return nc


def make_inputs(inputs, core):
    b, q = core // 4, core % 4
    x = np.asarray(inputs["x"], dtype=np.float32)
    p = np.asarray(inputs["p"], dtype=np.float32)
    own = np.concatenate([np.arange((4 * j + q) * 128, (4 * j + q + 1) * 128) for j in range(8)])
    m = {}
    pad = 3 - q
    xa = np.zeros((SEQ, D), np.float32)
    xa[pad * 128:] = x[b][:SEQ - pad * 128]
    m["x_all"] = xa
    m["x_own"] = np.ascontiguousarray(x[b][own])
    m["p_own"] = np.ascontiguousarray(p[0, b][own])
    ms = np.zeros((128, 4), np.float32)
    for kb in range(4):
        ms[:, kb] = 1.0 if kb >= pad else 0.0
    m["msel"] = ms
    am = np.zeros((128, 4, 128), np.float32)
    am[:, 3, :] = (np.arange(128)[:, None] < np.arange(128)[None, :]).astype(np.float32)
    m["amask"] = am
    for k in ["w_in", "conv_w", "conv_b", "dt_bias", "a_log", "d_skip", "ssd_norm_w", "w_branch_a", "w_branch_b",
              "w_out", "g_mix", "g_ffn", "w_router", "b_router", "w_gate_up", "b_gate_up", "w_down", "b_down",
              "g_ple", "w_ple_gate", "w_ple_proj", "g_ple_post"]:
        m[k] = np.ascontiguousarray(np.asarray(inputs[k], dtype=np.float32)[0])
    m["g_final"] = np.ascontiguousarray(np.asarray(inputs["g_final"], dtype=np.float32))
    return m, own


def kernel(**inputs):
    nc = build()
    in_maps = []
    owns = []
    for c in range(8):
        m, own = make_inputs(inputs, c)
        in_maps.append(m)
        owns.append(own)
    res = run_bass_kernel_spmd(nc, in_maps, core_ids=list(range(8)))
    outp = np.zeros((2, SEQ, D), np.float32)
    for c in range(8):
        outp[c // 4, owns[c]] = res.results[c]["out"]
    return outp
```

```python
import numpy as np
from contextlib import ExitStack
import concourse.bass as bass
import concourse.mybir as mybir
from concourse.bass_utils import run_bass_kernel_spmd

F32 = mybir.dt.float32
BF16 = mybir.dt.bfloat16
AF = mybir.ActivationFunctionType
ALU = mybir.AluOpType
AX = mybir.AxisListType

ENGS = ("pe", "act", "dve", "pool", "sp")
NDMASEM = 8
D = 2048
SEQ = 4096
NE = 32
CAP = 256
EPS = 1e-6
IN_DIM = 15392
C_Z, C_XBC, C_DT, C_Q, C_K, C_V, C_GA, C_GB = 0, 2048, 5120, 5152, 7200, 9248, 11296, 13344


class Sched:
    def __init__(self, nc, stack):
        self.nc = nc
        self.ops = []
        self.last_w = {}
        self.rd_eng = {}
        self.rd_dma = {}
        self.stack = stack

    def add(self, eng, fn, r=(), w=(), dma=False, prio=None):
        oid = len(self.ops)
        deps = set()
        for k in r:
            d = self.last_w.get(k)
            if d is not None:
                deps.add(d)
        for k in w:
            d = self.last_w.get(k)
            if d is not None:
                deps.add(d)
            for d in self.rd_eng.get(k, {}).values():
                deps.add(d)
            for d in self.rd_dma.get(k, ()):
                deps.add(d)
        for k in w:
            self.last_w[k] = oid
            self.rd_eng[k] = {}
            self.rd_dma[k] = []
        for k in r:
            if dma:
                self.rd_dma.setdefault(k, []).append(oid)
            else:
                self.rd_eng.setdefault(k, {})[eng] = oid
        deps.discard(oid)
        self.ops.append(dict(eng=eng, fn=fn, deps=deps, dma=dma, prio=(oid if prio is None else prio)))
        return oid

    def wait_all(self, eng, ids):
        self.ops.append(dict(eng=eng, fn=None, deps=set(ids), dma=False, prio=len(self.ops)))

    def barrier(self):
        self.nbar = getattr(self, "nbar", 0) + 1
        last = {}
        dmas = []
        for i, o in enumerate(self.ops):
            if o["fn"] is None:
                continue
            if o["dma"]:
                dmas.append(i)
            else:
                last[o["eng"]] = i
        ids = list(last.values()) + dmas[-64:]
        for e in ENGS:
            self.wait_all(e, ids)

    def emit(self):
        nc = self.nc
        ops = self.ops

        def skip(p, o):
            return (not p["dma"]) and (not o["dma"]) and p["eng"] == o["eng"] and p["eng"] == "pe"

        need = [False] * len(ops)
        for o in ops:
            for d in o["deps"]:
                if not skip(ops[d], o):
                    need[d] = True
        per_eng = {e: [] for e in ENGS}
        for i, o in enumerate(ops):
            per_eng[o["eng"]].append(i)
        for e in ENGS:
            per_eng[e].sort(key=lambda i: (ops[i]["prio"], i))
        cnt = {e: 0 for e in ENGS}
        dcnt = {e: 0 for e in ENGS}
        for e in ENGS:
            for i in per_eng[e]:
                o = ops[i]
                o["sig"] = None
                if o["fn"] is None:
                    continue
                if o["dma"]:
                    n = dcnt[e]
                    dcnt[e] += 1
                    o["sig"] = ("d", e, n % NDMASEM, 16 * (n // NDMASEM + 1))
                elif need[i]:
                    cnt[e] += 1
                    o["sig"] = ("c", e, 0, cnt[e])
        sems = {}
        for e in ENGS:
            sems[("c", e, 0)] = self.stack.enter_context(nc.semaphore(f"c_{e}"))
            if dcnt[e] > 0:
                for k in range(NDMASEM):
                    sems[("d", e, k)] = self.stack.enter_context(nc.semaphore(f"d_{e}_{k}"))

        def run_engine(ename, handle):
            known = {}
            for i in per_eng[ename]:
                o = ops[i]
                waits = {}
                for d in o["deps"]:
                    p = ops[d]
                    if skip(p, o):
                        continue
                    s = p["sig"]
                    if waits.get(s[:3], 0) < s[3]:
                        waits[s[:3]] = s[3]
                for key, v in waits.items():
                    if known.get(key, 0) >= v:
                        continue
                    known[key] = v
                    handle.wait_ge(sems[key], v)
                if o["fn"] is None:
                    continue
                ins = o["fn"](handle)
                s = o["sig"]
                if s is not None:
                    ins.then_inc(sems[s[:3]], 16 if s[0] == "d" else 1)

        block = self.stack.enter_context(nc.Block())

        @block.tensor
        def _(e):
            run_engine("pe", e)

        @block.scalar
        def _(e):
            run_engine("act", e)

        @block.vector
        def _(e):
            run_engine("dve", e)

        @block.gpsimd
        def _(e):
            run_engine("pool", e)

        @block.sync
        def _(e):
            run_engine("sp", e)


class Arena:
    def __init__(self, t, nbytes):
        self.t = t
        self.nbytes = nbytes
        self.off = 0

    def alloc(self, dt, shape):
        shape = list(shape)
        if shape[0] == 128 and len(shape) >= 2:
            shape = shape[1:]
        n = int(np.prod(shape))
        sz = n * (4 if dt == F32 else 2)
        sz = (sz + 31) // 32 * 32
        assert self.off + sz <= self.nbytes, ("arena overflow", self.off, sz)
        v = self.t[:, self.off // 4:(self.off + sz) // 4]
        self.off += sz
        if dt != F32:
            v = v.bitcast(dt)
        v = v[:, 0:n]
        if len(shape) == 2:
            v = v.rearrange("p (a b) -> p a b", a=shape[0])
        elif len(shape) == 3:
            v = v.rearrange("p (a b c) -> p a b c", a=shape[0], b=shape[1])
        return v


def build(NG=8, dbg=False, phases="ABCDE"):
    nc = bass.Bass("TRN2", target_bir_lowering=False)
    dram_in = lambda n, s: nc.dram_tensor(n, list(s), F32, kind="ExternalInput").ap()
    x_all = dram_in("x_all", [SEQ, D])
    x_own = dram_in("x_own", [1024, D])
    p_own = dram_in("p_own", [1024, 256])
    msel = dram_in("msel", [128, 4])
    amask = dram_in("amask", [128, 4, 128])
    w_in = dram_in("w_in", [D, IN_DIM])
    conv_w = dram_in("conv_w", [4, 3072])
    conv_b = dram_in("conv_b", [3072])
    dt_bias = dram_in("dt_bias", [32])
    a_log = dram_in("a_log", [32])
    d_skip = dram_in("d_skip", [32])
    ssd_norm_w = dram_in("ssd_norm_w", [D])
    w_branch_a = dram_in("w_branch_a", [D, D])
    w_branch_b = dram_in("w_branch_b", [D, D])
    w_out = dram_in("w_out", [D, D])
    g_mix = dram_in("g_mix", [D])
    g_ffn = dram_in("g_ffn", [D])
    w_router = dram_in("w_router", [D, NE])
    b_router = dram_in("b_router", [NE])
    w_gate_up = dram_in("w_gate_up", [NE, D, 2 * D])
    b_gate_up = dram_in("b_gate_up", [NE, 2 * D])
    w_down = dram_in("w_down", [NE, D, D])
    b_down = dram_in("b_down", [NE, D])
    g_ple = dram_in("g_ple", [D])
    w_ple_gate = dram_in("w_ple_gate", [D, D])
    w_ple_proj = dram_in("w_ple_proj", [256, D])
    g_ple_post = dram_in("g_ple_post", [D])
    g_final = dram_in("g_final", [D])
    out = nc.dram_tensor("out", [1024, D], F32, kind="ExternalOutput").ap()
    kT_d = nc.dram_tensor("kT_d", [16, 128, SEQ], BF16).ap()
    v_d = nc.dram_tensor("v_d", [16, 128, 32, 128], BF16).ap()
    ya_d = nc.dram_tensor("ya_d", [8, 128, D], BF16).ap()
    dbg_t = {}
    if dbg:
        dbg_t["ya"] = nc.dram_tensor("dbg_ya", [NG, 128, D], F32, kind="ExternalOutput").ap()
        dbg_t["yb"] = nc.dram_tensor("dbg_yb", [128, 16, 1024], F32, kind="ExternalOutput").ap()
        dbg_t["x1"] = nc.dram_tensor("dbg_x1", [1024, D], F32, kind="ExternalOutput").ap()
        dbg_t["q"] = nc.dram_tensor("dbg_q", [128, 16, 1024], F32, kind="ExternalOutput").ap()
        dbg_t["x2"] = nc.dram_tensor("dbg_x2", [1024, D], F32, kind="ExternalOutput").ap()

    st = ExitStack()
    with st:
        S = Sched(nc, st)
        ARN = 207 * 1024
        arena_t = st.enter_context(nc.sbuf_tensor("arena", [128, ARN // 4], F32))
        AR = Arena(arena_t, ARN)
        pgs = [st.enter_context(nc.psum_tensor(f"pg{i}", [128, 512], F32))[:] for i in range(6)]
        pts = [st.enter_context(nc.psum_tensor(f"pt{i}", [128, 1024], BF16))[:] for i in range(2)]
        cnt = {"pg": 0, "pt": 0, "wb": 0, "npg": 6}

        def bank():
            i = cnt["pg"] % cnt["npg"]
            cnt["pg"] += 1
            return pgs[i], f"pg{i}"

        def tbank():
            i = cnt["pt"] % 2
            cnt["pt"] += 1
            return pts[i], f"pt{i}"

        out_dmas = []

        ident = AR.alloc(BF16, [128, 128])
        io_row = AR.alloc(F32, [128, 256])
        pidx = AR.alloc(F32, [128, 2])
        UI = AR.alloc(F32, [128, 128])
        SL = AR.alloc(F32, [128, 128])
        ones32 = AR.alloc(F32, [128, 128])
        UIb = AR.alloc(BF16, [128, 128])
        onesb = AR.alloc(BF16, [128, 128])
        SLTb = AR.alloc(BF16, [128, 128])
        junkB2 = AR.alloc(BF16, [128, D])
        msel_t = AR.alloc(F32, [128, 4])
        amask_t = AR.alloc(F32, [128, 4, 128])
        gain = [AR.alloc(F32, [128, D]) for _ in range(2)]
        CONST_END = AR.off

        S.add("pool", lambda e: e.iota(io_row, pattern=[[1, 256]], base=0, channel_multiplier=0,
                                       allow_small_or_imprecise_dtypes=True), w=["io_row"])
        S.add("pool", lambda e: e.iota(pidx[:, 0:1], pattern=[[0, 1]], base=0, channel_multiplier=1,
                                       allow_small_or_imprecise_dtypes=True), w=["pidx"])
        S.add("pool", lambda e: e.iota(pidx[:, 1:2], pattern=[[0, 1]], base=128, channel_multiplier=1,
                                       allow_small_or_imprecise_dtypes=True), w=["pidx"])
        S.add("dve", lambda e: e.tensor_scalar(ones32, io_row[:, 0:128], pidx[:, 0:1], None, ALU.is_equal),
              r=["io_row", "pidx"], w=["ones32"])
        S.add("dve", lambda e: e.tensor_copy(ident, ones32), r=["ones32"], w=["ident"])
        S.add("dve", lambda e: e.tensor_scalar(UI, io_row[:, 0:128], pidx[:, 0:1], None, ALU.is_ge),
              r=["io_row", "pidx"], w=["UI"])
        S.add("dve", lambda e: e.tensor_scalar(SL, io_row[:, 0:128], pidx[:, 0:1], None, ALU.is_lt),
              r=["io_row", "pidx"], w=["SL"])
        S.add("dve", lambda e: e.tensor_scalar(UIb, io_row[:, 0:128], pidx[:, 0:1], None, ALU.is_le),
              r=["io_row", "pidx"], w=["UIb"])
        S.add("dve", lambda e: e.tensor_scalar(SLTb, io_row[:, 0:128], pidx[:, 0:1], None, ALU.is_gt),
              r=["io_row", "pidx"], w=["SLTb"])
        S.add("pool", lambda e: e.memset(ones32, 1.0), r=["ident"], w=["ones32"])
        S.add("pool", lambda e: e.memset(onesb, 1.0), w=["onesb"])
        S.add("sp", lambda e: e.dma_start(out=msel_t, in_=msel[:, :]), w=["msel"], dma=True)
        S.add("sp", lambda e: e.dma_start(out=amask_t, in_=amask[:, :, :]), w=["amask"], dma=True)

        def load_gain(slot, src):
            S.add("sp", lambda e: e.dma_start(out=gain[slot], in_=src.partition_broadcast(128)),
                  w=[f"gain{slot}"], dma=True)

        NWB = 2
        wbs = [AR.alloc(BF16, [16, 512]) for _ in range(NWB)]
        WB_END = AR.off

        def wload(src2d, ncols=512, rows=D):
            i = cnt["wb"] % NWB
            cnt["wb"] += 1
            nch = rows // 128
            dst = wbs[i][:, 0:nch, 0:ncols]
            pos_now = len(S.ops)
            pr = cnt.get("wmark")
            S.add("pool", lambda e: e.dma_start(out=dst, in_=src2d.rearrange("(c p) n -> p c n", p=128)),
                  w=[f"wb{i}"], dma=True)
            cnt["wmark"] = pos_now
            return wbs[i], f"wb{i}"

        def rmsnorm_rows(xt, xkey, gslot, outb, outkey, ss, sskey, junk, junkkey):
            S.add("act", lambda e: e.activation(junk, xt, AF.Square, accum_out=ss), r=[xkey], w=[junkkey, sskey])
            S.add("act", lambda e: e.activation(ss, ss, AF.Ln, bias=EPS, scale=1.0 / D), r=[sskey], w=[sskey])
            S.add("act", lambda e: e.activation(ss, ss, AF.Exp, scale=-0.5), r=[sskey], w=[sskey])
            S.add("dve", lambda e: e.scalar_tensor_tensor(outb, xt, ss, gain[gslot], ALU.mult, ALU.mult),
                  r=[xkey, sskey, f"gain{gslot}"], w=[outkey])

        def transpose_rows(srcb, srckey, dstT, dstkey_fn, tok0, ntok=128, nfc=16):
            for half in range(nfc // 8):
                pt, ptk = tbank()
                for k in range(8):
                    fc = half * 8 + k
                    S.add("pe", lambda e, fc=fc, k=k, pt=pt: e.transpose(pt[:, k * 128:(k + 1) * 128],
                                                                        srcb[:, fc * 128:(fc + 1) * 128], ident),
                          r=[srckey, "ident"], w=[ptk])
                S.add("act", lambda e, half=half, pt=pt: e.copy(
                    dstT[:, half * 8:(half + 1) * 8, tok0:tok0 + ntok],
                    pt.rearrange("p (a b) -> p a b", a=8)),
                    r=[ptk], w=[dstkey_fn(half)])

        A0 = AR.off
        xt = AR.alloc(F32, [128, D])
        hb = AR.alloc(BF16, [128, D])
        hT = AR.alloc(BF16, [16, 512])
        zs = AR.alloc(BF16, [4, D])
        xtm = AR.alloc(BF16, [4, D])
        Btm = AR.alloc(BF16, [4, 512])
        BT = AR.alloc(BF16, [4, 512])
        CT = AR.alloc(BF16, [4, 512])
        raw = [AR.alloc(F32, [128, 515]) for _ in range(2)]
        ctmp = [AR.alloc(F32, [128, 512]) for _ in range(2)]
        xcb = [AR.alloc(BF16, [128, 512]) for _ in range(2)]
        halo = AR.alloc(F32, [24, 3])
        convw_t = AR.alloc(F32, [4, 24])
        convb_t = AR.alloc(F32, [128, 24])
        wdt = AR.alloc(BF16, [16, 32])
        dtb_b = AR.alloc(F32, [128, 32])
        negA_b = AR.alloc(F32, [128, 32])
        dskip_b = AR.alloc(F32, [128, 32])
        ss = AR.alloc(F32, [128, 8])
        dt_all = AR.alloc(F32, [4, 32])
        a_all = AR.alloc(F32, [4, 32])
        dtr = AR.alloc(F32, [128, 32])
        acs = AR.alloc(F32, [128, 64])
        eacs = AR.alloc(F32, [128, 32])
        dst = AR.alloc(F32, [128, 32])
        cdec = AR.alloc(F32, [128, 32])
        Xdt = AR.alloc(BF16, [128, D])
        Xds = AR.alloc(BF16, [128, D])
        CBm = AR.alloc(F32, [4, 128])
        lh = [AR.alloc(F32, [8, 128]) for _ in range(2)]
        dec = [AR.alloc(F32, [4, 128]) for _ in range(2)]
        Mb = [AR.alloc(BF16, [4, 128]) for _ in range(2)]
        yo = [AR.alloc(F32, [128, 512]) for _ in range(2)]
        yy = [AR.alloc(F32, [128, 512]) for _ in range(2)]
        ssg = AR.alloc(F32, [128, 4])
        rm = AR.alloc(F32, [128, 4])
        state = AR.alloc(F32, [128, D])
        prevb = AR.alloc(BF16, [128, D])
        kst = [AR.alloc(BF16, [128, 512]) for _ in range(2)]
        vst = [AR.alloc(BF16, [128, 512]) for _ in range(2)]
        ya_cur = AR.alloc(BF16, [128, D])
        A_END = AR.off

        if "A" in phases:
            load_gain(0, g_mix)
            load_gain(1, ssd_norm_w)
            for k in range(4):
                S.add("sp", lambda e, k=k: e.dma_start(out=convw_t[:, k, :], in_=conv_w[k].rearrange("(c p) -> p c", p=128),
                                                       allow_slow_non_contiguous=True), w=[("convw", k)], dma=True)
            S.add("sp", lambda e: e.dma_start(out=convb_t, in_=conv_b.rearrange("(c p) -> p c", p=128),
                                              allow_slow_non_contiguous=True), w=["convb"], dma=True)
            S.add("pool", lambda e: e.dma_start(out=wdt, in_=w_in[:, C_DT:C_DT + 32].rearrange("(c p) n -> p c n", p=128)),
                  w=["wdt"], dma=True)
            S.add("sp", lambda e: e.dma_start(out=dtb_b, in_=dt_bias.partition_broadcast(128)), w=["dtb"], dma=True)
            S.add("sp", lambda e: e.dma_start(out=negA_b, in_=a_log.partition_broadcast(128)), w=["negA"], dma=True)
            S.add("sp", lambda e: e.dma_start(out=dskip_b, in_=d_skip.partition_broadcast(128)), w=["dskip"], dma=True)
            S.add("act", lambda e: e.activation(negA_b, negA_b, AF.Exp), r=["negA"], w=["negA"])
            S.add("dve", lambda e: e.tensor_scalar(negA_b, negA_b, -1.0, None, ALU.mult), r=["negA"], w=["negA"])
            S.add("pool", lambda e: e.memset(halo, 0.0), w=["halo"])
            S.add("pool", lambda e: e.memset(state, 0.0), w=["state"])
            S.add("pool", lambda e: e.memset(prevb, 0.0), w=["prevb"])

            own_flag = [False]

            def SY(*a_, **k_):
                if own_flag[0]:
                    S.add(*a_, **k_)

            for G in range(NG):
                for cc in range(4):
                    c = 4 * G + cc
                    S.add("sp", lambda e, c=c: e.dma_start(out=xt, in_=x_all[c * 128:(c + 1) * 128, :]), w=["xt"], dma=True)
                    rmsnorm_rows(xt, "xt", 0, hb, "hb", ss[:, 0:1], "ss0", junkB2, "junkB2")
                    transpose_rows(hb, "hb", hT, lambda half, cc=cc: ("hT", cc, half), cc * 128)
                hTkeys = [("hT", cc, half) for cc in range(4) for half in range(2)]
                for jb in range(8):
                    col0 = (C_Z + jb * 512) if jb < 4 else (C_V + (jb - 4) * 512)
                    wb, wk = wload(w_in[:, col0:col0 + 512])
                    for cc in ((3,) if jb < 4 else range(4)):
                        c = 4 * G + cc
                        pg, pk = bank()
                        for fc in range(16):
                            S.add("pe", lambda e, pg=pg, fc=fc, cc=cc, wb=wb: e.matmul(
                                pg, hT[:, fc, cc * 128:(cc + 1) * 128], wb[:, fc, :], start=(fc == 0), stop=(fc == 15)),
                                r=[("hT", cc, fc // 8), wk], w=[pk])
                        if jb < 4:
                            S.add("act", lambda e, pg=pg, cc=cc, jb=jb: e.activation(
                                zs[:, cc, jb * 512:(jb + 1) * 512], pg, AF.Silu), r=[pk], w=[("zs", cc, jb)])
                        else:
                            vi = cnt.setdefault("vst", 0) % 2
                            cnt["vst"] += 1
                            S.add("act", lambda e, pg=pg, vi=vi: e.copy(vst[vi], pg), r=[pk], w=[f"vst{vi}"])
                            S.add("sp", lambda e, vi=vi, c=c, jb=jb: e.dma_start(
                                out=v_d[(jb - 4) * 4:(jb - 3) * 4, :, c, :].rearrange("h p d -> p h d"),
                                in_=vst[vi].rearrange("p (h d) -> p h d", h=4)),
                                r=[f"vst{vi}"], w=[("v_d", c)], dma=True)
                for cc in range(4):
                    pg, pk = bank()
                    for fc in range(16):
                        S.add("pe", lambda e, pg=pg, fc=fc, cc=cc: e.matmul(
                            pg[:, 0:32], hT[:, fc, cc * 128:(cc + 1) * 128], wdt[:, fc, :], start=(fc == 0), stop=(fc == 15)),
                            r=[("hT", cc, fc // 8), "wdt"], w=[pk])
                    S.add("dve", lambda e, pg=pg: e.tensor_tensor(dtr, pg[:, 0:32], dtb_b, ALU.add), r=[pk, "dtb"], w=["dtr"])
                    S.add("act", lambda e: e.activation(dtr, dtr, AF.Exp), r=["dtr"], w=["dtr"])
                    S.add("act", lambda e, cc=cc: e.activation(dt_all[:, cc, :], dtr, AF.Ln, bias=1.0), r=["dtr"], w=[("dt", cc)])
                    S.add("dve", lambda e, cc=cc: e.tensor_tensor(a_all[:, cc, :], dt_all[:, cc, :], negA_b, ALU.mult),
                          r=[("dt", cc), "negA"], w=[("a", cc)])
                for piece in range(10):
                    col0 = (C_XBC + piece * 512) if piece < 6 else (C_K + (piece - 6) * 512)
                    wb, wk = wload(w_in[:, col0:col0 + 512])
                    for sub in range(4):
                        i = piece * 4 + sub
                        pg, pk = bank()
                        for fc in range(16):
                            S.add("pe", lambda e, pg=pg, fc=fc, sub=sub, wb=wb: e.matmul(
                                pg, wb[:, fc, sub * 128:(sub + 1) * 128], hT[:, fc, :], start=(fc == 0), stop=(fc == 15)),
                                r=hTkeys + [wk], w=[pk])
                        if i < 24:
                            ri = i % 2
                            rw, ct, xc = raw[ri], ctmp[ri], xcb[ri]
                            S.add("act", lambda e, pg=pg, rw=rw: e.copy(rw[:, 3:515], pg), r=[pk], w=[f"raw{ri}"])
                            S.add("act", lambda e, rw=rw, i=i: e.copy(rw[:, 0:3], halo[:, i, :]),
                                  r=[("halo", i)], w=[f"rawh{ri}"])
                            rk = [f"raw{ri}", f"rawh{ri}"] + [("convw", k) for k in range(4)]
                            S.add("dve", lambda e, rw=rw, ct=ct, i=i: e.tensor_scalar(
                                ct, rw[:, 0:512], convw_t[:, 0, i:i + 1], None, ALU.mult), r=rk, w=[f"ct{ri}"])
                            for k in (1, 2, 3):
                                S.add("dve", lambda e, rw=rw, ct=ct, i=i, k=k: e.scalar_tensor_tensor(
                                    ct, rw[:, k:512 + k], convw_t[:, k, i:i + 1], ct, ALU.mult, ALU.add),
                                    r=rk + [f"ct{ri}"], w=[f"ct{ri}"])
                            S.add("act", lambda e, rw=rw, i=i: e.copy(halo[:, i, :], rw[:, 512:515]),
                                  r=[f"raw{ri}"], w=[("halo", i)])
                            if i < 16:
                                S.add("act", lambda e, ct=ct, xc=xc, i=i: e.activation(xc, ct, AF.Silu, bias=convb_t[:, i:i + 1]),
                                      r=[f"ct{ri}", "convb"], w=[f"xc{ri}"])
                                src, sk = xc, f"xc{ri}"
                            elif i < 20:
                                g = i - 16
                                S.add("act", lambda e, ct=ct, g=g, i=i: e.activation(BT[:, g, :], ct, AF.Silu, bias=convb_t[:, i:i + 1]),
                                      r=[f"ct{ri}", "convb"], w=[("BT", g)])
                                src, sk = BT[:, g, :], ("BT", g)
                            else:
                                g = i - 20
                                S.add("act", lambda e, ct=ct, g=g, i=i: e.activation(CT[:, g, :], ct, AF.Silu, bias=convb_t[:, i:i + 1]),
                                      r=[f"ct{ri}", "convb"], w=[("CT", g)])
                                src = None
                            if src is not None:
                                pt, ptk = tbank()
                                for cc in range(4):
                                    S.add("pe", lambda e, pt=pt, cc=cc, src=src: e.transpose(
                                        pt[:, cc * 128:(cc + 1) * 128], src[:, cc * 128:(cc + 1) * 128], ident),
                                        r=[sk, "ident"], w=[ptk])
                                if i < 16:
                                    S.add("act", lambda e, pt=pt, i=i: e.copy(
                                        xtm[:, :, i * 128:(i + 1) * 128], pt[:, 0:512].rearrange("p (a b) -> p a b", a=4)),
                                        r=[ptk], w=[("xtm", i)])
                                else:
                                    S.add("act", lambda e, pt=pt, g=g: e.copy(
                                        Btm[:, :, g * 128:(g + 1) * 128], pt[:, 0:512].rearrange("p (a b) -> p a b", a=4)),
                                        r=[ptk], w=[("Btm", g)])
                        else:
                            hd = i - 24
                            ki = hd % 2
                            S.add("act", lambda e, pg=pg, ki=ki: e.copy(kst[ki], pg), r=[pk], w=[f"kst{ki}"])
                            S.add("sp", lambda e, ki=ki, hd=hd, G=G: e.dma_start(
                                out=kT_d[hd, :, G * 512:(G + 1) * 512], in_=kst[ki]),
                                r=[f"kst{ki}"], w=[("kT_d", hd)], dma=True)
                xtm_keys = [("xtm", i) for i in range(16)]
                for cc in range(4):
                    own_flag[0] = (cc == 3)
                    pg, pk = bank()
                    S.add("pe", lambda e, pg=pg, cc=cc: e.matmul(pg[:, 0:32], UI, a_all[:, cc, :], start=True, stop=True),
                          r=[("a", cc), "UI"], w=[pk])
                    S.add("pe", lambda e, pg=pg, cc=cc: e.matmul(pg[:, 32:64], ones32, a_all[:, cc, :], start=True, stop=True),
                          r=[("a", cc), "ones32"], w=[pk])
                    S.add("act", lambda e, pg=pg: e.copy(acs, pg[:, 0:64]), r=[pk], w=["acs"])
                    SY("act", lambda e: e.activation(eacs, acs[:, 0:32], AF.Exp), r=["acs"], w=["eacs"])
                    S.add("act", lambda e: e.activation(cdec, acs[:, 32:64], AF.Exp), r=["acs"], w=["cdec"])
                    S.add("dve", lambda e: e.tensor_tensor(dst, acs[:, 32:64], acs[:, 0:32], ALU.subtract), r=["acs"], w=["dst"])
                    S.add("act", lambda e: e.activation(dst, dst, AF.Exp), r=["dst"], w=["dst"])
                    if G == 0 and cc < 3:
                        S.add("dve", lambda e, cc=cc: e.tensor_scalar(dst, dst, msel_t[:, cc:cc + 1], None, ALU.mult),
                              r=["dst", "msel"], w=["dst"])
                    x3 = xtm[:, cc, :].rearrange("p (h d) -> p h d", h=32)
                    S.add("dve", lambda e, cc=cc, x3=x3: e.tensor_tensor(
                        Xdt.rearrange("p (h d) -> p h d", h=32), x3,
                        dt_all[:, cc, :].unsqueeze(2).to_broadcast([128, 32, 64]), ALU.mult),
                        r=xtm_keys + [("dt", cc)], w=["Xdt"])
                    S.add("dve", lambda e: e.tensor_tensor(
                        Xds.rearrange("p (h d) -> p h d", h=32), Xdt.rearrange("p (h d) -> p h d", h=32),
                        dst.unsqueeze(2).to_broadcast([128, 32, 64]), ALU.mult), r=["Xdt", "dst"], w=["Xds"])
                    pg, pk = bank()
                    for g in range(4):
                        SY("pe", lambda e, pg=pg, g=g, cc=cc: e.matmul(
                            pg[:, g * 128:(g + 1) * 128], BT[:, g, cc * 128:(cc + 1) * 128], CT[:, g, cc * 128:(cc + 1) * 128],
                            start=True, stop=True), r=[("BT", g), ("CT", g)], w=[pk])
                    SY("dve", lambda e, pg=pg: e.tensor_tensor(
                        CBm, pg.rearrange("p (g l) -> p g l", g=4), UI.unsqueeze(1).to_broadcast([128, 4, 128]), ALU.mult),
                        r=[pk, "UI"], w=["CBm"])
                    for g in range(4):
                        gi = g % 2
                        SY("dve", lambda e, g=g, gi=gi, cc=cc: e.tensor_tensor(
                            lh[gi], SL.unsqueeze(1).to_broadcast([128, 8, 128]),
                            a_all[:, cc, g * 8:(g + 1) * 8].unsqueeze(2).to_broadcast([128, 8, 128]), ALU.mult),
                            r=["SL", ("a", cc)], w=[f"lh{gi}"])
                        pY, pYk = bank()
                        for half in range(2):
                            pseg, psk = bank()
                            for j in range(4):
                                SY("pe", lambda e, pseg=pseg, j=j, gi=gi, half=half: e.matmul(
                                    pseg[:, j * 128:(j + 1) * 128], lh[gi][:, half * 4 + j, :], UI, start=True, stop=True),
                                    r=[f"lh{gi}", "UI"], w=[psk])
                            SY("act", lambda e, pseg=pseg, half=half: e.activation(
                                dec[half], pseg.rearrange("p (j l) -> p j l", j=4), AF.Exp), r=[psk], w=[f"dec{half}"])
                            SY("dve", lambda e, half=half, g=g: e.tensor_tensor(
                                Mb[half], dec[half], CBm[:, g:g + 1, :].to_broadcast([128, 4, 128]), ALU.mult),
                                r=[f"dec{half}", "CBm"], w=[f"Mb{half}"])
                            for j in range(4):
                                h = g * 8 + half * 4 + j
                                SY("pe", lambda e, pY=pY, half=half, j=j, h=h: e.matmul(
                                    pY[:, (half * 4 + j) * 64:(half * 4 + j + 1) * 64], Mb[half][:, j, :],
                                    Xdt[:, h * 64:(h + 1) * 64], start=True, stop=True),
                                    r=[f"Mb{half}", "Xdt"], w=[pYk])
                        pYo, pYok = bank()
                        SY("pe", lambda e, pYo=pYo, g=g, cc=cc: e.matmul(
                            pYo, CT[:, g, cc * 128:(cc + 1) * 128], prevb[:, g * 512:(g + 1) * 512], start=True, stop=True),
                            r=[("CT", g), ("prevb", g)], w=[pYok])
                        SY("act", lambda e, pYo=pYo, gi=gi: e.copy(yo[gi], pYo), r=[pYok], w=[f"yo{gi}"])
                        SY("dve", lambda e, gi=gi, g=g: e.tensor_tensor(
                            yo[gi].rearrange("p (h d) -> p h d", h=8), yo[gi].rearrange("p (h d) -> p h d", h=8),
                            eacs[:, g * 8:(g + 1) * 8].unsqueeze(2).to_broadcast([128, 8, 64]), ALU.mult),
                            r=[f"yo{gi}", "eacs"], w=[f"yo{gi}"])
                        SY("dve", lambda e, gi=gi, pY=pY: e.tensor_tensor(yy[gi], pY, yo[gi], ALU.add),
                              r=[pYk, f"yo{gi}"], w=[f"yy{gi}"])
                        SY("dve", lambda e, gi=gi, g=g, cc=cc: e.tensor_tensor(
                            yo[gi].rearrange("p (h d) -> p h d", h=8),
                            xtm[:, cc, g * 512:(g + 1) * 512].rearrange("p (h d) -> p h d", h=8),
                            dskip_b[:, g * 8:(g + 1) * 8].unsqueeze(2).to_broadcast([128, 8, 64]), ALU.mult),
                            r=xtm_keys + ["dskip", f"yy{gi}"], w=[f"yo{gi}"])
                        SY("dve", lambda e, gi=gi: e.tensor_tensor(yy[gi], yy[gi], yo[gi], ALU.add),
                              r=[f"yy{gi}", f"yo{gi}"], w=[f"yy{gi}"])
                        SY("dve", lambda e, gi=gi, g=g, cc=cc: e.tensor_tensor(
                            yy[gi], yy[gi], zs[:, cc, g * 512:(g + 1) * 512], ALU.mult),
                            r=[f"yy{gi}", ("zs", cc, g)], w=[f"yy{gi}"])
                        SY("act", lambda e, gi=gi, g=g: e.activation(yo[gi], yy[gi], AF.Square, accum_out=ssg[:, g:g + 1]),
                              r=[f"yy{gi}"], w=[f"yo{gi}", ("ssg", g)])
                        SY("act", lambda e, g=g: e.activation(rm[:, g:g + 1], ssg[:, g:g + 1], AF.Ln, bias=EPS, scale=1.0 / 512),
                              r=[("ssg", g)], w=[("rm", g)])
                        SY("act", lambda e, g=g: e.activation(rm[:, g:g + 1], rm[:, g:g + 1], AF.Exp, scale=-0.5),
                              r=[("rm", g)], w=[("rm", g)])
                        SY("dve", lambda e, gi=gi, g=g: e.tensor_tensor(
                            yy[gi], yy[gi], gain[1][:, g * 512:(g + 1) * 512], ALU.mult),
                            r=[f"yy{gi}", "gain1"], w=[f"yy{gi}"])
                        dstv = ya_cur[:, g * 512:(g + 1) * 512]
                        if True:
                            SY("dve", lambda e, dstv=dstv, g=g, gi=gi: e.tensor_scalar(
                                dstv, yy[gi], rm[:, g:g + 1], None, ALU.mult),
                                r=[f"yy{gi}", ("rm", g)], w=[("ya", g)])
                        else:
                            SY("dve", lambda e, dstv=dstv, g=g, gi=gi: e.scalar_tensor_tensor(
                                dstv, yy[gi], rm[:, g:g + 1], dstv, ALU.mult, ALU.add),
                                r=[f"yy{gi}", ("rm", g), ("ya", g)], w=[("ya", g)])
                        pSt, pStk = bank()
                        S.add("pe", lambda e, pSt=pSt, g=g, cc=cc: e.matmul(
                            pSt, Btm[:, cc, g * 128:(g + 1) * 128], Xds[:, g * 512:(g + 1) * 512], start=True, stop=True),
                            r=[("Btm", g), "Xds"], w=[pStk])
                        sv = state[:, g * 512:(g + 1) * 512]
                        S.add("dve", lambda e, sv=sv, g=g: e.tensor_tensor(
                            sv.rearrange("p (h d) -> p h d", h=8), sv.rearrange("p (h d) -> p h d", h=8),
                            cdec[:, g * 8:(g + 1) * 8].unsqueeze(2).to_broadcast([128, 8, 64]), ALU.mult),
                            r=[("state", g), "cdec"], w=[("state", g)])
                        S.add("dve", lambda e, sv=sv, pSt=pSt: e.tensor_tensor(sv, sv, pSt, ALU.add),
                              r=[("state", g), pStk], w=[("state", g)])
                        S.add("act", lambda e, sv=sv, g=g: e.copy(prevb[:, g * 512:(g + 1) * 512], sv),
                              r=[("state", g)], w=[("prevb", g)])
                S.add("sp", lambda e, G=G: e.dma_start(out=ya_d[G], in_=ya_cur),
                      r=[("ya", g) for g in range(4)], w=[("ya_d", G)], dma=True)
                if dbg:
                    S.add("pool", lambda e, G=G: e.dma_start(out=dbg_t["ya"][G], in_=ya_cur),
                          r=[("ya", g) for g in range(4)], dma=True)
                    out_dmas.append(len(S.ops) - 1)


        NJ = NG
        NT = NJ * 128
        R1 = WB_END

        def at(off_kb, dt, shape):
            AR.off = R1 + off_kb * 1024
            return AR.alloc(dt, shape)

        S.barrier()
        hTo = at(0, BF16, [16, 1024])
        qT = at(32, BF16, [16, 1024])
        ybT = at(64, BF16, [16, 1024])
        kTh = [at(96 + 8 * i, BF16, [128, SEQ]) for i in range(2)]
        vh = [at(112 + 8 * i, BF16, [32, 128]) for i in range(2)]
        xo = at(64, F32, [128, D])
        hbo = at(72, BF16, [128, D])
        junkB = at(76, BF16, [128, D])
        AR.off = R1 + 128 * 1024
        ssb = AR.alloc(F32, [128, 8])
        Et = [AR.alloc(F32, [128, 512]) for _ in range(2)]
        spb = [AR.alloc(BF16, [128, 512]) for _ in range(2)]
        expc = [AR.alloc(F32, [128, 512]) for _ in range(2)]
        wbf = [AR.alloc(BF16, [128, 512]) for _ in range(2)]
        Trow = [AR.alloc(BF16, [128, 512]) for _ in range(2)]

        def own_hT(gslot, src_tiles_fn, dstT, keyname):
            for j in range(NJ):
                src_tiles_fn(j)
                rmsnorm_rows(xo, "xo", gslot, hbo, "hbo", ssb[:, 0:1], "ssb0", junkB, "junkB")
                transpose_rows(hbo, "hbo", dstT, lambda half, j=j: (keyname, j, half), j * 128)

        if "B" in phases:
            load_gain(0, g_mix)
            own_hT(0, lambda j: S.add("sp", lambda e: e.dma_start(out=xo, in_=x_own[j * 128:(j + 1) * 128, :]),
                                      w=["xo"], dma=True), hTo, "hTo")
            hTo_keys = [("hTo", j, half) for j in range(NJ) for half in range(2)]
            NTB = (NT + 511) // 512
            for piece in range(4):
                wb, wk = wload(w_in[:, C_Q + piece * 512:C_Q + (piece + 1) * 512])
                for sub in range(4):
                    hd = piece * 4 + sub
                    for tb in range(NTB):
                        n = min(512, NT - tb * 512)
                        pg, pk = bank()
                        for fc in range(16):
                            S.add("pe", lambda e, pg=pg, fc=fc, sub=sub, wb=wb, tb=tb, n=n: e.matmul(
                                pg[:, 0:n], wb[:, fc, sub * 128:(sub + 1) * 128], hTo[:, fc, tb * 512:tb * 512 + n],
                                start=(fc == 0), stop=(fc == 15)), r=hTo_keys + [wk], w=[pk])
                        S.add("act", lambda e, pg=pg, hd=hd, tb=tb, n=n: e.mul(
                            qT[:, hd, tb * 512:tb * 512 + n], pg[:, 0:n], float(128 ** -0.5)), r=[pk], w=[("qT", hd)])

            if dbg:
                S.add("pool", lambda e: e.dma_start(out=dbg_t["q"][:, :, 0:NT], in_=qT[:, :, 0:NT]),
                      r=[("qT", hd) for hd in range(16)], dma=True)
            NKB = 4 * NJ
            cnt["npg"] = 4
            for hp in range(8):
                for ci in range(2):
                    hd = hp * 2 + ci
                    S.add("sp", lambda e, hd=hd, ci=ci: e.dma_start(out=kTh[ci][:, 0:NKB * 128], in_=kT_d[hd, :, 0:NKB * 128]),
                          r=[("kT_d", hd)], w=[f"kTh{ci}"], dma=True)
                    S.add("sp", lambda e, hd=hd, ci=ci: e.dma_start(
                        out=vh[ci][:, 0:NKB, :], in_=v_d[hd, :, 0:NKB, :]),
                        r=[("v_d", c) for c in range(NKB)], w=[f"vh{ci}"], dma=True)
                for Q in range((NJ + 3) // 4):
                    j0 = 4 * Q
                    W = min(4, NJ - j0)
                    WN = W * 128
                    pys = []
                    for ci in range(2):
                        S.add("dve", lambda e, ci=ci: e.memset(Trow[ci], 0.0), w=[f"Trow{ci}"])
                        pys.append((pgs[4 + ci], f"pg{4 + ci}"))
                        S.add("pe", lambda e, ci=ci, WN=WN: e.matmul(pgs[4 + ci][:, 0:WN], onesb[0:1, :], Trow[ci][0:1, 0:WN], start=True, stop=False),
                              r=["onesb", f"Trow{ci}"], w=[f"pg{4 + ci}"])
                    kmax = 4 * (j0 + W - 1) + 3
                    def emit_qk(kb, j0=j0, WN=WN, hp=hp):
                        c0 = (max(j0, kb // 4) - j0) * 128
                        res = []
                        for ci in range(2):
                            hd = hp * 2 + ci
                            pz, pzk = bank()
                            res.append((pz, pzk))
                            S.add("pe", lambda e, pz=pz, ci=ci, hd=hd, kb=kb, c0=c0, WN=WN, j0=j0: e.matmul(
                                pz[:, c0:WN], kTh[ci][:, kb * 128:(kb + 1) * 128], qT[:, hd, j0 * 128 + c0:j0 * 128 + WN],
                                start=True, stop=True), r=[f"kTh{ci}", ("qT", hd)], w=[pzk])
                        return res

                    nxt_pz = emit_qk(kmax)
                    for kb in range(kmax, -1, -1):
                        jmin = max(j0, (kb - 3 + 3) // 4)
                        c0 = (jmin - j0) * 128
                        jm = kb // 4
                        pzs = nxt_pz
                        for ci in range(2):
                            hd = hp * 2 + ci
                            pz, pzk = pzs[ci]
                            S.add("act", lambda e, pz=pz, ci=ci, c0=c0, WN=WN: e.activation(Et[ci][:, c0:WN], pz[:, c0:WN], AF.Exp),
                                  r=[pzk], w=[f"E{ci}"])
                            if kb % 4 == 3 and j0 <= jm < j0 + W:
                                cm = (jm - j0) * 128
                                S.add("dve", lambda e, ci=ci, cm=cm: e.tensor_tensor(
                                    Et[ci][:, cm:cm + 128], Et[ci][:, cm:cm + 128], amask_t[:, 3, :], ALU.mult),
                                    r=[f"E{ci}", "amask"], w=[f"E{ci}"])
                            if kb < 3:
                                S.add("dve", lambda e, ci=ci, c0=c0, WN=WN, kb=kb: e.tensor_scalar(
                                    Et[ci][:, c0:WN], Et[ci][:, c0:WN], msel_t[:, kb:kb + 1], None, ALU.mult),
                                    r=[f"E{ci}", "msel"], w=[f"E{ci}"])
                            S.add("act", lambda e, ci=ci, c0=c0, WN=WN: e.activation(spb[ci][:, c0:WN], Et[ci][:, c0:WN], AF.Ln, bias=1.0),
                                  r=[f"E{ci}"], w=[f"sp{ci}"])
                        pcs = []
                        for ci in range(2):
                            pc, pck = bank()
                            pcs.append((pc, pck))
                            S.add("pe", lambda e, pc=pc, ci=ci, c0=c0, WN=WN: e.matmul(
                                pc[:, c0:WN], UIb, spb[ci][:, c0:WN], start=True, stop=False), r=["UIb", f"sp{ci}"], w=[pck])
                            S.add("pe", lambda e, pc=pc, ci=ci, c0=c0, WN=WN: e.matmul(
                                pc[:, c0:WN], onesb[0:1, :], Trow[ci][0:1, c0:WN], start=False, stop=True), r=["onesb", f"Trow{ci}"], w=[pck])
                            S.add("act", lambda e, pc=pc, ci=ci, c0=c0, WN=WN: e.copy(Trow[ci][0:1, c0:WN], pc[0:1, c0:WN]),
                                  r=[pck], w=[f"Trow{ci}"])
                            S.add("act", lambda e, pc=pc, ci=ci, c0=c0, WN=WN: e.activation(expc[ci][:, c0:WN], pc[:, c0:WN], AF.Exp, scale=-1.0),
                                  r=[pck], w=[f"expc{ci}"])
                            S.add("dve", lambda e, ci=ci, c0=c0, WN=WN: e.tensor_tensor(
                                wbf[ci][:, c0:WN], Et[ci][:, c0:WN], expc[ci][:, c0:WN], ALU.mult),
                                r=[f"E{ci}", f"expc{ci}"], w=[f"w{ci}"])
                        nxt_pz = emit_qk(kb - 1) if kb > 0 else None
                        for ci in range(2):
                            py, pyk = pys[ci]
                            newj = (kb - 3) // 4 if (kb - 3) % 4 == 0 and j0 <= (kb - 3) // 4 < j0 + W else None
                            last = (kb == 0)
                            if newj is not None:
                                cn = (newj - j0) * 128
                                S.add("pe", lambda e, py=py, ci=ci, kb=kb, cn=cn, last=last: e.matmul(
                                    py[:, cn:cn + 128], vh[ci][:, kb, :], wbf[ci][:, cn:cn + 128], start=False, stop=last),
                                    r=[f"vh{ci}", f"w{ci}"], w=[pyk])
                                c1 = cn + 128
                            else:
                                c1 = c0
                            if c1 < WN:
                                S.add("pe", lambda e, py=py, ci=ci, kb=kb, c1=c1, WN=WN, last=last: e.matmul(
                                    py[:, c1:WN], vh[ci][:, kb, :], wbf[ci][:, c1:WN], start=False, stop=last),
                                    r=[f"vh{ci}", f"w{ci}"], w=[pyk])
                    for ci in range(2):
                        hd = hp * 2 + ci
                        py, pyk = pys[ci]
                        S.add("act", lambda e, py=py, hd=hd, WN=WN, j0=j0: e.copy(ybT[:, hd, j0 * 128:j0 * 128 + WN], py[:, 0:WN]),
                              r=[pyk], w=[("ybT", hd)])
            cnt["npg"] = 6
            if dbg:
                S.add("pool", lambda e: e.dma_start(out=dbg_t["yb"][:, :, 0:NT], in_=ybT[:, :, 0:NT]),
                      r=[("ybT", hd) for hd in range(16)], dma=True)

        S.barrier()
        yaT = at(32, BF16, [16, 1024])
        mT = at(96, BF16, [16, 1024])
        AR.off = R1 + 128 * 1024
        yab = AR.alloc(BF16, [128, D])
        sg = [AR.alloc(F32, [128, 512]) for _ in range(2)]
        tt = [AR.alloc(F32, [128, 512]) for _ in range(2)]
        NTB = (NT + 511) // 512
        if "C" in phases:
            for j in range(NJ):
                S.add("sp", lambda e, j=j: e.dma_start(out=yab, in_=ya_d[j]), r=[("ya_d", j)], w=["yab"], dma=True)
                transpose_rows(yab, "yab", yaT, lambda half, j=j: ("yaT", j, half), j * 128)
            yaT_keys = [("yaT", j, half) for j in range(NJ) for half in range(2)]
            hTo_keys = [("hTo", j, half) for j in range(NJ) for half in range(2)]
            ybT_keys = [("ybT", hd) for hd in range(16)]
            srcs = [(w_branch_a, 0, yaT, yaT_keys), (w_branch_b, 0, ybT, ybT_keys),
                    (w_in, C_GA, hTo, hTo_keys), (w_in, C_GB, hTo, hTo_keys)]
            for piece in range(4):
                for tb in range(NTB):
                    n = min(512, NT - tb * 512)
                    for sub in range(4):
                        pass
                for pair in range(2):
                    res = []
                    wa = wload(srcs[pair][0][:, srcs[pair][1] + piece * 512:srcs[pair][1] + (piece + 1) * 512])
                    wg = wload(srcs[2 + pair][0][:, srcs[2 + pair][1] + piece * 512:srcs[2 + pair][1] + (piece + 1) * 512])
                    for sub in range(4):
                        nb = piece * 4 + sub
                        for tb in range(NTB):
                            n = min(512, NT - tb * 512)
                            pa, pak = bank()
                            pgt, pgk = bank()
                            act_in, act_keys = srcs[pair][2], srcs[pair][3]
                            for fc in range(16):
                                S.add("pe", lambda e, pa=pa, fc=fc, sub=sub, tb=tb, n=n, wa=wa, act_in=act_in: e.matmul(
                                    pa[:, 0:n], wa[0][:, fc, sub * 128:(sub + 1) * 128], act_in[:, fc, tb * 512:tb * 512 + n],
                                    start=(fc == 0), stop=(fc == 15)), r=act_keys + [wa[1]], w=[pak])
                            for fc in range(16):
                                S.add("pe", lambda e, pgt=pgt, fc=fc, sub=sub, tb=tb, n=n, wg=wg: e.matmul(
                                    pgt[:, 0:n], wg[0][:, fc, sub * 128:(sub + 1) * 128], hTo[:, fc, tb * 512:tb * 512 + n],
                                    start=(fc == 0), stop=(fc == 15)), r=hTo_keys + [wg[1]], w=[pgk])
                            si = cnt.setdefault("sg", 0) % 2
                            cnt["sg"] += 1
                            S.add("act", lambda e, pgt=pgt, si=si, n=n: e.activation(sg[si][:, 0:n], pgt[:, 0:n], AF.Sigmoid),
                                  r=[pgk], w=[f"sg{si}"])
                            mv = mT[:, nb, tb * 512:tb * 512 + n]
                            if pair == 0:
                                S.add("dve", lambda e, pa=pa, si=si, n=n, mv=mv: e.tensor_tensor(mv, pa[:, 0:n], sg[si][:, 0:n], ALU.mult),
                                      r=[pak, f"sg{si}"], w=[("mT", nb, tb)])
                            else:
                                S.add("dve", lambda e, pa=pa, si=si, n=n: e.tensor_tensor(tt[si][:, 0:n], pa[:, 0:n], sg[si][:, 0:n], ALU.mult),
                                      r=[pak, f"sg{si}"], w=[f"tt{si}"])
                                S.add("dve", lambda e, si=si, n=n, mv=mv: e.tensor_tensor(mv, mv, tt[si][:, 0:n], ALU.add),
                                      r=[f"tt{si}", ("mT", nb, tb)], w=[("mT", nb, tb)])
        S.barrier()
        x1 = at(0, F32, [8, D])
        if "C" in phases:
            mT_keys = [("mT", nb, tb) for nb in range(16) for tb in range(NTB)]
            for j in range(NJ):
                S.add("sp", lambda e, j=j: e.dma_start(out=x1[:, j, :], in_=x_own[j * 128:(j + 1) * 128, :]),
                      w=[("x1", j)], dma=True)
            for fb in range(4):
                wb, wk = wload(w_out[:, fb * 512:(fb + 1) * 512])
                for j in range(NJ):
                    pg, pk = bank()
                    for fc in range(16):
                        S.add("pe", lambda e, pg=pg, fc=fc, j=j, wb=wb: e.matmul(
                            pg, mT[:, fc, j * 128:(j + 1) * 128], wb[:, fc, :], start=(fc == 0), stop=(fc == 15)),
                            r=mT_keys + [wk], w=[pk])
                    xv = x1[:, j, fb * 512:(fb + 1) * 512]
                    S.add("dve", lambda e, pg=pg, xv=xv: e.tensor_tensor(xv, xv, pg, ALU.add), r=[pk, ("x1", j)], w=[("x1", j)])
            if dbg:
                for j in range(NJ):
                    S.add("sp", lambda e, j=j: e.dma_start(out=dbg_t["x1"][j * 128:(j + 1) * 128, :], in_=x1[:, j, :]),
                          r=[("x1", j)], dma=True)


        S.barrier()
        h2 = at(64, BF16, [8, D])
        h2T = at(96, BF16, [16, 1024])
        AR.off = R1 + 140 * 1024
        lg = AR.alloc(F32, [8, 32])
        top8 = AR.alloc(F32, [8, 8])
        mask = AR.alloc(F32, [8, 32])
        maskb = AR.alloc(BF16, [8, 32])
        Gt = AR.alloc(F32, [8, 32])
        pos = AR.alloc(F32, [8, 32])
        sm = AR.alloc(F32, [8, 4])
        wrb = AR.alloc(BF16, [16, 32])
        brb = AR.alloc(BF16, [128, 32])
        D_SMALL_END = AR.off
        posT = gain[1][:, 0:1024]
        GT = gain[1][:, 1024:2048]
        if "D" in phases:
            load_gain(0, g_ffn)
            S.add("pool", lambda e: e.dma_start(out=wrb, in_=w_router.rearrange("(c p) n -> p c n", p=128)), w=["wrb"], dma=True)
            S.add("pool", lambda e: e.dma_start(out=brb[0:1, :], in_=b_router.unsqueeze(0)), w=["brb"], dma=True)
            for j in range(NJ):
                xj = x1[:, j, :]
                S.add("act", lambda e, xj=xj, j=j: e.activation(junkB2, xj, AF.Square, accum_out=sm[:, j, 0:1]),
                      r=[("x1", j)], w=["junkB2", ("sm", j)])
                S.add("act", lambda e, j=j: e.activation(sm[:, j, 0:1], sm[:, j, 0:1], AF.Ln, bias=EPS, scale=1.0 / D), r=[("sm", j)], w=[("sm", j)])
                S.add("act", lambda e, j=j: e.activation(sm[:, j, 0:1], sm[:, j, 0:1], AF.Exp, scale=-0.5), r=[("sm", j)], w=[("sm", j)])
                S.add("dve", lambda e, xj=xj, j=j: e.scalar_tensor_tensor(h2[:, j, :], xj, sm[:, j, 0:1], gain[0], ALU.mult, ALU.mult),
                      r=[("x1", j), ("sm", j), "gain0"], w=[("h2", j)])
                transpose_rows(h2[:, j, :], ("h2", j), h2T, lambda half, j=j: ("h2T", j, half), j * 128)
            for j in range(NJ):
                pg, pk = bank()
                for fc in range(16):
                    S.add("pe", lambda e, pg=pg, fc=fc, j=j: e.matmul(pg[:, 0:32], h2T[:, fc, j * 128:(j + 1) * 128], wrb[:, fc, :],
                                                                  start=(fc == 0), stop=False),
                          r=[("h2T", j, fc // 8), "wrb"], w=[pk])
                S.add("pe", lambda e, pg=pg: e.matmul(pg[:, 0:32], onesb[0:1, :], brb[0:1, :], start=False, stop=True),
                      r=["onesb", "brb"], w=[pk])
                S.add("act", lambda e, pg=pg, j=j: e.copy(lg[:, j, :], pg[:, 0:32]), r=[pk], w=[("lg", j)])
                S.add("dve", lambda e, j=j: e.max(out=top8[:, j, :], in_=lg[:, j, :]), r=[("lg", j)], w=[("top8", j)])
                S.add("dve", lambda e, j=j: e.tensor_scalar(mask[:, j, :], lg[:, j, :], top8[:, j, 3:4], None, ALU.is_ge),
                      r=[("lg", j), ("top8", j)], w=[("mask", j)])
                S.add("dve", lambda e, j=j: e.tensor_scalar(sm[:, j, 1:2], top8[:, j, 0:1], -1.0, None, ALU.mult),
                      r=[("top8", j)], w=[("smb", j)])
                S.add("act", lambda e, j=j: e.activation(Gt[:, j, :], lg[:, j, :], AF.Exp, bias=sm[:, j, 1:2]),
                      r=[("lg", j), ("smb", j)], w=[("Gt", j)])
                S.add("dve", lambda e, j=j: e.tensor_tensor(Gt[:, j, :], Gt[:, j, :], mask[:, j, :], ALU.mult),
                      r=[("Gt", j), ("mask", j)], w=[("Gt", j)])
                S.add("dve", lambda e, j=j: e.reduce_sum(sm[:, j, 2:3], Gt[:, j, :], axis=AX.X), r=[("Gt", j)], w=[("sms", j)])
                S.add("dve", lambda e, j=j: e.reciprocal(sm[:, j, 2:3], sm[:, j, 2:3]), r=[("sms", j)], w=[("sms", j)])
                S.add("dve", lambda e, j=j: e.tensor_scalar(Gt[:, j, :], Gt[:, j, :], sm[:, j, 2:3], None, ALU.mult),
                      r=[("Gt", j), ("sms", j)], w=[("Gt", j)])
                S.add("dve", lambda e, j=j: e.tensor_copy(maskb[:, j, :], mask[:, j, :]), r=[("mask", j)], w=[("maskb", j)])
            for j in range(NJ):
                pg, pk = bank()
                for j2 in range(j):
                    S.add("pe", lambda e, pg=pg, j2=j2: e.matmul(pg[:, 0:32], onesb, maskb[:, j2, :], start=(j2 == 0), stop=False),
                          r=["onesb", ("maskb", j2)], w=[pk])
                S.add("pe", lambda e, pg=pg, j=j: e.matmul(pg[:, 0:32], SLTb, maskb[:, j, :], start=(j == 0), stop=True),
                      r=["SLTb", ("maskb", j)], w=[pk])
                S.add("act", lambda e, pg=pg, j=j: e.copy(pos[:, j, :], pg[:, 0:32]), r=[pk], w=[("pos", j)])
        S.barrier()
        ident32 = at(96, F32, [128, 128])
        XeT = AR.alloc(BF16, [16, CAP])
        actT = AR.alloc(BF16, [16, CAP])
        bgr_off = AR.off
        Oe = AR.alloc(BF16, [2, D])
        Sel = AR.alloc(BF16, [8, CAP])
        SelT = AR.alloc(BF16, [2, 1024])
        Gbs = AR.alloc(BF16, [128, 1024])
        bguT = AR.alloc(F32, [32, 32])
        oh = [AR.alloc(F32, [128, 128]) for _ in range(2)]
        glc = [AR.alloc(F32, [128, CAP]) for _ in range(2)]
        sgm = [AR.alloc(F32, [128, CAP]) for _ in range(2)]
        assert AR.off <= R1 + 140 * 1024, AR.off - R1
        _save = AR.off
        AR.off = bgr_off
        bgr = AR.alloc(F32, [128, 4096])
        AR.off = _save
        if "D" in phases:
            S.add("dve", lambda e: e.tensor_scalar(ident32, io_row[:, 0:128], pidx[:, 0:1], None, ALU.is_equal),
                  r=["io_row", "pidx"], w=["ident32"])
            for j in range(NJ):
                pg, pk = bank()
                S.add("pe", lambda e, pg=pg, j=j: e.transpose(pg[0:32, 0:128], pos[:, j, :], ident32), r=[("pos", j), "ident32"], w=[pk])
                S.add("pe", lambda e, pg=pg, j=j: e.transpose(pg[0:32, 128:256], Gt[:, j, :], ident32), r=[("Gt", j), "ident32"], w=[pk])
                S.add("act", lambda e, pg=pg, j=j: e.copy(posT[0:32, j * 128:(j + 1) * 128], pg[0:32, 0:128]), r=[pk], w=[("posT", j)])
                S.add("act", lambda e, pg=pg, j=j: e.copy(GT[0:32, j * 128:(j + 1) * 128], pg[0:32, 128:256]), r=[pk], w=[("GT", j)])
            S.add("sp", lambda e: e.dma_start(out=bgr[0:32, :], in_=b_gate_up[:, :]), w=["bgr"], dma=True)
            for half in range(2):
                pg, pk = bank()
                for c in range(16):
                    cc_ = half * 16 + c
                    S.add("pe", lambda e, pg=pg, c=c, cc_=cc_: e.transpose(pg[:, c * 32:(c + 1) * 32], bgr[0:32, cc_ * 128:(cc_ + 1) * 128],
                                                                       ident32[0:32, 0:32]), r=["bgr", "ident32"], w=[pk])
                S.add("act", lambda e, pg=pg, half=half: e.copy(bguT[:, half * 16:(half + 1) * 16, :],
                                                             pg.rearrange("p (c e) -> p c e", c=16)), r=[pk], w=[("bguT", half)])
            posT_keys = [("posT", j) for j in range(NJ)]
            GT_keys = [("GT", j) for j in range(NJ)]
            NTB = (NT + 511) // 512
            S.barrier()
            def moe_prep(ex):
                oi = ex % 2
                S.add("dve", lambda e, oi=oi, ex=ex: e.tensor_scalar(oh[oi][0:32, :], ones32[0:32, :], ident32[0:32, ex:ex + 1], None, ALU.mult),
                      r=["ones32", "ident32"], w=[f"oh{oi}"])
                for tb in range(NTB):
                    n = min(512, NT - tb * 512)
                    pgG, pgGk = bank()
                    S.add("pe", lambda e, pgG=pgG, oi=oi, tb=tb, n=n: e.matmul(pgG[:, 0:n], oh[oi][0:32, :], GT[0:32, tb * 512:tb * 512 + n],
                                                                          start=True, stop=True), r=[f"oh{oi}"] + GT_keys, w=[pgGk])
                    S.add("act", lambda e, pgG=pgG, tb=tb, n=n: e.copy(Gbs[:, tb * 512:tb * 512 + n], pgG[:, 0:n]), r=[pgGk], w=[("Gbs", tb)])
                    pgP, pgPk = bank()
                    S.add("pe", lambda e, pgP=pgP, oi=oi, tb=tb, n=n: e.matmul(pgP[:, 0:n], oh[oi][0:32, :], posT[0:32, tb * 512:tb * 512 + n],
                                                                          start=True, stop=True), r=[f"oh{oi}"] + posT_keys, w=[pgPk])
                    for stt in range(2):
                        S.add("dve", lambda e, pgP=pgP, stt=stt, tb=tb, n=n: e.scalar_tensor_tensor(
                            SelTs[ex % 2][:, stt, tb * 512:tb * 512 + n], pgP[:, 0:n], pidx[:, stt:stt + 1], Gbs[:, tb * 512:tb * 512 + n],
                            ALU.is_equal, ALU.mult), r=[pgPk, "pidx", ("Gbs", tb)], w=[("SelT", ex % 2, stt, tb)])
                for j in range(NJ):
                    S.add("dve", lambda e, j=j, ex=ex: e.tensor_scalar(
                        Sel[:, j, :], io_row[:, 0:CAP], pos[:, j, ex:ex + 1], mask[:, j, ex:ex + 1], ALU.is_equal, ALU.mult),
                        r=["io_row", ("pos", j), ("mask", j)], w=[("Sel", j)])
            def moe_gather(ex, fcs):
                for fc in fcs:
                    pg, pk = bank()
                    for j in range(NJ):
                        S.add("pe", lambda e, pg=pg, fc=fc, j=j: e.matmul(pg[:, 0:CAP], h2[:, j, fc * 128:(fc + 1) * 128], Sel[:, j, :],
                                                                      start=(j == 0), stop=(j == NJ - 1)),
                              r=[("h2", j), ("Sel", j)], w=[pk])
                    S.add("act", lambda e, pg=pg, fc=fc: e.copy(XeT[:, fc, :], pg[:, 0:CAP]), r=[pk], w=[("XeT", fc)])
            def moe_gu(ex, p_lo, p_hi):
                for piece in range(p_lo, p_hi):
                    wb, wk = wload(w_gate_up[ex][:, piece * 512:(piece + 1) * 512])
                    for sub in range(4):
                        nci = piece * 4 + sub
                        pg, pk = bank()
                        for fc in range(16):
                            S.add("pe", lambda e, pg=pg, fc=fc, sub=sub, wb=wb: e.matmul(
                                pg[:, 0:CAP], wb[:, fc, sub * 128:(sub + 1) * 128], XeT[:, fc, :], start=(fc == 0), stop=(fc == 15)),
                                r=XeT_keys + [wk], w=[pk])
                        gi = nci % 2
                        bias_ap = bguT[:, nci, ex:ex + 1]
                        if nci < 16:
                            S.add("dve", lambda e, pg=pg, gi=gi, bias_ap=bias_ap: e.tensor_scalar(
                                glc[gi], pg[:, 0:CAP], bias_ap, 7.0, ALU.add, ALU.min), r=[pk, ("bguT", nci // 16)], w=[f"glc{gi}"])
                            S.add("act", lambda e, gi=gi: e.activation(sgm[gi], glc[gi], AF.Sigmoid, scale=1.702), r=[f"glc{gi}"], w=[f"sgm{gi}"])
                            S.add("dve", lambda e, gi=gi, nci=nci: e.tensor_tensor(actT[:, nci, :], glc[gi], sgm[gi], ALU.mult),
                                  r=[f"glc{gi}", f"sgm{gi}"], w=[("actT", nci)])
                        else:
                            m_ = nci - 16
                            S.add("dve", lambda e, pg=pg, gi=gi, bias_ap=bias_ap: e.tensor_scalar(
                                glc[gi], pg[:, 0:CAP], bias_ap, 7.0, ALU.add, ALU.min), r=[pk, ("bguT", nci // 16)], w=[f"glc{gi}"])
                            S.add("dve", lambda e, gi=gi: e.tensor_scalar(sgm[gi], glc[gi], -7.0, 1.0, ALU.max, ALU.add),
                                  r=[f"glc{gi}"], w=[f"sgm{gi}"])
                            S.add("dve", lambda e, gi=gi, m_=m_: e.tensor_tensor(actT[:, m_, :], actT[:, m_, :], sgm[gi], ALU.mult),
                                  r=[("actT", m_), f"sgm{gi}"], w=[("actT", m_)])
            def moe_down(ex, fbs):
                for fb in fbs:
                    wb, wk = wload(w_down[ex][:, fb * 512:(fb + 1) * 512])
                    for stt in range(2):
                        pg, pk = bank()
                        for mc in range(16):
                            S.add("pe", lambda e, pg=pg, mc=mc, stt=stt, wb=wb: e.matmul(
                                pg, actT[:, mc, stt * 128:(stt + 1) * 128], wb[:, mc, :], start=(mc == 0), stop=(mc == 15)),
                                r=actT_keys + [wk], w=[pk])
                        S.add("act", lambda e, pg=pg, stt=stt, fb=fb: e.copy(Oe[:, stt, fb * 512:(fb + 1) * 512], pg), r=[pk], w=[("Oe", stt, fb)])
            def moe_scatter(ex, tiles):
                for (j, fb) in tiles:
                    if True:
                        pg, pk = bank()
                        for stt in range(2):
                            S.add("pe", lambda e, pg=pg, stt=stt, j=j, fb=fb: e.matmul(
                                pg, SelTs[ex % 2][:, stt, j * 128:(j + 1) * 128], Oe[:, stt, fb * 512:(fb + 1) * 512], start=(stt == 0), stop=(stt == 1)),
                                r=[("SelT", ex % 2, stt, j // 4), ("Oe", stt, fb)], w=[pk])
                        xv = x1[:, j, fb * 512:(fb + 1) * 512]
                        S.add("dve", lambda e, pg=pg, xv=xv: e.tensor_tensor(xv, xv, pg, ALU.add), r=[pk, ("x1", j)], w=[("x1", j)])

            XeT_keys = [("XeT", fc) for fc in range(16)]
            actT_keys = [("actT", m_) for m_ in range(16)]
            SelTs = [SelT, gain[0].bitcast(BF16)[:, 0:2048].rearrange("p (a b) -> p a b", a=2)]
            sc_tiles = [(j, fb) for j in range(NJ) for fb in range(4)]
            nsc = (len(sc_tiles) + 7) // 8
            moe_prep(0)
            moe_gather(0, range(16))
            for ex in range(NE):
                for k in range(8):
                    moe_gu(ex, k, k + 1)
                    if ex > 0:
                        moe_scatter(ex - 1, sc_tiles[k * nsc:(k + 1) * nsc])
                if ex + 1 < NE:
                    moe_prep(ex + 1)
                for fb in range(4):
                    moe_down(ex, [fb])
                    if ex + 1 < NE:
                        moe_gather(ex + 1, range(4 * fb, 4 * fb + 4))
            moe_scatter(NE - 1, sc_tiles)
            S.barrier()
            S.add("sp", lambda e: e.dma_start(out=bgr[0:32, 0:D], in_=b_down[:, :]), w=["bgr"], dma=True)
            for j in range(NJ):
                for fb in range(4):
                    pg, pk = bank()
                    S.add("pe", lambda e, pg=pg, j=j, fb=fb: e.matmul(pg, GT[0:32, j * 128:(j + 1) * 128], bgr[0:32, fb * 512:(fb + 1) * 512],
                                                                    start=True, stop=True), r=[("GT", j), "bgr"], w=[pk])
                    xv = x1[:, j, fb * 512:(fb + 1) * 512]
                    S.add("dve", lambda e, pg=pg, xv=xv: e.tensor_tensor(xv, xv, pg, ALU.add), r=[pk, ("x1", j)], w=[("x1", j)])
            if dbg:
                for j in range(NJ):
                    S.add("sp", lambda e, j=j: e.dma_start(out=dbg_t["x2"][j * 128:(j + 1) * 128, :], in_=x1[:, j, :]),
                          r=[("x1", j)], dma=True)

        S.barrier()
        h3T = at(64, BF16, [16, 1024])
        AR.off = R1 + 96 * 1024
        h3b = AR.alloc(BF16, [128, D])
        pT_ = AR.alloc(BF16, [2, 1024])
        pin = AR.alloc(F32, [128, 256])
        pinb = AR.alloc(BF16, [128, 256])
        u = AR.alloc(F32, [128, D])
        sgE = [AR.alloc(F32, [128, 512]) for _ in range(2)]
        obuf = AR.alloc(F32, [128, D])
        smE = AR.alloc(F32, [8, 4])
        if "E" in phases:
            load_gain(0, g_ple)
            for j in range(NJ):
                xj = x1[:, j, :]
                S.add("act", lambda e, xj=xj, j=j: e.activation(junkB2, xj, AF.Square, accum_out=smE[:, j, 0:1]),
                      r=[("x1", j)], w=["junkB2", ("smE", j)])
                S.add("act", lambda e, j=j: e.activation(smE[:, j, 0:1], smE[:, j, 0:1], AF.Ln, bias=EPS, scale=1.0 / D), r=[("smE", j)], w=[("smE", j)])
                S.add("act", lambda e, j=j: e.activation(smE[:, j, 0:1], smE[:, j, 0:1], AF.Exp, scale=-0.5), r=[("smE", j)], w=[("smE", j)])
                S.add("dve", lambda e, xj=xj, j=j: e.scalar_tensor_tensor(h3b, xj, smE[:, j, 0:1], gain[0], ALU.mult, ALU.mult),
                      r=[("x1", j), ("smE", j), "gain0"], w=["h3b"])
                transpose_rows(h3b, "h3b", h3T, lambda half, j=j: ("h3T", j, half), j * 128)
                S.add("sp", lambda e, j=j: e.dma_start(out=pin, in_=p_own[j * 128:(j + 1) * 128, :]), w=["pin"], dma=True)
                S.add("dve", lambda e: e.tensor_copy(pinb, pin), r=["pin"], w=["pinb"])
                pt, ptk = tbank()
                for k in range(2):
                    S.add("pe", lambda e, pt=pt, k=k: e.transpose(pt[:, k * 128:(k + 1) * 128], pinb[:, k * 128:(k + 1) * 128], ident),
                          r=["pinb", "ident"], w=[ptk])
                S.add("act", lambda e, pt=pt, j=j: e.copy(pT_[:, :, j * 128:(j + 1) * 128], pt[:, 0:256].rearrange("p (a b) -> p a b", a=2)),
                      r=[ptk], w=[("pT", j)])
            load_gain(0, g_ple_post)
            load_gain(1, g_final)
            for j in range(NJ):
                for fb in range(4):
                    wb, wk = wload(w_ple_gate[:, fb * 512:(fb + 1) * 512])
                    wp, wpk = wload(w_ple_proj[:, fb * 512:(fb + 1) * 512], rows=256)
                    pg, pk = bank()
                    for fc in range(16):
                        S.add("pe", lambda e, pg=pg, fc=fc, j=j, wb=wb: e.matmul(pg, h3T[:, fc, j * 128:(j + 1) * 128], wb[:, fc, :],
                                                                             start=(fc == 0), stop=(fc == 15)),
                              r=[("h3T", j, fc // 8), wk], w=[pk])
                    pp_, ppk = bank()
                    for k in range(2):
                        S.add("pe", lambda e, pp_=pp_, k=k, j=j, wp=wp: e.matmul(pp_, pT_[:, k, j * 128:(j + 1) * 128], wp[:, k, :],
                                                                             start=(k == 0), stop=(k == 1)),
                              r=[("pT", j), wpk], w=[ppk])
                    si = fb % 2
                    S.add("act", lambda e, pg=pg, si=si: e.activation(sgE[si], pg, AF.Sigmoid), r=[pk], w=[f"sgE{si}"])
                    S.add("dve", lambda e, pp_=pp_, si=si, fb=fb: e.tensor_tensor(u[:, fb * 512:(fb + 1) * 512], pp_, sgE[si], ALU.mult),
                          r=[ppk, f"sgE{si}"], w=[("u", fb)])
                ukeys = [("u", fb) for fb in range(4)]
                S.add("act", lambda e, j=j: e.activation(junkB2, u, AF.Square, accum_out=smE[:, j, 1:2]), r=ukeys, w=["junkB2", ("smE1", j)])
                S.add("act", lambda e, j=j: e.activation(smE[:, j, 1:2], smE[:, j, 1:2], AF.Ln, bias=EPS, scale=1.0 / D), r=[("smE1", j)], w=[("smE1", j)])
                S.add("act", lambda e, j=j: e.activation(smE[:, j, 1:2], smE[:, j, 1:2], AF.Exp, scale=-0.5), r=[("smE1", j)], w=[("smE1", j)])
                S.add("dve", lambda e, j=j: e.scalar_tensor_tensor(u, u, smE[:, j, 1:2], gain[0], ALU.mult, ALU.mult),
                      r=ukeys + [("smE1", j), "gain0"], w=ukeys)
                xj = x1[:, j, :]
                S.add("dve", lambda e, xj=xj: e.tensor_tensor(xj, xj, u, ALU.add), r=ukeys + [("x1", j)], w=[("x1", j)])
                S.add("act", lambda e, xj=xj, j=j: e.activation(junkB2, xj, AF.Square, accum_out=smE[:, j, 2:3]), r=[("x1", j)], w=["junkB2", ("smE2", j)])
                S.add("act", lambda e, j=j: e.activation(smE[:, j, 2:3], smE[:, j, 2:3], AF.Ln, bias=EPS, scale=1.0 / D), r=[("smE2", j)], w=[("smE2", j)])
                S.add("act", lambda e, j=j: e.activation(smE[:, j, 2:3], smE[:, j, 2:3], AF.Exp, scale=-0.5), r=[("smE2", j)], w=[("smE2", j)])
                S.add("dve", lambda e, xj=xj, j=j: e.scalar_tensor_tensor(obuf, xj, smE[:, j, 2:3], gain[1], ALU.mult, ALU.mult),
                      r=[("x1", j), ("smE2", j), "gain1"], w=["obuf"])
                S.add("sp", lambda e, j=j: e.dma_start(out=out[j * 128:(j + 1) * 128, :], in_=obuf), r=["obuf"], dma=True)
                out_dmas.append(len(S.ops) - 1)

        S.barrier()
        outs = [i for i, o in enumerate(S.ops) if o["dma"]]
        S.wait_all("sp", outs[-32:] + out_dmas)
        S.emit()
    return nc


def make_inputs(inputs, core):
    b, q = core // 4, core % 4
    x = np.asarray(inputs["x"], dtype=np.float32)
    p = np.asarray(inputs["p"], dtype=np.float32)
    own = np.concatenate([np.arange((4 * j + q) * 128, (4 * j + q + 1) * 128) for j in range(8)])
    m = {}
    pad = 3 - q
    xa = np.zeros((SEQ, D), np.float32)
    xa[pad * 128:] = x[b][:SEQ - pad * 128]
    m["x_all"] = xa
    m["x_own"] = np.ascontiguousarray(x[b][own])
    m["p_own"] = np.ascontiguousarray(p[0, b][own])
    ms = np.zeros((128, 4), np.float32)
    for kb in range(4):
        ms[:, kb] = 1.0 if kb >= pad else 0.0
    m["msel"] = ms
    am = np.zeros((128, 4, 128), np.float32)
    am[:, 3, :] = (np.arange(128)[:, None] < np.arange(128)[None, :]).astype(np.float32)
    m["amask"] = am
    for k in ["w_in", "conv_w", "conv_b", "dt_bias", "a_log", "d_skip", "ssd_norm_w", "w_branch_a", "w_branch_b",
              "w_out", "g_mix", "g_ffn", "w_router", "b_router", "w_gate_up", "b_gate_up", "w_down", "b_down",
              "g_ple", "w_ple_gate", "w_ple_proj", "g_ple_post"]:
        m[k] = np.ascontiguousarray(np.asarray(inputs[k], dtype=np.float32)[0])
    m["g_final"] = np.ascontiguousarray(np.asarray(inputs["g_final"], dtype=np.float32))
    return m, own


def kernel(**inputs):
    nc = build()
    in_maps = []
    owns = []
    for c in range(8):
        m, own = make_inputs(inputs, c)
        in_maps.append(m)
        owns.append(own)
    res = run_bass_kernel_spmd(nc, in_maps, core_ids=list(range(8)))
    outp = np.zeros((2, SEQ, D), np.float32)
    for c in range(8):
        outp[c // 4, owns[c]] = res.results[c]["out"]
    return outp
```
